# Optimizing a Trainium2 kernel written in Bass

```python
import math
import jax, jax.numpy as jnp
from jax import lax
import numpy as np

D_MODEL = 2048
BATCH = 4
SEQ = 8192
DEPTH = 2

D_SSM = D_MODEL // 4
SSM_GROUP = 16
N_SSM_GROUPS = D_SSM // SSM_GROUP
SSM_STATE = 64
D_SG = 3 * D_MODEL // 8
SG_HEADS = 8
SG_HEAD_DIM = D_SG // SG_HEADS
SG_CHUNK = 128
D_CONV = 3 * D_MODEL // 8
CONV_GROUPS = 8
CONV_WIDTH = 3
N_BRANCHES = 3
SPLIT_SIZES = (D_SSM, D_SG, D_SG, D_CONV, D_CONV, D_CONV)
SPLIT_POINTS = tuple(int(s) for s in np.cumsum(SPLIT_SIZES))
D_IN_PROJ = sum(SPLIT_SIZES) + N_BRANCHES * D_MODEL

FFN_DIM = 256 * ((8 * D_MODEL // 3 + 255) // 256)
N_EXPERTS = 8
TOP_K = 2
EXPERT_DIM = 7 * D_MODEL // 2
MOE_BLOCK = 512
N_DENSE = (DEPTH + 1) // 2
N_MOE = DEPTH // 2

DEEPNORM_ALPHA = (2.0 * DEPTH) ** 0.25
DEEPNORM_BETA = (8.0 * DEPTH) ** -0.25
LN_EPS = 1e-5

kernel_name = "hybrid_ssm_gmlp_shortconv_moe_deepnorm_adaln"


def layer_norm(z):
    z32 = z.astype(jnp.float32)
    mu = jnp.mean(z32, axis=-1, keepdims=True)
    var = jnp.mean(jnp.square(z32 - mu), axis=-1, keepdims=True)
    return ((z32 - mu) * lax.rsqrt(var + LN_EPS)).astype(z.dtype)


def ssm_branch(u, a_re, a_im, log_dt, b_re, b_im, c_re, c_im, d_skip, glu_w, glu_b):
    bsz, seq, _ = u.shape
    f32 = jnp.float32
    u32 = u.astype(f32).reshape(bsz, seq, N_SSM_GROUPS, SSM_GROUP)
    a_re, a_im = a_re.astype(f32), a_im.astype(f32)
    b_re, b_im = b_re.astype(f32), b_im.astype(f32)
    c_re, c_im = c_re.astype(f32), c_im.astype(f32)
    dt = jnp.exp(log_dt.astype(f32))[:, None]
    mag = jnp.exp(dt * a_re)
    ang = dt * a_im
    abar_re = mag * jnp.cos(ang)
    abar_im = mag * jnp.sin(ang)
    den = jnp.square(a_re) + jnp.square(a_im)
    num_re = abar_re - 1.0
    f_re = (num_re * a_re + abar_im * a_im) / den
    f_im = (abar_im * a_re - num_re * a_im) / den
    bbar_re = f_re[..., None] * b_re - f_im[..., None] * b_im
    bbar_im = f_re[..., None] * b_im + f_im[..., None] * b_re
    bu_re = jnp.einsum('bsgc,gnc->bsgn', u32, bbar_re)
    bu_im = jnp.einsum('bsgc,gnc->bsgn', u32, bbar_im)
    lam_re = jnp.broadcast_to(abar_re, (1, seq, N_SSM_GROUPS, SSM_STATE))
    lam_im = jnp.broadcast_to(abar_im, (1, seq, N_SSM_GROUPS, SSM_STATE))

    def combine(left, right):
        ar1, ai1, br1, bi1 = left
        ar2, ai2, br2, bi2 = right
        return (ar2 * ar1 - ai2 * ai1,
                ar2 * ai1 + ai2 * ar1,
                ar2 * br1 - ai2 * bi1 + br2,
                ar2 * bi1 + ai2 * br1 + bi2)

    _, _, h_re, h_im = lax.associative_scan(combine, (lam_re, lam_im, bu_re, bu_im), axis=1)
    y = (jnp.einsum('bsgn,gcn->bsgc', h_re, c_re)
         - jnp.einsum('bsgn,gcn->bsgc', h_im, c_im)
         + d_skip.astype(f32) * u32)
    y = y.reshape(bsz, seq, D_SSM).astype(u.dtype)
    z = jax.nn.gelu(y)
    return z * jax.nn.sigmoid(z @ glu_w + glu_b)


def spatial_gating_branch(u, v, ln_g, ln_b, w_s, b_s):
    bsz, seq, _ = u.shape
    v = layer_norm(v) * ln_g + ln_b
    v = v.reshape(bsz, seq // SG_CHUNK, SG_CHUNK, SG_HEADS, SG_HEAD_DIM)
    mask = jnp.tril(jnp.ones((SG_CHUNK, SG_CHUNK), dtype=bool))
    w = jnp.where(mask[None], w_s, jnp.zeros_like(w_s))
    s = jnp.einsum('hts,bnshd->bnthd', w, v) + b_s.T[None, None, :, :, None]
    return u * s.reshape(bsz, seq, D_SG)


def short_conv_branch(b_gate, c_gate, h_in, conv_w):
    z = c_gate * h_in
    y = lax.conv_general_dilated(z, conv_w, window_strides=(1,),
                                 padding=[(CONV_WIDTH - 1, 0)],
                                 dimension_numbers=('NWC', 'WIO', 'NWC'),
                                 feature_group_count=D_CONV)
    return b_gate * y


def hybrid_mixer(h, w_in, a_re, a_im, log_dt, b_re, b_im, c_re, c_im, d_skip,
                 glu_w, glu_b, sg_ln_g, sg_ln_b, sg_w, sg_b, conv_w,
                 w_branch_a, w_branch_b, w_branch_c, w_o):
    bsz, seq, _ = h.shape
    proj = h @ w_in
    u_ssm, u_sg, v_sg, b_cv, c_cv, h_cv, gate_logits = jnp.split(proj, SPLIT_POINTS, axis=-1)
    ya = ssm_branch(u_ssm, a_re, a_im, log_dt, b_re, b_im, c_re, c_im, d_skip, glu_w, glu_b) @ w_branch_a
    yb = spatial_gating_branch(u_sg, v_sg, sg_ln_g, sg_ln_b, sg_w, sg_b) @ w_branch_b
    yc = short_conv_branch(b_cv, c_cv, h_cv, conv_w) @ w_branch_c
    gates = jax.nn.sigmoid(gate_logits.reshape(bsz, seq, N_BRANCHES, D_MODEL))
    merged = gates[:, :, 0] * ya + gates[:, :, 1] * yb + gates[:, :, 2] * yc
    return merged @ w_o


def swiglu(h, w13, w2):
    gate, up = jnp.split(h @ w13, 2, axis=-1)
    return (jax.nn.silu(gate) * up) @ w2


def moe_swiglu(h, router_w, router_b, w13, w2):
    bsz, seq, dm = h.shape
    n_tok = bsz * seq
    ht = h.reshape(n_tok, dm)
    logits = (ht @ router_w).astype(jnp.float32) + router_b.astype(jnp.float32)
    top_logits, top_idx = lax.top_k(logits, TOP_K)
    top_w = jax.nn.softmax(top_logits, axis=-1).astype(h.dtype)
    n_pairs = TOP_K * n_tok
    flat_e = top_idx.reshape(-1)
    flat_tok = jnp.repeat(jnp.arange(n_tok, dtype=jnp.int32), TOP_K)
    flat_w = top_w.reshape(-1)
    order = jnp.argsort(flat_e)
    sorted_e = flat_e[order]
    counts = jnp.bincount(flat_e, length=N_EXPERTS)
    padded = ((counts + MOE_BLOCK - 1) // MOE_BLOCK) * MOE_BLOCK
    ends_p = jnp.cumsum(padded)
    starts_p = ends_p - padded
    starts = jnp.cumsum(counts) - counts
    rank = jnp.arange(n_pairs) - starts[sorted_e]
    dest = starts_p[sorted_e] + rank
    buf_len = ((n_pairs + MOE_BLOCK - 1) // MOE_BLOCK) * MOE_BLOCK + N_EXPERTS * MOE_BLOCK
    n_blocks = buf_len // MOE_BLOCK
    buf_tok = jnp.full((buf_len,), n_tok, dtype=jnp.int32).at[dest].set(flat_tok[order])
    buf_w = jnp.zeros((buf_len,), h.dtype).at[dest].set(flat_w[order])
    block_e = jnp.minimum(jnp.searchsorted(ends_p, jnp.arange(n_blocks) * MOE_BLOCK, side='right'),
                          N_EXPERTS - 1)
    x_pad = jnp.concatenate([ht, jnp.zeros((1, dm), ht.dtype)], axis=0)
    xs = x_pad[buf_tok].reshape(n_blocks, MOE_BLOCK, dm)

    def expert_block(args):
        xb, e = args
        return swiglu(xb, w13[e], w2[e])

    ys = lax.map(expert_block, (xs, block_e)).reshape(buf_len, dm)
    out = jax.ops.segment_sum(ys * buf_w[:, None], buf_tok, num_segments=n_tok + 1)[:n_tok]
    return out.reshape(bsz, seq, dm)


def adaln_post_norm(x, y, shift, scale, gate):
    z = DEEPNORM_ALPHA * x + (1.0 + gate)[:, None, :] * y
    return layer_norm(z) * (1.0 + scale)[:, None, :] + shift[:, None, :]


def setup_inputs(seed: int = 0) -> dict:
    key = jax.random.key(seed)
    ks = jax.random.split(key, 32)
    nrm = jax.random.normal
    L, G, N, C = DEPTH, N_SSM_GROUPS, SSM_STATE, SSM_GROUP
    a_im_init = jnp.pi * jnp.arange(N, dtype=jnp.float32)
    return {
        "x": nrm(ks[0], (BATCH, SEQ, D_MODEL), jnp.float32),
        "c": nrm(ks[1], (BATCH, D_MODEL), jnp.float32),
        "w_in": nrm(ks[2], (L, D_MODEL, D_IN_PROJ)) * D_MODEL ** -0.5,
        "ssm_a_re": -0.5 + 0.01 * nrm(ks[3], (L, G, N)),
        "ssm_a_im": a_im_init + 0.01 * nrm(ks[4], (L, G, N)),
        "ssm_log_dt": jax.random.uniform(ks[5], (L, G), minval=math.log(1e-3), maxval=math.log(1e-1)),
        "ssm_b_re": nrm(ks[6], (L, G, N, C)) * (2.0 * C) ** -0.5,
        "ssm_b_im": nrm(ks[7], (L, G, N, C)) * (2.0 * C) ** -0.5,
        "ssm_c_re": nrm(ks[8], (L, G, C, N)) * (2.0 * N) ** -0.5,
        "ssm_c_im": nrm(ks[9], (L, G, C, N)) * (2.0 * N) ** -0.5,
        "ssm_d": nrm(ks[10], (L, G, C)),
        "glu_w": nrm(ks[11], (L, D_SSM, D_SSM)) * D_SSM ** -0.5,
        "glu_b": 0.02 * nrm(ks[12], (L, D_SSM)),
        "sg_ln_g": 1.0 + 0.02 * nrm(ks[13], (L, D_SG)),
        "sg_ln_b": 0.02 * nrm(ks[14], (L, D_SG)),
        "sg_w": nrm(ks[15], (L, SG_HEADS, SG_CHUNK, SG_CHUNK)) * SG_CHUNK ** -0.5,
        "sg_b": 1.0 + 0.02 * nrm(ks[16], (L, SG_HEADS, SG_CHUNK)),
        "conv_w": nrm(ks[17], (L, CONV_WIDTH, 1, D_CONV)) * CONV_WIDTH ** -0.5,
        "w_branch_a": nrm(ks[18], (L, D_SSM, D_MODEL)) * D_SSM ** -0.5,
        "w_branch_b": nrm(ks[19], (L, D_SG, D_MODEL)) * D_SG ** -0.5,
        "w_branch_c": nrm(ks[20], (L, D_CONV, D_MODEL)) * D_CONV ** -0.5,
        "w_o": nrm(ks[21], (L, D_MODEL, D_MODEL)) * (D_MODEL ** -0.5 * DEEPNORM_BETA),
        "ada_w": nrm(ks[22], (L, D_MODEL, 6 * D_MODEL)) * (0.1 * D_MODEL ** -0.5),
        "ada_b": 0.02 * nrm(ks[23], (L, 6 * D_MODEL)),
        "ffn_w13": nrm(ks[24], (N_DENSE, D_MODEL, 2 * FFN_DIM)) * D_MODEL ** -0.5,
        "ffn_w2": nrm(ks[25], (N_DENSE, FFN_DIM, D_MODEL)) * (FFN_DIM ** -0.5 * DEEPNORM_BETA),
        "moe_router_w": nrm(ks[26], (N_MOE, D_MODEL, N_EXPERTS)) * D_MODEL ** -0.5,
        "moe_router_b": 0.01 * nrm(ks[27], (N_MOE, N_EXPERTS)),
        "moe_w13": nrm(ks[28], (N_MOE, N_EXPERTS, D_MODEL, 2 * EXPERT_DIM)) * D_MODEL ** -0.5,
        "moe_w2": nrm(ks[29], (N_MOE, N_EXPERTS, EXPERT_DIM, D_MODEL)) * (EXPERT_DIM ** -0.5 * DEEPNORM_BETA),
    }


def reference(x, c, w_in, ssm_a_re, ssm_a_im, ssm_log_dt, ssm_b_re, ssm_b_im, ssm_c_re, ssm_c_im,
              ssm_d, glu_w, glu_b, sg_ln_g, sg_ln_b, sg_w, sg_b, conv_w,
              w_branch_a, w_branch_b, w_branch_c, w_o, ada_w, ada_b,
              ffn_w13, ffn_w2, moe_router_w, moe_router_b, moe_w13, moe_w2):
    for l in range(DEPTH):
        mod = c @ ada_w[l] + ada_b[l]
        sh_m, sc_m, g_m, sh_f, sc_f, g_f = jnp.split(mod, 6, axis=-1)
        y = hybrid_mixer(x, w_in[l], ssm_a_re[l], ssm_a_im[l], ssm_log_dt[l],
                         ssm_b_re[l], ssm_b_im[l], ssm_c_re[l], ssm_c_im[l], ssm_d[l],
                         glu_w[l], glu_b[l], sg_ln_g[l], sg_ln_b[l], sg_w[l], sg_b[l], conv_w[l],
                         w_branch_a[l], w_branch_b[l], w_branch_c[l], w_o[l])
        x = adaln_post_norm(x, y, sh_m, sc_m, g_m)
        if l % 2 == 0:
            y = swiglu(x, ffn_w13[l // 2], ffn_w2[l // 2])
        else:
            y = moe_swiglu(x, moe_router_w[l // 2], moe_router_b[l // 2], moe_w13[l // 2], moe_w2[l // 2])
        x = adaln_post_norm(x, y, sh_f, sc_f, g_f)
    return x
```

```python
import contextlib
import math
import numpy as np
import concourse.bass as bass
import concourse.mybir as mybir
from concourse.bass_utils import run_bass_kernel_spmd

F32 = mybir.dt.float32
BF16 = mybir.dt.bfloat16
AF = mybir.ActivationFunctionType
ALU = mybir.AluOpType

D = 2048
KC = 16
T = 512
DIN = 10496
FFN = 5632
EXP = 7168
NE = 8
ALPHA = 4.0 ** 0.25
EPS = 1e-5
TWO_PI = 2.0 * math.pi
MAGIC = 12582912.0
PI_CL = 3.1415925


class Buf:
    __slots__ = ("w", "r", "name")

    def __init__(self, name=""):
        self.w = None
        self.r = {}
        self.name = name


class Ctx:
    SELF_WAIT = True

    def __init__(self, nc, es):
        self.nc = nc
        self.es = es
        self.eng = {"pe": nc.tensor, "act": nc.scalar, "dve": nc.vector, "pool": nc.gpsimd, "sp": nc.sync}
        self.sems = {}
        self.cnt = {}
        self.waited = {e: {} for e in self.eng}
        self.pending = {e: [] for e in self.eng}
        self.nops = 0

    def sem(self, key):
        if key not in self.sems:
            self.sems[key] = self.es.enter_context(self.nc.semaphore(key))
            self.cnt[key] = 0
        return self.sems[key]

    def wait(self, e, ev):
        if ev is None:
            return
        k, v = ev
        if self.waited[e].get(k, 0) >= v:
            return
        if k == "c_" + e and (e == "pe" or not self.SELF_WAIT):
            return
        self.eng[e].wait_ge(self.sems[k], v)
        self.waited[e][k] = v

    def _deps(self, e, reads, writes):
        for b in reads:
            self.wait(e, b.w)
        for b in writes:
            self.wait(e, b.w)
            for k, v in list(b.r.items()):
                self.wait(e, (k, v))

    def op(self, e, fn, reads=(), writes=(), inc=True):
        self._deps(e, reads, writes)
        ins = fn()
        self.nops += 1
        pend = self.pending[e]
        for b in reads:
            pend.append((b, 0))
        for b in writes:
            pend.append((b, 1))
        if inc:
            k = "c_" + e
            self.sem(k)
            self.cnt[k] += 1
            ins.then_inc(self.sems[k], 1)
            ev = (k, self.cnt[k])
            for b, iw in pend:
                if iw:
                    b.w = ev
                    b.r = {}
            for b, iw in pend:
                if not iw:
                    b.r[k] = ev[1]
            pend.clear()
            return ev
        return None

    def dma(self, q, out_ap, in_ap, semkey, reads=(), writes=()):
        self._deps(q, reads, writes)
        self.sem(semkey)
        self.cnt[semkey] += 16
        self.eng[q].dma_start(out=out_ap, in_=in_ap).then_inc(self.sems[semkey], 16)
        ev = (semkey, self.cnt[semkey])
        self.nops += 1
        for b in writes:
            b.w = ev
            b.r = {}
        for b in reads:
            b.r[semkey] = ev[1]
        return ev

    def sync_group(self, key, bufs):
        ev = (key, self.cnt[key])
        for b in bufs:
            b.w = ev

    def barrier(self):
        for e in self.eng:
            assert not self.pending[e], e
        for e in self.eng:
            for k, v in self.cnt.items():
                if v > 0:
                    self.wait(e, (k, v))


class Pool:
    UID = [0]

    def __init__(self, nc, es, name, n, shape, dt):
        Pool.UID[0] += 1
        self.name = name
        name = f"{name}u{Pool.UID[0]}_"
        self.t = [es.enter_context(nc.sbuf_tensor(f"{name}{i}", shape, dt)) for i in range(n)]
        self.b = [Buf(f"{name}{i}") for i in range(n)]
        self.i = 0
        self.n = n
        self.uses = 0
        self.R = {"w": 3, "w2_": 4}.get(self.name, 1)

    def get(self):
        i = self.i
        self.i = (i + 1) % self.n
        r = (self.uses // self.n) % self.R
        self.uses += 1
        return self.t[i], self.b[i], f"{self.name}{i}r{r}"


class Prog:
    def __init__(self, NT, do_moe=True, debug=False, n_layers=2, stop_after=None, single=False):
        self.single = single
        self.stop_after = stop_after
        import os as _os
        self.ckpt = int(_os.environ.get("DBG_CKPT", "0"))
        self.phase_i = 0
        self.NT = NT
        self.NTOK = NT * T
        self.debug = debug
        self.do_moe = do_moe
        self.n_layers = n_layers
        nc = bass.Bass("TRN2", target_bir_lowering=False)
        self.nc = nc
        self.I = {}
        NTOK = self.NTOK

        def inp(name, shape):
            self.I[name] = nc.dram_tensor(name, list(shape), F32, kind="ExternalInput").ap()

        self.in_shapes = {
            "xA": (D, NTOK), "xB": (D, NTOK), "cT": (128, KC), "flag": (128, 1),
            "ident": (128, 128), "swapm": (128, 128), "tril": (128, 128), "iota": (128, T),
            "sgn": (128, 2), "rowmask": (128, 8),
            "w_in": (2, D, DIN), "a_re2": (2, 128, 32), "a_im2": (2, 128, 32), "ldt2": (2, 128, 32),
            "bA": (2, 128, 32, 16), "bB": (2, 128, 32, 16), "cA": (2, 128, 32, 16), "cB": (2, 128, 32, 16),
            "dcol": (2, 128, 4), "glu_w": (2, 512, 512), "glu_bc": (2, 128, 4),
            "sg_ln_g": (2, 768), "sg_ln_b": (2, 768), "sgwT": (2, 8, 128, 128), "sg_b": (2, 1, 1024),
            "convw": (2, 128, 6, 3), "w_branch_a": (2, 512, D), "w_branch_b": (2, 768, D),
            "w_branch_c": (2, 768, D), "w_o": (2, D, D), "ada_w": (2, D, 6 * D), "ada_bc": (2, 128, 96),
            "ffn_w13": (1, D, 2 * FFN), "ffn_w2": (1, FFN, D),
            "router_w": (D, NE), "router_b4": (1, 4 * NE),
            "moe_w13": (NE, D, 2 * EXP), "moe_w2": (NE, EXP, D),
        }
        if not do_moe:
            for k in ("router_w", "router_b4", "moe_w13", "moe_w2"):
                del self.in_shapes[k]
        prog_self = self

        class LazyI(dict):
            def __missing__(d, k):
                inp(k, prog_self.in_shapes[k])
                return d[k]
        self.I = LazyI()
        self.out = nc.dram_tensor("outT", [D, NTOK], F32, kind="ExternalOutput").ap()
        dk = "ExternalOutput" if debug else "Internal"

        def scr(name, shape, dt=F32, kind="Internal"):
            return nc.dram_tensor(name, list(shape), dt, kind=kind).ap()

        self.S = {
            "xmA": scr("xmA", (D, NTOK)), "x1A": scr("x1A", (D, NTOK)),
            "xmB": scr("xmB", (D, NTOK), kind=dk), "x1B": scr("x1B", (D, NTOK), kind=dk),
            "xm2": scr("xm2", (D, NTOK), kind=dk),
            "zsc": scr("zsc", (D, T)),
            "ssmW": scr("ssmW", (128, 32, 4, 128), BF16), "rot": scr("rot", (128, 32, 128)),
            "tabs": scr("tabs", (32, 128, 2, T)),
        }
        self.dB = {}
        with contextlib.ExitStack() as es:
            self.es = es
            self.c = Ctx(nc, es)
            self.build()

    def db(self, *key):
        if key not in self.dB:
            self.dB[key] = Buf(str(key))
        return self.dB[key]

    def gsb(self, name, shape, dt=F32):
        return self.es.enter_context(self.nc.sbuf_tensor(name, list(shape), dt)), Buf(name)

    def bank(self):
        i = self.rot_banks[self.bi]
        self.bi = (self.bi + 1) % len(self.rot_banks)
        return self.ps[i], self.psB[i]

    def mm(self, out, lhsT, rhs, start, stop, reads, wb, inc=None):
        nc = self.nc
        self.c.op("pe", lambda: nc.tensor.matmul(out, lhsT=lhsT, rhs=rhs, start=start, stop=stop),
                  reads=reads, writes=[wb], inc=(stop if inc is None else inc))

    def wload(self, src3, shape, q="pool"):
        t, b, key = self.wp.get()
        n = 1
        for s in shape[1:]:
            n *= s
        if len(shape) == 3:
            v = t[0:shape[0], 0:n].rearrange("p (k n) -> p k n", k=shape[1])
        else:
            v = t[0:shape[0], 0:n]
        self.c.dma(q, v, src3, "d_" + key, writes=[b])
        return v, b

    def act(self, out, in_, func, reads, writes, **kw):
        nc = self.nc
        return self.c.op("act", lambda: nc.scalar.activation(out=out, in_=in_, func=func, **kw), reads=reads, writes=writes)

    def tt(self, e, out, in0, in1, op, reads, writes):
        eng = self.c.eng[e]
        return self.c.op(e, lambda: eng.tensor_tensor(out=out, in0=in0, in1=in1, op=op), reads=reads, writes=writes)

    def ts(self, e, out, in0, s1, s2, op0, op1, reads, writes):
        eng = self.c.eng[e]
        if op1 is None:
            return self.c.op(e, lambda: eng.tensor_scalar(out=out, in0=in0, scalar1=s1, scalar2=None, op0=op0), reads=reads, writes=writes)
        return self.c.op(e, lambda: eng.tensor_scalar(out=out, in0=in0, scalar1=s1, scalar2=s2, op0=op0, op1=op1), reads=reads, writes=writes)

    def stt(self, e, out, in0, scalar, in1, op0, op1, reads, writes):
        eng = self.c.eng[e]
        return self.c.op(e, lambda: eng.scalar_tensor_tensor(out=out, in0=in0, scalar=scalar, in1=in1, op0=op0, op1=op1), reads=reads, writes=writes)

    def build(self):
        nc, c, es = self.nc, self.c, self.es
        I = self.I
        self.ps = [es.enter_context(nc.psum_tensor(f"ps{i}", [128, T], F32)) for i in range(8)]
        self.psB = [Buf(f"ps{i}") for i in range(8)]
        self.rot_banks = [0, 1, 2, 3, 4, 7]
        self.bi = 0
        G = {}
        for name, shape in (("ident", (128, 128)), ("swapm", (128, 128)), ("tril", (128, 128)),
                            ("sgn", (128, 2)), ("rowmask", (128, 8)), ("flag", (128, 1))):
            G[name] = self.gsb("g_" + name, shape)
            c.dma("sp", G[name][0][:], I[name], "gl", writes=[G[name][1]])
        c.sync_group("gl", [G[n_][1] for n_ in ("ident", "swapm", "tril", "sgn", "rowmask", "flag")])
        G["onesf"] = self.gsb("g_onesf", (128, 128))
        c.op("dve", lambda: nc.vector.memset(G["onesf"][0][:], 1.0), writes=[G["onesf"][1]])
        G["onesb"] = self.gsb("g_onesb", (128, 128), BF16)
        c.op("dve", lambda: nc.vector.memset(G["onesb"][0][:], 1.0), writes=[G["onesb"][1]])
        G["cTb"] = self.gsb("g_cTb", (128, KC), BF16)
        c.dma("pool", G["cTb"][0][:], I["cT"], "gl2", writes=[G["cTb"][1]])
        for name, shape, dt in (("rho", (128, 32), F32), ("modv", (128, 96), F32), ("mod1", (128, 96), F32),
                                ("WsT", (128, 8, 128), BF16), ("Gt", (128, 768), F32), ("Bt", (128, 768), F32),
                                ("diagD", (128, 4, 128), BF16), ("glub", (128, 4), F32), ("convw", (128, 6, 3), F32),
                                ("sgb", (128, 1024), BF16), ("carry", (128, 6, 2), F32)):
            G[name] = self.gsb("g_" + name, shape, dt)
        self.ginit = [self.gsb(f"g_ginit{q}", (128, 8)) for q in range(4)]
        self.G = G

        if self.ckpt == -1:
            c.barrier()
            return
        for l in range(self.n_layers):
            self.layer_init(l)
            if l == 0:
                self.zero_state()
                if not self.single:
                    self.mixer_phase(l, I["xA"], self.S["xmA"], "xA", "xmA")
                    self.ffn_phase(l, self.S["xmA"], self.S["x1A"], "xmA", "x1A")
                    self.apply_flag()
                self.mixer_phase(l, I["xB"], self.S["xmB"], "xB", "xmB")
                last = self.n_layers == 1
                self.ffn_phase(l, self.S["xmB"], self.out if last else self.S["x1B"], "xmB", "out" if last else "x1B")
            else:
                self.zero_state()
                if not self.single:
                    self.mixer_phase(l, self.S["x1A"], None, "x1A", None, state_only=True)
                    self.apply_flag()
                self.mixer_phase(l, self.S["x1B"], self.S["xm2"], "x1B", "xm2")
                if self.do_moe:
                    self.moe_phase(self.S["xm2"], self.out, "xm2", "out")
                else:
                    self.copy_phase(self.S["xm2"], self.out, "xm2", "out")
        c.barrier()

    def zero_state(self):
        nc, c = self.nc, self.c
        for q in range(4):
            t, b = self.ginit[q]
            c.op("dve", lambda: nc.vector.memset(t[:], 0.0), writes=[b])
        t, b = self.G["carry"]
        c.op("dve", lambda: nc.vector.memset(t[:], 0.0), writes=[b])

    def apply_flag(self):
        fl, fb = self.G["flag"]
        for q in range(4):
            t, b = self.ginit[q]
            self.ts("dve", t[:], t[:], fl[:, 0:1], None, ALU.mult, None, [b, fb], [b])
        t, b = self.G["carry"]
        self.ts("dve", t[:], t[:], fl[:, 0:1], None, ALU.mult, None, [b, fb], [b])

    def layer_init(self, l):
        self.phase_i += 1
        if self.stop_after is not None and self.phase_i > self.stop_after:
            return
        nc, c, I, G = self.nc, self.c, self.I, self.G
        c.barrier()
        with contextlib.ExitStack() as pes:
            def sb(name, shape, dt=F32):
                Pool.UID[0] += 1
                return pes.enter_context(nc.sbuf_tensor(f"{name}_u{Pool.UID[0]}", list(shape), dt)), Buf(name)
            self.wp = Pool(nc, pes, "w", 3, [128, 8192], BF16)
            cTb, cTbB = G["cTb"]
            cTrep, cTrepB = sb("cTrep", (128, KC, 128), BF16)
            c.op("dve", lambda: nc.vector.tensor_copy(out=cTrep[:], in_=cTb[:].unsqueeze(2).to_broadcast([128, KC, 128])), reads=[cTbB], writes=[cTrepB])
            mraw, mrawB = sb("mraw", (128, 96))
            dtmp, dtmpB = sb("dtmp", (128, 4, 128))
            ident, identB = G["ident"]
            for blk in range(24):
                wv, wb = self.wload(I["ada_w"][l].rearrange("(k p) n -> p k n", p=128)[:, :, blk * 512:(blk + 1) * 512], (128, KC, 512))
                pm, pmB = self.bank()
                for k in range(KC):
                    self.mm(pm[:], cTrep[:, k, :], wv[:, k, :], k == 0, k == KC - 1, [wb, cTrepB], pmB)
                self.tt("dve", dtmp[:], pm[:].rearrange("p (a b) -> p a b", a=4), ident[:].unsqueeze(1).to_broadcast([128, 4, 128]), ALU.mult, [pmB, identB], [dtmpB])
                c.op("dve", lambda: nc.vector.reduce_sum(out=mraw[:, blk * 4:(blk + 1) * 4], in_=dtmp[:], axis=mybir.AxisListType.X), reads=[dtmpB], writes=[mrawB])
            if self.ckpt == 1:
                c.barrier()
                return
            adab, adabB = sb("adab", (128, 96))
            c.dma("sp", adab[:], I["ada_bc"][l], "li0", writes=[adabB])
            mv, mvB = G["modv"]
            m1, m1B = G["mod1"]
            self.tt("dve", mv[:], mraw[:], adab[:], ALU.add, [mrawB, adabB], [mvB])
            self.ts("dve", m1[:], mv[:], 1.0, None, ALU.add, None, [mvB], [m1B])
            if self.ckpt == 2:
                c.barrier()
                return
            c.dma("sp", G["glub"][0][:], I["glu_bc"][l], "li1", writes=[G["glub"][1]])
            c.dma("sp", G["convw"][0][:], I["convw"][l], "li1", writes=[G["convw"][1]])
            c.sync_group("li1", [G["glub"][1], G["convw"][1]])
            c.dma("sp", G["Gt"][0][:], I["sg_ln_g"][l:l + 1, :].partition_broadcast(128), "li2", writes=[G["Gt"][1]])
            c.dma("sp", G["Bt"][0][:], I["sg_ln_b"][l:l + 1, :].partition_broadcast(128), "li2", writes=[G["Bt"][1]])
            c.sync_group("li2", [G["Gt"][1], G["Bt"][1]])
            c.op("dve", lambda: nc.vector.memset(G["sgb"][0][:], 0.0), writes=[G["sgb"][1]])
            c.dma("pool", G["sgb"][0][0:1, :], I["sg_b"][l], "li3", reads=[G["sgb"][1]], writes=[G["sgb"][1]])
            if self.ckpt == 3:
                c.barrier()
                return
            wsf, wsfB = sb("wsf", (128, 8, 128))
            c.dma("sp", wsf[:], I["sgwT"][l].rearrange("h s t -> s h t"), "li4", writes=[wsfB])
            tril, trilB = G["tril"]
            for h in range(8):
                self.tt("dve", G["WsT"][0][:, h, :], wsf[:, h, :], tril[:], ALU.mult, [wsfB, trilB], [G["WsT"][1]])
            dcol, dcolB = sb("dcol", (128, 4))
            c.dma("sp", dcol[:], I["dcol"][l], "li5", writes=[dcolB])
            for q in range(4):
                self.ts("dve", G["diagD"][0][:, q, :], ident[:], dcol[:, q:q + 1], None, ALU.mult, None, [identB, dcolB], [G["diagD"][1]])
            if self.ckpt == 4:
                c.barrier()
                return
            are, areB = sb("are", (128, 32)); aim, aimB = sb("aim", (128, 32)); dt_, dtB = sb("dt", (128, 32))
            c.dma("sp", are[:], I["a_re2"][l], "li6", writes=[areB])
            c.dma("sp", aim[:], I["a_im2"][l], "li6", writes=[aimB])
            c.dma("sp", dt_[:], I["ldt2"][l], "li6", writes=[dtB])
            c.sync_group("li6", [areB, aimB, dtB])
            self.act(dt_[:], dt_[:], AF.Exp, [dtB], [dtB])
            S = {}
            for nm in ("ang", "k", "r", "cosA", "sinA", "are_dt", "abr", "abi", "den", "nre", "t1", "t2", "fre", "fim",
                       "fimS", "freS", "th", "ang5", "Ere", "Eim", "Eim2", "rden"):
                S[nm] = sb("s_" + nm, (128, 32))
            rho, rhoB = G["rho"]

            def V(nm):
                return S[nm][0][:]

            def B_(nm):
                return S[nm][1]

            def reduce_sin(dn, sn, shift):
                self.ts("dve", V("k"), V(sn), 1.0, shift, ALU.mult, ALU.add, [B_(sn)], [B_("k")])
                self.ts("dve", V("r"), V("k"), 1.0 / TWO_PI, MAGIC, ALU.mult, ALU.add, [B_("k")], [B_("r")])
                self.ts("dve", V("r"), V("r"), MAGIC, None, ALU.subtract, None, [B_("r")], [B_("r")])
                self.stt("dve", V("r"), V("r"), -TWO_PI, V("k"), ALU.mult, ALU.add, [B_("r"), B_("k")], [B_("r")])
                self.ts("dve", V("r"), V("r"), -PI_CL, PI_CL, ALU.max, ALU.min, [B_("r")], [B_("r")])
                self.act(V(dn), V("r"), AF.Sin, [B_("r")], [B_(dn)])

            self.tt("dve", V("ang"), dt_[:], aim[:], ALU.mult, [dtB, aimB], [B_("ang")])
            self.tt("dve", V("are_dt"), dt_[:], are[:], ALU.mult, [dtB, areB], [B_("are_dt")])
            self.act(rho[:], V("are_dt"), AF.Exp, [B_("are_dt")], [rhoB])
            reduce_sin("sinA", "ang", 0.0)
            reduce_sin("cosA", "ang", math.pi / 2)
            self.tt("dve", V("abr"), rho[:], V("cosA"), ALU.mult, [rhoB, B_("cosA")], [B_("abr")])
            self.tt("dve", V("abi"), rho[:], V("sinA"), ALU.mult, [rhoB, B_("sinA")], [B_("abi")])
            self.tt("dve", V("den"), are[:], are[:], ALU.mult, [areB], [B_("den")])
            self.tt("dve", V("t1"), aim[:], aim[:], ALU.mult, [aimB], [B_("t1")])
            self.tt("dve", V("den"), V("den"), V("t1"), ALU.add, [B_("den"), B_("t1")], [B_("den")])
            c.op("dve", lambda: nc.vector.reciprocal(out=V("rden"), in_=V("den")), reads=[B_("den")], writes=[B_("rden")])
            self.ts("dve", V("nre"), V("abr"), -1.0, None, ALU.add, None, [B_("abr")], [B_("nre")])
            self.tt("dve", V("t1"), V("nre"), are[:], ALU.mult, [B_("nre"), areB], [B_("t1")])
            self.tt("dve", V("t2"), V("abi"), aim[:], ALU.mult, [B_("abi"), aimB], [B_("t2")])
            self.tt("dve", V("t1"), V("t1"), V("t2"), ALU.add, [B_("t1"), B_("t2")], [B_("t1")])
            self.tt("dve", V("fre"), V("t1"), V("rden"), ALU.mult, [B_("t1"), B_("rden")], [B_("fre")])
            self.tt("dve", V("t1"), V("abi"), are[:], ALU.mult, [B_("abi"), areB], [B_("t1")])
            self.tt("dve", V("t2"), V("nre"), aim[:], ALU.mult, [B_("nre"), aimB], [B_("t2")])
            self.tt("dve", V("t1"), V("t1"), V("t2"), ALU.subtract, [B_("t1"), B_("t2")], [B_("t1")])
            self.tt("dve", V("fim"), V("t1"), V("rden"), ALU.mult, [B_("t1"), B_("rden")], [B_("fim")])
            sgn, sgnB = G["sgn"]
            self.ts("dve", V("fimS"), V("fim"), sgn[:, 0:1], None, ALU.mult, None, [B_("fim"), sgnB], [B_("fimS")])
            self.ts("dve", V("freS"), V("fre"), sgn[:, 1:2], None, ALU.mult, None, [B_("fre"), sgnB], [B_("freS")])
            if self.ckpt == 5:
                c.barrier()
                return
            bA, bAB = sb("bA", (128, 32, 16)); bB, bBB = sb("bB", (128, 32, 16))
            c.dma("sp", bA[:], I["bA"][l], "li7", writes=[bAB])
            c.dma("sp", bB[:], I["bB"][l], "li7", writes=[bBB])
            c.sync_group("li7", [bAB, bBB])
            X1, X1B = sb("X1", (128, 32, 16)); X2, X2B = sb("X2", (128, 32, 16)); Xt, XtB = sb("Xt", (128, 32, 16))

            def bc16(nm):
                return S[nm][0][:].unsqueeze(2).to_broadcast([128, 32, 16])
            self.tt("dve", X1[:], bA[:], bc16("fre"), ALU.mult, [bAB, B_("fre")], [X1B])
            self.tt("dve", Xt[:], bB[:], bc16("fimS"), ALU.mult, [bBB, B_("fimS")], [XtB])
            self.tt("dve", X1[:], X1[:], Xt[:], ALU.add, [X1B, XtB], [X1B])
            self.tt("dve", X2[:], bB[:], bc16("freS"), ALU.mult, [bBB, B_("freS")], [X2B])
            self.tt("dve", Xt[:], bA[:], bc16("fim"), ALU.mult, [bAB, B_("fim")], [XtB])
            self.tt("dve", X2[:], X2[:], Xt[:], ALU.add, [X2B, XtB], [X2B])
            if self.ckpt == 6:
                c.barrier()
                return
            Wall, WallB = sb("Wall", (128, 32, 4, 128), BF16)
            c.op("pool", lambda: nc.gpsimd.memset(Wall[:], 0.0), writes=[WallB])
            rowmask, rmB = G["rowmask"]
            for mi, (X, XB) in enumerate(((X1, X1B), (X2, X2B))):
                for q in range(4):
                    pt, ptB = self.bank()
                    c.op("pe", lambda: nc.tensor.transpose(out=pt[:, 0:128], in_=X[:, q * 8:(q + 1) * 8, :].rearrange("p g c -> p (g c)"), identity=ident[:]),
                         reads=[XB, identB], writes=[ptB])
                    for j in range(8):
                        self.ts("dve", Wall[:, q * 8 + j, mi, :], pt[:, 0:128], rowmask[:, j:j + 1], None, ALU.mult, None, [ptB, rmB], [WallB])
            if self.ckpt == 7:
                c.barrier()
                return
            cA, cAB = sb("cA", (128, 32, 16)); cB, cBB = sb("cB", (128, 32, 16))
            c.dma("sp", cA[:], I["cA"][l], "li8", writes=[cAB])
            c.dma("sp", cB[:], I["cB"][l], "li8", writes=[cBB])
            c.sync_group("li8", [cAB, cBB])
            W5 = Wall[:].rearrange("p (q j) m (jj cc) -> p q j m jj cc", j=8, jj=8)
            for j in range(8):
                self.ts("dve", W5[:, :, j, 2, j, :], cA[:].rearrange("p (q j) cc -> p q j cc", j=8)[:, :, j, :], sgn[:, 1:2], None, ALU.mult, None, [cAB, sgnB], [WallB])
                self.ts("dve", W5[:, :, j, 3, j, :], cB[:].rearrange("p (q j) cc -> p q j cc", j=8)[:, :, j, :], -1.0, None, ALU.mult, None, [cBB], [WallB])
            SB_W = self.db("ssmW")
            c.dma("sp", self.S["ssmW"], Wall[:], "li9", reads=[WallB], writes=[SB_W])
            if self.ckpt == 8:
                c.barrier()
                return
            self.ts("dve", V("th"), V("ang"), 1.0 / TWO_PI, MAGIC, ALU.mult, ALU.add, [B_("ang")], [B_("th")])
            self.ts("dve", V("th"), V("th"), MAGIC, None, ALU.subtract, None, [B_("th")], [B_("th")])
            self.stt("dve", V("th"), V("th"), -TWO_PI, V("ang"), ALU.mult, ALU.add, [B_("th"), B_("ang")], [B_("th")])
            self.ts("dve", V("ang5"), V("th"), float(T), None, ALU.mult, None, [B_("th")], [B_("ang5")])
            reduce_sin("Eim", "ang5", 0.0)
            reduce_sin("Ere", "ang5", math.pi / 2)
            self.ts("dve", V("Eim2"), V("Eim"), sgn[:, 1:2], None, ALU.mult, None, [B_("Eim"), sgnB], [B_("Eim2")])
            rotS, rotSB = sb("rotS", (128, 32, 128))
            swapm, swB = G["swapm"]
            for g in range(32):
                self.ts("dve", rotS[:, g, :], ident[:], S["Ere"][0][:, g:g + 1], None, ALU.mult, None, [identB, B_("Ere")], [rotSB])
                self.stt("dve", rotS[:, g, :], swapm[:], S["Eim2"][0][:, g:g + 1], rotS[:, g, :], ALU.mult, ALU.add, [swB, B_("Eim2"), rotSB], [rotSB])
            SB_R = self.db("rot")
            c.dma("sp", self.S["rot"], rotS[:], "li9b", reads=[rotSB], writes=[SB_R])
            if self.ckpt == 9:
                c.barrier()
                return
            iota, iotaB = sb("iota", (128, T))
            c.dma("sp", iota[:], I["iota"], "li10", writes=[iotaB])
            tp = Pool(nc, pes, "tabt", 2, [128, 2, T], F32)
            xk, xkB = sb("xk", (128, T)); rr, rrB = sb("rr", (128, T))
            SB_T = self.db("tabs")
            for g in range(32):
                tb, tbB, key = tp.get()
                for which, shift in ((0, math.pi / 2), (1, 0.0)):
                    self.ts("dve", xk[:], iota[:], S["th"][0][:, g:g + 1], shift, ALU.mult, ALU.add, [iotaB, B_("th")], [xkB])
                    self.ts("dve", rr[:], xk[:], 1.0 / TWO_PI, MAGIC, ALU.mult, ALU.add, [xkB], [rrB])
                    self.ts("dve", rr[:], rr[:], MAGIC, None, ALU.subtract, None, [rrB], [rrB])
                    self.stt("dve", rr[:], rr[:], -TWO_PI, xk[:], ALU.mult, ALU.add, [rrB, xkB], [rrB])
                    self.ts("dve", rr[:], rr[:], -PI_CL, PI_CL, ALU.max, ALU.min, [rrB], [rrB])
                    self.act(tb[:, which, :], rr[:], AF.Sin, [rrB], [tbB])
                c.dma("sp", self.S["tabs"][g], tb[:], "s_" + key, reads=[tbB], writes=[SB_T])
            c.barrier()

    def post_begin(self):
        self.S1, self.S1B = self.ps[5], self.psB[5]
        self.S2, self.S2B = self.ps[6], self.psB[6]

    def post_chunk(self, f, ysrc, yB, xres_dram, xres_key, ti, gcol):
        nc, c, G = self.nc, self.c, self.G
        xr, xrB, key = self.ftp.get()
        c.dma("sp", xr[:, 0:T], xres_dram[f * 128:(f + 1) * 128, ti * T:(ti + 1) * T], "d_" + key,
              reads=[self.db(xres_key, ti, f)], writes=[xrB])
        self.act(xr[:, 0:T], xr[:, 0:T], AF.Copy, [xrB], [xrB], scale=ALPHA)
        m1, m1B = G["mod1"]
        self.stt("dve", xr[:, 0:T], ysrc, m1[:, gcol + f:gcol + f + 1], xr[:, 0:T], ALU.mult, ALU.add, [yB, m1B, xrB], [xrB])
        zq, zqB, _ = self.ftp.get()
        self.act(zq[:, 0:T], xr[:, 0:T], AF.Square, [xrB], [zqB])
        onesf, onesB = G["onesf"]
        self.mm(self.S1[:], onesf[:], xr[:, 0:T], f == 0, f == KC - 1, [onesB, xrB], self.S1B)
        self.mm(self.S2[:], onesf[:], zq[:, 0:T], f == 0, f == KC - 1, [onesB, zqB], self.S2B, inc=True)
        c.dma("sp", self.S["zsc"][f * 128:(f + 1) * 128, :], xr[:, 0:T], "s_" + key, reads=[xrB], writes=[self.db("zsc", f)])

    def post_finish(self, xout_dram, xout_key, ti, shcol, sccol, also_bf16=None):
        nc, c, G = self.nc, self.c, self.G
        (mean, meanB), (rstd, rstdB), (msq, msqB) = self.lnb
        self.act(mean[:, 0:T], self.S1[:], AF.Copy, [self.S1B], [meanB], scale=1.0 / D)
        self.tt("dve", msq[:, 0:T], mean[:, 0:T], mean[:, 0:T], ALU.mult, [meanB], [msqB])
        self.stt("dve", msq[:, 0:T], self.S2[:], 1.0 / D, msq[:, 0:T], ALU.mult, ALU.subtract, [self.S2B, msqB], [msqB])
        self.ts("dve", msq[:, 0:T], msq[:, 0:T], EPS, None, ALU.add, None, [msqB], [msqB])
        self.act(msq[:, 0:T], msq[:, 0:T], AF.Sqrt, [msqB], [msqB])
        c.op("dve", lambda: nc.vector.reciprocal(out=rstd[:, 0:T], in_=msq[:, 0:T]), reads=[msqB], writes=[rstdB])
        mv, mvB = G["modv"]
        m1, m1B = G["mod1"]
        for f in range(KC):
            zt, ztB, key = self.ftp.get()
            c.dma("sp", zt[:, 0:T], self.S["zsc"][f * 128:(f + 1) * 128, :], "d_" + key, reads=[self.db("zsc", f)], writes=[ztB])
            self.tt("dve", zt[:, 0:T], zt[:, 0:T], mean[:, 0:T], ALU.subtract, [ztB, meanB], [ztB])
            self.tt("dve", zt[:, 0:T], zt[:, 0:T], rstd[:, 0:T], ALU.mult, [ztB, rstdB], [ztB])
            self.act(zt[:, 0:T], zt[:, 0:T], AF.Identity, [ztB, m1B, mvB], [ztB],
                     scale=m1[:, sccol + f:sccol + f + 1], bias=mv[:, shcol + f:shcol + f + 1])
            c.dma("sp", xout_dram[f * 128:(f + 1) * 128, ti * T:(ti + 1) * T], zt[:, 0:T], "s_" + key, reads=[ztB],
                  writes=[self.db(xout_key, ti, f)])

    def mixer_phase(self, l, xin, xout, xin_key, xout_key, state_only=False):
        self.phase_i += 1
        if self.stop_after is not None and self.phase_i > self.stop_after:
            return
        nc, c, I, G = self.nc, self.c, self.I, self.G
        c.barrier()
        with contextlib.ExitStack() as pes:
            def sb(name, shape, dt=F32):
                Pool.UID[0] += 1
                return pes.enter_context(nc.sbuf_tensor(f"{name}_u{Pool.UID[0]}", list(shape), dt)), Buf(name)
            self.wp = Pool(nc, pes, "w", 6, [128, 4096], BF16)
            self.ftp = Pool(nc, pes, "f", 10, [128, T + 2], F32)
            self.lnb = [sb("lnb%d" % i, (128, T)) for i in range(3)]
            btp = Pool(nc, pes, "mb", 4, [128, T], BF16)
            tabp = Pool(nc, pes, "mt", 3, [128, 2, T], F32)
            xb, xbB = sb("xb", (128, KC, T), BF16)
            ub, ubB = sb("ub", (128, 4, T), BF16)
            csb, csB = sb("cs", (128, 8, 4, 128), BF16)
            rsb, rsB = sb("rs", (128, 8, 128))
            gsl, gslB = sb("gsl", (128, 32))
            if not state_only:
                zab, zabB = sb("zab", (128, 4, T), BF16)
                zag, zagB = sb("zag", (128, 4, T), BF16)
                zb, zbB = sb("zb", (128, 8, T), BF16)
                zcb, zcbB = sb("zcb", (128, 6, T), BF16)
                vnb, vnbB = sb("vnb", (128, 4, 768), BF16)
                vf, vfB = sb("vf", (128, 768))
                merged, mergedB = sb("merged", (128, KC, T), BF16)
                brA, brAB = sb("brA", (128, 10, 512), BF16)
                brB, brBB = sb("brB", (128, 8, 512), BF16)
                st, stB = sb("st", (128, 12))
            Yb_, YB = self.ps[5], self.psB[5]
            Gn, GnB = self.ps[6], self.psB[6]
            rho, rhoB = G["rho"]
            w_in = I["w_in"][l].rearrange("(k p) n -> p k n", p=128)
            for ti in range(self.NT):
                tsl = slice(ti * T, (ti + 1) * T)
                c.dma("pool", xb[:], xin.rearrange("(k p) t -> p k t", p=128)[:, :, tsl], "d_xb",
                      reads=[self.db(xin_key, ti, f) for f in range(KC)], writes=[xbB])
                wvs = [self.wload(w_in[:, :, hh * 256:(hh + 1) * 256], (128, KC, 256)) for hh in range(2)]
                for q in range(4):
                    wv, wb = wvs[q // 2]
                    pb, pbB = self.bank()
                    for k in range(KC):
                        self.mm(pb[:], wv[:, k, (q % 2) * 128:(q % 2 + 1) * 128], xb[:, k, :], k == 0, k == KC - 1, [wb, xbB], pbB)
                    self.act(ub[:, q, :], pb[:], AF.Copy, [pbB], [ubB])
                for q in range(4):
                    c.dma("sp", csb[:], self.S["ssmW"][:, q * 8:(q + 1) * 8], "d_cs", reads=[self.db("ssmW")], writes=[csB])
                    c.dma("sp", rsb[:], self.S["rot"][:, q * 8:(q + 1) * 8, :], "d_rs", reads=[self.db("rot")], writes=[rsB])
                    gi, giB = self.ginit[q]
                    if not state_only:
                        self.mm(Yb_[:], G["diagD"][0][:, q, :], ub[:, q, :], True, False, [G["diagD"][1], ubB], YB, inc=False)
                    for j in range(8):
                        g = q * 8 + j
                        tab, tabB, tkey = tabp.get()
                        c.dma("sp", tab[:], self.S["tabs"][g], "d_" + tkey, reads=[self.db("tabs")], writes=[tabB])
                        p1, p1B = self.bank()
                        self.mm(p1[:], csb[:, j, 0, :], ub[:, q, :], True, True, [csB, ubB], p1B)
                        p2, p2B = self.bank()
                        self.mm(p2[:], csb[:, j, 1, :], ub[:, q, :], True, True, [csB, ubB], p2B)
                        t1, t1B, _ = self.ftp.get()
                        t2, t2B, _ = self.ftp.get()
                        self.tt("dve", t1[:, 0:T], p1[:], tab[:, 0, :], ALU.mult, [p1B, tabB], [t1B])
                        self.tt("dve", t2[:, 0:T], p2[:], tab[:, 1, :], ALU.mult, [p2B, tabB], [t2B])
                        self.tt("dve", t1[:, 0:T], t1[:, 0:T], t2[:, 0:T], ALU.add, [t1B, t2B], [t1B])
                        gs, gsB, _ = self.ftp.get()
                        c.op("dve", lambda: nc.vector.tensor_tensor_scan(out=gs[:, 0:T], data0=rho[:, g:g + 1].to_broadcast([128, T]), data1=t1[:, 0:T],
                                                                          initial=gi[:, j:j + 1], op0=ALU.mult, op1=ALU.add),
                             reads=[rhoB, t1B, giB], writes=[gsB])
                        self.mm(Gn[:, g:g + 1], rsb[:, j, :], gs[:, T - 1:T], True, True, [rsB, gsB], GnB, inc=True)
                        if not state_only:
                            gc, gcB, _ = btp.get()
                            gsn, gsnB, _ = btp.get()
                            self.tt("dve", gc[:], gs[:, 0:T], tab[:, 0, :], ALU.mult, [gsB, tabB], [gcB])
                            self.tt("dve", gsn[:], gs[:, 0:T], tab[:, 1, :], ALU.mult, [gsB, tabB], [gsnB])
                            self.mm(Yb_[:], csb[:, j, 2, :], gc[:], False, False, [csB, gcB], YB, inc=False)
                            self.mm(Yb_[:], csb[:, j, 3, :], gsn[:], False, j == 7, [csB, gsnB], YB, inc=True)
                    self.act(gi[:], Gn[:, q * 8:(q + 1) * 8], AF.Copy, [GnB], [giB])
                    if not state_only:
                        self.act(zab[:, q, :], Yb_[:], AF.Gelu_apprx_tanh, [YB], [zabB])
                if state_only:
                    if ti == self.NT - 1:
                        self.conv_z(l, w_in, xb, xbB, None, None, carry_only=True)
                    continue
                gw, gwB = self.wload(I["glu_w"][l].rearrange("(k p) n -> p k n", p=128), (128, 4, 512))
                glub, glubB = G["glub"]
                for qo in range(4):
                    pb, pbB = self.bank()
                    for k in range(4):
                        self.mm(pb[:], gw[:, k, qo * 128:(qo + 1) * 128], zab[:, k, :], k == 0, k == 3, [gwB, zabB], pbB)
                    sg, sgB, _ = self.ftp.get()
                    self.act(sg[:, 0:T], pb[:], AF.Sigmoid, [pbB, glubB], [sgB], bias=glub[:, qo:qo + 1], scale=1.0)
                    self.tt("dve", zag[:, qo, :], zab[:, qo, :], sg[:, 0:T], ALU.mult, [zabB, sgB], [zagB])
                wvh = [self.wload(w_in[:, :, 1280 + j * 256:1280 + (j + 1) * 256], (128, KC, 256)) for j in range(3)]
                Gt, GtB = G["Gt"]
                Bt, BtB = G["Bt"]
                for cc in range(4):
                    pv = [self.bank() for _ in range(3)]
                    for j in range(3):
                        for k in range(KC):
                            self.mm(pv[j][0][:, 0:256], xb[:, k, cc * 128:(cc + 1) * 128], wvh[j][0][:, k, :], k == 0, k == KC - 1,
                                    [xbB, wvh[j][1]], pv[j][1])
                    jk, jkB, _ = self.ftp.get()
                    for j in range(3):
                        self.act(vf[:, j * 256:(j + 1) * 256], pv[j][0][:, 0:256], AF.Copy, [pv[j][1]], [vfB, stB], accum_out=st[:, j:j + 1])
                        self.act(jk[:, 0:256], pv[j][0][:, 0:256], AF.Square, [pv[j][1]], [jkB, stB], accum_out=st[:, 3 + j:4 + j])
                    self.tt("dve", st[:, 8:9], st[:, 0:1], st[:, 1:2], ALU.add, [stB], [stB])
                    self.tt("dve", st[:, 8:9], st[:, 8:9], st[:, 2:3], ALU.add, [stB], [stB])
                    self.ts("dve", st[:, 8:9], st[:, 8:9], 1.0 / 768, None, ALU.mult, None, [stB], [stB])
                    self.tt("dve", st[:, 9:10], st[:, 3:4], st[:, 4:5], ALU.add, [stB], [stB])
                    self.tt("dve", st[:, 9:10], st[:, 9:10], st[:, 5:6], ALU.add, [stB], [stB])
                    self.tt("dve", st[:, 10:11], st[:, 8:9], st[:, 8:9], ALU.mult, [stB], [stB])
                    self.stt("dve", st[:, 9:10], st[:, 9:10], 1.0 / 768, st[:, 10:11], ALU.mult, ALU.subtract, [stB], [stB])
                    self.ts("dve", st[:, 9:10], st[:, 9:10], EPS, None, ALU.add, None, [stB], [stB])
                    self.act(st[:, 9:10], st[:, 9:10], AF.Sqrt, [stB], [stB])
                    c.op("dve", lambda: nc.vector.reciprocal(out=st[:, 11:12], in_=st[:, 9:10]), reads=[stB], writes=[stB])
                    self.ts("dve", vf[:], vf[:], st[:, 8:9], st[:, 11:12], ALU.subtract, ALU.mult, [vfB, stB], [vfB])
                    self.tt("dve", vf[:], vf[:], Gt[:], ALU.mult, [vfB, GtB], [vfB])
                    self.tt("dve", vnb[:, cc, :], vf[:], Bt[:], ALU.add, [vfB, BtB], [vnbB])
                WsT, WsTB = G["WsT"]
                sgb, sgbB = G["sgb"]
                onesb, onesbB = G["onesb"]
                wuh = [self.wload(w_in[:, :, 512 + j * 192:512 + (j + 1) * 192], (128, KC, 192)) for j in range(4)]
                for h in range(8):
                    pu, puB = self.bank()
                    wu, wuB = wuh[h // 2]
                    for k in range(KC):
                        self.mm(pu[0:96, :], wu[:, k, (h % 2) * 96:(h % 2 + 1) * 96], xb[:, k, :], k == 0, k == KC - 1, [wuB, xbB], puB)
                    uf, ufB, _ = self.ftp.get()
                    self.act(uf[0:96, 0:T], pu[0:96, :], AF.Copy, [puB], [ufB])
                    pss, pssB = self.bank()
                    for cc in range(4):
                        self.mm(pss[0:96, cc * 128:(cc + 1) * 128], vnb[:, cc, h * 96:(h + 1) * 96], WsT[:, h, :], True, False, [vnbB, WsTB], pssB, inc=False)
                        self.mm(pss[0:96, cc * 128:(cc + 1) * 128], onesb[:, 0:96], sgb[:, h * 128:(h + 1) * 128], False, True,
                                [onesbB, sgbB], pssB, inc=(cc == 3))
                    self.tt("dve", zb[0:96, h, :], pss[0:96, :], uf[0:96, 0:T], ALU.mult, [pssB, ufB], [zbB])
                self.conv_z(l, w_in, xb, xbB, zcb, zcbB)
                for fb8 in range(8):
                    fb, fh = fb8 // 2, fb8 % 2
                    wg = [self.wload(w_in[:, :, 4352 + j * D + fb8 * 256:4352 + j * D + (fb8 + 1) * 256], (128, KC, 256)) for j in range(3)]
                    if fh == 0:
                        c.dma("pool", brA[:, 0:4, :], I["w_branch_a"][l].rearrange("(k p) n -> p k n", p=128)[:, :, fb * 512:(fb + 1) * 512], "d_brA", writes=[brAB])
                        c.dma("pool", brA[:, 4:10, :], I["w_branch_c"][l].rearrange("(k p) n -> p k n", p=128)[:, :, fb * 512:(fb + 1) * 512], "d_brA", writes=[brAB])
                        c.dma("pool", brB[0:96, :, :], I["w_branch_b"][l].rearrange("(k p) n -> p k n", p=96)[:, :, fb * 512:(fb + 1) * 512], "d_brB", writes=[brBB])
                    for fi2 in range(2):
                        fi = fh * 2 + fi2
                        f = fb * 4 + fi
                        fs = slice(fi * 128, (fi + 1) * 128)
                        gsl_ = slice(fi2 * 128, (fi2 + 1) * 128)
                        sig = []
                        for j in range(3):
                            pg, pgB = self.bank()
                            for k in range(KC):
                                self.mm(pg[:], wg[j][0][:, k, gsl_], xb[:, k, :], k == 0, k == KC - 1, [wg[j][1], xbB], pgB)
                            s_, sB_, _ = self.ftp.get()
                            self.act(s_[:, 0:T], pg[:], AF.Sigmoid, [pgB], [sB_])
                            sig.append((s_, sB_))
                        ya, yaB = self.bank()
                        for k in range(4):
                            self.mm(ya[:], brA[:, k, fs], zag[:, k, :], k == 0, k == 3, [brAB, zagB], yaB)
                        self.tt("dve", sig[0][0][:, 0:T], ya[:], sig[0][0][:, 0:T], ALU.mult, [yaB, sig[0][1]], [sig[0][1]])
                        yb2, yb2B = self.bank()
                        for k in range(8):
                            self.mm(yb2[:], brB[0:96, k, fs], zb[0:96, k, :], k == 0, k == 7, [brBB, zbB], yb2B)
                        self.tt("dve", sig[1][0][:, 0:T], yb2[:], sig[1][0][:, 0:T], ALU.mult, [yb2B, sig[1][1]], [sig[1][1]])
                        yc, ycB = self.bank()
                        for k in range(6):
                            self.mm(yc[:], brA[:, 4 + k, fs], zcb[:, k, :], k == 0, k == 5, [brAB, zcbB], ycB)
                        self.tt("dve", sig[2][0][:, 0:T], yc[:], sig[2][0][:, 0:T], ALU.mult, [ycB, sig[2][1]], [sig[2][1]])
                        self.tt("dve", sig[0][0][:, 0:T], sig[0][0][:, 0:T], sig[1][0][:, 0:T], ALU.add, [sig[0][1], sig[1][1]], [sig[0][1]])
                        self.tt("dve", merged[:, f, :], sig[0][0][:, 0:T], sig[2][0][:, 0:T], ALU.add, [sig[0][1], sig[2][1]], [mergedB])
                self.post_begin()
                for fb in range(8):
                    wo, woB = self.wload(I["w_o"][l].rearrange("(k p) n -> p k n", p=128)[:, :, fb * 256:(fb + 1) * 256], (128, KC, 256))
                    for fi in range(2):
                        f = fb * 2 + fi
                        py, pyB = self.bank()
                        for k in range(KC):
                            self.mm(py[:], wo[:, k, fi * 128:(fi + 1) * 128], merged[:, k, :], k == 0, k == KC - 1, [woB, mergedB], pyB)
                        self.post_chunk(f, py[:], pyB, xin, xin_key, ti, 32)
                self.post_finish(xout, xout_key, ti, 0, 16)
            c.barrier()

    def conv_z(self, l, w_in, xb, xbB, zcb, zcbB, carry_only=False):
        nc, c, G = self.nc, self.c, self.G
        carry, carryB = G["carry"]
        convw, convwB = G["convw"]
        for hv in range(3):
            names = ("c", "h") if carry_only else ("b", "c", "h")
            col0 = {"b": 2048, "c": 2816, "h": 3584}
            w3 = {nm: self.wload(w_in[:, :, col0[nm] + hv * 256:col0[nm] + (hv + 1) * 256], (128, KC, 256)) for nm in names}
            for j in range(2):
                ch = hv * 2 + j
                pp = {}
                for nm in names:
                    pb, pbB = self.bank()
                    for k in range(KC):
                        self.mm(pb[:], w3[nm][0][:, k, j * 128:(j + 1) * 128], xb[:, k, :], k == 0, k == KC - 1, [w3[nm][1], xbB], pbB)
                    pp[nm] = (pb, pbB)
                cf, cfB, _ = self.ftp.get()
                self.act(cf[:, 0:T], pp["c"][0][:], AF.Copy, [pp["c"][1]], [cfB])
                zbuf, zbufB, _ = self.ftp.get()
                self.tt("dve", zbuf[:, 2:T + 2], cf[:, 0:T], pp["h"][0][:], ALU.mult, [cfB, pp["h"][1]], [zbufB])
                if not carry_only:
                    self.act(zbuf[:, 0:2], carry[:, ch, :], AF.Copy, [carryB], [zbufB])
                self.act(carry[:, ch, :], zbuf[:, T:T + 2], AF.Copy, [zbufB], [carryB])
                if carry_only:
                    continue
                acc, accB, _ = self.ftp.get()
                self.ts("dve", acc[:, 0:T], zbuf[:, 2:T + 2], convw[:, ch, 2:3], None, ALU.mult, None, [zbufB, convwB], [accB])
                self.stt("dve", acc[:, 0:T], zbuf[:, 1:T + 1], convw[:, ch, 1:2], acc[:, 0:T], ALU.mult, ALU.add, [zbufB, convwB, accB], [accB])
                self.stt("dve", acc[:, 0:T], zbuf[:, 0:T], convw[:, ch, 0:1], acc[:, 0:T], ALU.mult, ALU.add, [zbufB, convwB, accB], [accB])
                self.tt("dve", zcb[:, ch, :], acc[:, 0:T], pp["b"][0][:], ALU.mult, [accB, pp["b"][1]], [zcbB])

    def ffn_phase(self, l, xin, xout, xin_key, xout_key):
        self.phase_i += 1
        if self.stop_after is not None and self.phase_i > self.stop_after:
            return
        nc, c, I, G = self.nc, self.c, self.I, self.G
        c.barrier()
        with contextlib.ExitStack() as pes:
            def sb(name, shape, dt=F32):
                Pool.UID[0] += 1
                return pes.enter_context(nc.sbuf_tensor(f"{name}_u{Pool.UID[0]}", list(shape), dt)), Buf(name)
            self.wp = Pool(nc, pes, "w", 4, [128, 8192], BF16)
            w2p = Pool(nc, pes, "w2_", 2, [128, 44, 256], BF16)
            self.ftp = Pool(nc, pes, "f", 6, [128, T + 2], F32)
            self.lnb = [sb("lnb%d" % i, (128, T)) for i in range(3)]
            xb, xbB = sb("xb2", (128, KC, T), BF16)
            hb, hbB = sb("hb", (128, 44, T), BF16)
            w13 = I["ffn_w13"][0].rearrange("(k p) n -> p k n", p=128)
            w2 = I["ffn_w2"][0].rearrange("(k p) n -> p k n", p=128)
            for ti in range(self.NT):
                tsl = slice(ti * T, (ti + 1) * T)
                c.dma("pool", xb[:], xin.rearrange("(k p) t -> p k t", p=128)[:, :, tsl], "d_xb",
                      reads=[self.db(xin_key, ti, f) for f in range(KC)], writes=[xbB])
                for hb4 in range(11):
                    wgt, wgtB = self.wload(w13[:, :, hb4 * 512:(hb4 + 1) * 512], (128, KC, 512))
                    wup, wupB = self.wload(w13[:, :, FFN + hb4 * 512:FFN + (hb4 + 1) * 512], (128, KC, 512))
                    for hi in range(4):
                        hc = hb4 * 4 + hi
                        hs = slice(hi * 128, (hi + 1) * 128)
                        pg, pgB = self.bank()
                        for k in range(KC):
                            self.mm(pg[:], wgt[:, k, hs], xb[:, k, :], k == 0, k == KC - 1, [wgtB, xbB], pgB)
                        pu, puB = self.bank()
                        for k in range(KC):
                            self.mm(pu[:], wup[:, k, hs], xb[:, k, :], k == 0, k == KC - 1, [wupB, xbB], puB)
                        sg, sgB, _ = self.ftp.get()
                        self.act(sg[:, 0:T], pg[:], AF.Silu, [pgB], [sgB])
                        self.tt("dve", hb[:, hc, :], sg[:, 0:T], pu[:], ALU.mult, [sgB, puB], [hbB])
                self.post_begin()
                for fp in range(8):
                    wt, wtB, key = w2p.get()
                    for k0 in range(0, 44, 11):
                        c.dma("pool", wt[:, k0:k0 + 11, :], w2[:, k0:k0 + 11, fp * 256:(fp + 1) * 256], "d_" + key, writes=[wtB])
                    for fi in range(2):
                        f = fp * 2 + fi
                        py, pyB = self.bank()
                        for k in range(44):
                            self.mm(py[:], wt[:, k, fi * 128:(fi + 1) * 128], hb[:, k, :], k == 0, k == 43, [wtB, hbB], pyB)
                        self.post_chunk(f, py[:], pyB, xin, xin_key, ti, 80)
                self.post_finish(xout, xout_key, ti, 48, 64)
            c.barrier()

    def copy_phase(self, xin, xout, xin_key, xout_key):
        self.phase_i += 1
        if self.stop_after is not None and self.phase_i > self.stop_after:
            return
        nc, c = self.nc, self.c
        c.barrier()
        with contextlib.ExitStack() as pes:
            self.ftp = Pool(nc, pes, "f", 4, [128, T + 2], F32)
            for ti in range(self.NT):
                for f in range(KC):
                    t, b, key = self.ftp.get()
                    c.dma("sp", t[:, 0:T], xin[f * 128:(f + 1) * 128, ti * T:(ti + 1) * T], "d_" + key, reads=[self.db(xin_key, ti, f)], writes=[b])
                    c.dma("sp", xout[f * 128:(f + 1) * 128, ti * T:(ti + 1) * T], t[:, 0:T], "s_" + key, reads=[b], writes=[self.db(xout_key, ti, f)])
            c.barrier()

    def moe_phase(self, xin, xout, xin_key, xout_key):
        self.phase_i += 1
        if self.stop_after is not None and self.phase_i > self.stop_after:
            return
        nc, c, I, G = self.nc, self.c, self.I, self.G
        assert self.NT % 2 == 0
        HP, HC = 7, 8
        c.barrier()
        with contextlib.ExitStack() as pes:
            def sb(name, shape, dt=F32):
                Pool.UID[0] += 1
                return pes.enter_context(nc.sbuf_tensor(f"{name}_u{Pool.UID[0]}", list(shape), dt)), Buf(name)
            self.wp = Pool(nc, pes, "w", 5, [128, 4096], BF16)
            w2p = Pool(nc, pes, "w2_", 2, [128, HC, 256], BF16)
            self.ftp = Pool(nc, pes, "f", 6, [128, T + 2], F32)
            self.lnb = [sb("lnb%d" % i, (128, T)) for i in range(3)]
            xbs = [sb("xb3_%d" % h, (128, KC, T), BF16) for h in range(2)]
            hb, hbB = sb("hbe", (128, HC, 2 * T), BF16)
            yacc, yaccB = sb("yacc", (128, KC, 2 * T))
            rw, rwB = sb("rw", (128, KC, NE))
            rb4, rb4B = sb("rb4", (128, 4 * NE))
            lg, lgB = sb("lg", (128, 4 * NE))
            gwts = [sb("gwt%d" % h, (128, 4 * NE)) for h in range(2)]
            m8, m8B = sb("m8", (128, 8))
            sm, smB = sb("sm", (128, 4))
            dg, dgB = sb("dg", (128, 128))
            gwbs = [sb("gwb%d" % h, (128, T)) for h in range(2)]
            c.dma("sp", rw[:], I["router_w"].rearrange("(k p) e -> p k e", p=128), "d_rw", writes=[rwB])
            c.dma("sp", rb4[:], I["router_b4"].partition_broadcast(128), "d_rw", writes=[rb4B])
            c.sync_group("d_rw", [rwB, rb4B])
            ident, identB = G["ident"]
            onesf, onesB = G["onesf"]
            for st in range(self.NT // 2):
                for h in range(2):
                    ti = st * 2 + h
                    tsl = slice(ti * T, (ti + 1) * T)
                    xb, xbB = xbs[h]
                    gwt, gwtB = gwts[h]
                    c.dma("pool", xb[:], xin.rearrange("(k p) t -> p k t", p=128)[:, :, tsl], "d_xb%d" % h,
                          reads=[self.db(xin_key, ti, f) for f in range(KC)], writes=[xbB])
                    Lgs = [self.bank() for _ in range(4)]
                    for k in range(KC):
                        xf, xfB, key = self.ftp.get()
                        c.dma("sp", xf[:, 0:T], xin[k * 128:(k + 1) * 128, tsl], "d_" + key, reads=[self.db(xin_key, ti, k)], writes=[xfB])
                        for cc in range(4):
                            self.mm(Lgs[cc][0][:, 0:NE], xf[:, cc * 128:(cc + 1) * 128], rw[:, k, :], k == 0, k == KC - 1,
                                    [xfB, rwB], Lgs[cc][1], inc=(cc == 3 or k == KC - 1))
                    for cc in range(4):
                        self.tt("dve", lg[:, cc * NE:(cc + 1) * NE], Lgs[cc][0][:, 0:NE], rb4[:, cc * NE:(cc + 1) * NE], ALU.add, [Lgs[cc][1], rb4B], [lgB])
                    for cc in range(4):
                        cs_ = slice(cc * NE, (cc + 1) * NE)
                        c.op("dve", lambda: nc.vector.max(out=m8[:], in_=lg[:, cs_]), reads=[lgB], writes=[m8B])
                        self.ts("dve", sm[:, 0:1], m8[:, 0:1], -1.0, None, ALU.mult, None, [m8B], [smB])
                        self.act(gwt[:, cs_], lg[:, cs_], AF.Exp, [lgB, smB], [gwtB], bias=sm[:, 0:1], scale=1.0)
                        self.stt("dve", gwt[:, cs_], lg[:, cs_], m8[:, 1:2], gwt[:, cs_], ALU.is_ge, ALU.mult, [lgB, m8B, gwtB], [gwtB])
                        c.op("dve", lambda: nc.vector.reduce_sum(out=sm[:, 1:2], in_=gwt[:, cs_], axis=mybir.AxisListType.X), reads=[gwtB], writes=[smB])
                        c.op("dve", lambda: nc.vector.reciprocal(out=sm[:, 2:3], in_=sm[:, 1:2]), reads=[smB], writes=[smB])
                        self.ts("dve", gwt[:, cs_], gwt[:, cs_], sm[:, 2:3], None, ALU.mult, None, [gwtB, smB], [gwtB])
                for e in range(NE):
                    for h in range(2):
                        gwt, gwtB = gwts[h]
                        gwb, gwbB = gwbs[h]
                        pgw, pgwB = self.bank()
                        for cc in range(4):
                            self.ts("dve", dg[:], ident[:], gwt[:, cc * NE + e:cc * NE + e + 1], None, ALU.mult, None, [identB, gwtB], [dgB])
                            self.mm(pgw[:, cc * 128:(cc + 1) * 128], onesf[:], dg[:], True, True, [onesB, dgB], pgwB, inc=True)
                        self.act(gwb[:], pgw[:], AF.Copy, [pgwB], [gwbB])
                    w13 = I["moe_w13"][e].rearrange("(k p) n -> p k n", p=128)
                    w2 = I["moe_w2"][e].rearrange("(k p) n -> p k n", p=128)
                    for hp in range(HP):
                        for hb2 in range(HC // 2):
                            col = (hp * HC + hb2 * 2) * 128
                            wgt, wgtB = self.wload(w13[:, :, col:col + 256], (128, KC, 256))
                            wup, wupB = self.wload(w13[:, :, EXP + col:EXP + col + 256], (128, KC, 256))
                            for hi in range(2):
                                hc = hb2 * 2 + hi
                                hs = slice(hi * 128, (hi + 1) * 128)
                                for h in range(2):
                                    xb, xbB = xbs[h]
                                    pg, pgB = self.bank()
                                    for k in range(KC):
                                        self.mm(pg[:], wgt[:, k, hs], xb[:, k, :], k == 0, k == KC - 1, [wgtB, xbB], pgB)
                                    pu, puB = self.bank()
                                    for k in range(KC):
                                        self.mm(pu[:], wup[:, k, hs], xb[:, k, :], k == 0, k == KC - 1, [wupB, xbB], puB)
                                    sg, sgB, _ = self.ftp.get()
                                    self.act(sg[:, 0:T], pg[:], AF.Silu, [pgB], [sgB])
                                    self.tt("dve", sg[:, 0:T], sg[:, 0:T], pu[:], ALU.mult, [sgB, puB], [sgB])
                                    self.tt("dve", hb[:, hc, h * T:(h + 1) * T], sg[:, 0:T], gwbs[h][0][:], ALU.mult, [sgB, gwbs[h][1]], [hbB])
                        for fp in range(KC // 2):
                            wt, wtB, key = w2p.get()
                            c.dma("pool", wt[:], w2[:, hp * HC:(hp + 1) * HC, fp * 256:(fp + 1) * 256], "d_" + key, writes=[wtB])
                            for fi in range(2):
                                f = fp * 2 + fi
                                for h in range(2):
                                    py, pyB = self.bank()
                                    for k in range(HC):
                                        self.mm(py[:], wt[:, k, fi * 128:(fi + 1) * 128], hb[:, k, h * T:(h + 1) * T], k == 0, k == HC - 1, [wtB, hbB], pyB)
                                    ysl = yacc[:, f, h * T:(h + 1) * T]
                                    if e == 0 and hp == 0:
                                        self.act(ysl, py[:], AF.Copy, [pyB], [yaccB])
                                    else:
                                        self.tt("dve", ysl, ysl, py[:], ALU.add, [yaccB, pyB], [yaccB])
                for h in range(2):
                    ti = st * 2 + h
                    self.post_begin()
                    for f in range(KC):
                        self.post_chunk(f, yacc[:, f, h * T:(h + 1) * T], yaccB, xin, xin_key, ti, 80)
                    self.post_finish(xout, xout_key, ti, 48, 64)
            c.barrier()


def _consts():
    ident = np.eye(128, dtype=np.float32)
    swapm = np.zeros((128, 128), np.float32)
    for n in range(64):
        swapm[n, 64 + n] = 1.0
        swapm[64 + n, n] = 1.0
    tril = np.triu(np.ones((128, 128), np.float32))
    iota = np.tile(np.arange(T, dtype=np.float32)[None, :], (128, 1))
    sgn = np.ones((128, 2), np.float32)
    sgn[:64, 0] = -1.0
    sgn[64:, 1] = -1.0
    rowmask = np.zeros((128, 8), np.float32)
    for j in range(8):
        rowmask[j * 16:(j + 1) * 16, j] = 1.0
    return {"ident": ident, "swapm": swapm, "tril": tril, "iota": iota, "sgn": sgn, "rowmask": rowmask}


def layout_weights(w, do_moe=True):
    f = lambda a: np.ascontiguousarray(np.asarray(a, dtype=np.float32))
    L = 2
    o = dict(_consts())
    o["w_in"] = f(w["w_in"])
    are = np.transpose(np.asarray(w["ssm_a_re"]), (0, 2, 1))
    aim = np.transpose(np.asarray(w["ssm_a_im"]), (0, 2, 1))
    o["a_re2"] = f(np.concatenate([are, are], axis=1))
    o["a_im2"] = f(np.concatenate([aim, aim], axis=1))
    o["ldt2"] = f(np.broadcast_to(np.asarray(w["ssm_log_dt"])[:, None, :], (L, 128, 32)))
    bre = np.transpose(np.asarray(w["ssm_b_re"]), (0, 2, 1, 3))
    bim = np.transpose(np.asarray(w["ssm_b_im"]), (0, 2, 1, 3))
    o["bA"] = f(np.concatenate([bre, bim], axis=1))
    o["bB"] = f(np.concatenate([bim, bre], axis=1))
    cre = np.transpose(np.asarray(w["ssm_c_re"]), (0, 3, 1, 2))
    cim = np.transpose(np.asarray(w["ssm_c_im"]), (0, 3, 1, 2))
    o["cA"] = f(np.concatenate([cre, cim], axis=1))
    o["cB"] = f(np.concatenate([cim, cre], axis=1))
    o["dcol"] = f(np.transpose(np.asarray(w["ssm_d"]).reshape(L, 4, 128), (0, 2, 1)))
    o["glu_w"] = f(w["glu_w"])
    o["glu_bc"] = f(np.transpose(np.asarray(w["glu_b"]).reshape(L, 4, 128), (0, 2, 1)))
    o["sg_ln_g"] = f(w["sg_ln_g"])
    o["sg_ln_b"] = f(w["sg_ln_b"])
    o["sgwT"] = f(np.transpose(np.asarray(w["sg_w"]), (0, 1, 3, 2)))
    o["sg_b"] = f(np.asarray(w["sg_b"]).reshape(L, 1, 1024))
    o["convw"] = f(np.transpose(np.asarray(w["conv_w"])[:, :, 0, :].reshape(L, 3, 6, 128), (0, 3, 2, 1)))
    for k in ("w_branch_a", "w_branch_b", "w_branch_c", "w_o", "ada_w", "ffn_w13", "ffn_w2"):
        o[k] = f(w[k])
    o["ada_bc"] = f(np.transpose(np.asarray(w["ada_b"]).reshape(L, 96, 128), (0, 2, 1)))
    if do_moe:
        o["router_w"] = f(np.asarray(w["moe_router_w"])[0])
        o["router_b4"] = f(np.tile(np.asarray(w["moe_router_b"])[0][None, :], (1, 4)))
        o["moe_w13"] = f(np.asarray(w["moe_w13"])[0])
        o["moe_w2"] = f(np.asarray(w["moe_w2"])[0])
    return o


def run_single(x, c, w, do_moe=True, debug=False, n_layers=2, trace=False, cores=None):
    x = np.asarray(x, dtype=np.float32)
    c = np.asarray(c, dtype=np.float32)
    NT = x.shape[1] // T
    prog = Prog(NT, do_moe=do_moe, debug=debug, n_layers=n_layers, single=True)
    shared = layout_weights(w, do_moe)
    in_maps = []
    for b in range(x.shape[0]):
        m = {k: v for k, v in shared.items() if k in prog.I}
        m["xB"] = np.ascontiguousarray(x[b].T)
        m["cT"] = np.ascontiguousarray(c[b].reshape(KC, 128).T)
        m["flag"] = np.zeros((128, 1), np.float32)
        m = {k: v for k, v in m.items() if k in prog.I}
        in_maps.append(m)
    cores = list(range(len(in_maps))) if cores is None else cores
    res = run_bass_kernel_spmd(prog.nc, [in_maps[i] for i in cores], core_ids=list(range(len(cores))), **({"trace": True} if trace else {}))
    out = np.zeros_like(x)
    for i, b in enumerate(cores):
        out[b] = res.results[i]["outT"].T
    return out, res


def run(x, c, w, NT, do_moe=True, debug=False, n_layers=2, trace=False, stop_after=None, cores=None):
    prog = Prog(NT, do_moe=do_moe, debug=debug, n_layers=n_layers, stop_after=stop_after)
    shared = layout_weights(w, do_moe)
    NTOK = NT * T
    in_maps = []
    x = np.asarray(x, dtype=np.float32)
    c = np.asarray(c, dtype=np.float32)
    for core in range(8):
        b, half = core // 2, core % 2
        m = {k: v for k, v in shared.items() if k in prog.I}
        xB = np.ascontiguousarray(x[b, half * NTOK:(half + 1) * NTOK, :].T)
        xA = np.ascontiguousarray(x[b, 0:NTOK, :].T) if half == 1 else np.zeros((D, NTOK), np.float32)
        m["xA"], m["xB"] = xA, xB
        m["cT"] = np.ascontiguousarray(c[b].reshape(KC, 128).T)
        m["flag"] = np.full((128, 1), float(half), np.float32)
        m = {k: v for k, v in m.items() if k in prog.I}
        in_maps.append(m)
    cores = list(range(8)) if cores is None else cores
    res = run_bass_kernel_spmd(prog.nc, [in_maps[i] for i in cores], core_ids=list(range(len(cores))), **({"trace": True} if trace else {}))
    res.results = {core: res.results[i] for i, core in enumerate(cores)}
    out = np.zeros((4, 2 * NTOK, D), np.float32)
    for core in cores:
        b, half = core // 2, core % 2
        out[b, half * NTOK:(half + 1) * NTOK, :] = res.results[core]["outT"].T
    return out, res


def kernel(**inputs):
    x = np.asarray(inputs["x"])
    out, _ = run(x, inputs["c"], inputs, NT=x.shape[1] // (2 * T), do_moe=True)
    return out
```

```python
import contextlib
import math
import numpy as np
import concourse.bass as bass
import concourse.mybir as mybir
from concourse.bass_utils import run_bass_kernel_spmd

F32 = mybir.dt.float32
BF16 = mybir.dt.bfloat16
AF = mybir.ActivationFunctionType
ALU = mybir.AluOpType

D = 2048
KC = 16
T = 512
DIN = 10496
FFN = 5632
EXP = 7168
NE = 8
ALPHA = 4.0 ** 0.25
EPS = 1e-5
TWO_PI = 2.0 * math.pi
MAGIC = 12582912.0
PI_CL = 3.1415925


class Buf:
    __slots__ = ("w", "r", "name")

    def __init__(self, name=""):
        self.w = None
        self.r = {}
        self.name = name


class Ctx:
    SELF_WAIT = True

    def __init__(self, nc, es):
        self.nc = nc
        self.es = es
        self.eng = {"pe": nc.tensor, "act": nc.scalar, "dve": nc.vector, "pool": nc.gpsimd, "sp": nc.sync}
        self.sems = {}
        self.cnt = {}
        self.waited = {e: {} for e in self.eng}
        self.pending = {e: [] for e in self.eng}
        self.nops = 0

    def sem(self, key):
        if key not in self.sems:
            self.sems[key] = self.es.enter_context(self.nc.semaphore(key))
            self.cnt[key] = 0
        return self.sems[key]

    def wait(self, e, ev):
        if ev is None:
            return
        k, v = ev
        if self.waited[e].get(k, 0) >= v:
            return
        if k == "c_" + e and (e == "pe" or not self.SELF_WAIT):
            return
        self.eng[e].wait_ge(self.sems[k], v)
        self.waited[e][k] = v

    def _deps(self, e, reads, writes):
        for b in reads:
            self.wait(e, b.w)
        for b in writes:
            self.wait(e, b.w)
            for k, v in list(b.r.items()):
                self.wait(e, (k, v))

    def op(self, e, fn, reads=(), writes=(), inc=True):
        self._deps(e, reads, writes)
        ins = fn()
        self.nops += 1
        pend = self.pending[e]
        for b in reads:
            pend.append((b, 0))
        for b in writes:
            pend.append((b, 1))
        if inc:
            k = "c_" + e
            self.sem(k)
            self.cnt[k] += 1
            ins.then_inc(self.sems[k], 1)
            ev = (k, self.cnt[k])
            for b, iw in pend:
                if iw:
                    b.w = ev
                    b.r = {}
            for b, iw in pend:
                if not iw:
                    b.r[k] = ev[1]
            pend.clear()
            return ev
        return None

    def dma(self, q, out_ap, in_ap, semkey, reads=(), writes=()):
        self._deps(q, reads, writes)
        self.sem(semkey)
        self.cnt[semkey] += 16
        self.eng[q].dma_start(out=out_ap, in_=in_ap).then_inc(self.sems[semkey], 16)
        ev = (semkey, self.cnt[semkey])
        self.nops += 1
        for b in writes:
            b.w = ev
            b.r = {}
        for b in reads:
            b.r[semkey] = ev[1]
        return ev

    def sync_group(self, key, bufs):
        ev = (key, self.cnt[key])
        for b in bufs:
            b.w = ev

    def barrier(self):
        for e in self.eng:
            assert not self.pending[e], e
        for e in self.eng:
            for k, v in self.cnt.items():
                if v > 0:
                    self.wait(e, (k, v))


class Pool:
    UID = [0]

    def __init__(self, nc, es, name, n, shape, dt):
        Pool.UID[0] += 1
        self.name = name
        name = f"{name}u{Pool.UID[0]}_"
        self.t = [es.enter_context(nc.sbuf_tensor(f"{name}{i}", shape, dt)) for i in range(n)]
        self.b = [Buf(f"{name}{i}") for i in range(n)]
        self.i = 0
        self.n = n
        self.uses = 0
        self.R = {"w": 3, "w2_": 4}.get(self.name, 1)

    def get(self):
        i = self.i
        self.i = (i + 1) % self.n
        r = (self.uses // self.n) % self.R
        self.uses += 1
        return self.t[i], self.b[i], f"{self.name}{i}r{r}"


class Prog:
    def __init__(self, NT, do_moe=True, debug=False, n_layers=2, stop_after=None, single=False):
        self.single = single
        self.stop_after = stop_after
        import os as _os
        self.ckpt = int(_os.environ.get("DBG_CKPT", "0"))
        self.phase_i = 0
        self.NT = NT
        self.NTOK = NT * T
        self.debug = debug
        self.do_moe = do_moe
        self.n_layers = n_layers
        nc = bass.Bass("TRN2", target_bir_lowering=False)
        self.nc = nc
        self.I = {}
        NTOK = self.NTOK

        def inp(name, shape):
            self.I[name] = nc.dram_tensor(name, list(shape), F32, kind="ExternalInput").ap()

        self.in_shapes = {
            "xA": (D, NTOK), "xB": (D, NTOK), "cT": (128, KC), "flag": (128, 1),
            "ident": (128, 128), "swapm": (128, 128), "tril": (128, 128), "iota": (128, T),
            "sgn": (128, 2), "rowmask": (128, 8),
            "w_in": (2, D, DIN), "a_re2": (2, 128, 32), "a_im2": (2, 128, 32), "ldt2": (2, 128, 32),
            "bA": (2, 128, 32, 16), "bB": (2, 128, 32, 16), "cA": (2, 128, 32, 16), "cB": (2, 128, 32, 16),
            "dcol": (2, 128, 4), "glu_w": (2, 512, 512), "glu_bc": (2, 128, 4),
            "sg_ln_g": (2, 768), "sg_ln_b": (2, 768), "sgwT": (2, 8, 128, 128), "sg_b": (2, 1, 1024),
            "convw": (2, 128, 6, 3), "w_branch_a": (2, 512, D), "w_branch_b": (2, 768, D),
            "w_branch_c": (2, 768, D), "w_o": (2, D, D), "ada_w": (2, D, 6 * D), "ada_bc": (2, 128, 96),
            "ffn_w13": (1, D, 2 * FFN), "ffn_w2": (1, FFN, D),
            "router_w": (D, NE), "router_b4": (1, 4 * NE),
            "moe_w13": (NE, D, 2 * EXP), "moe_w2": (NE, EXP, D),
        }
        if not do_moe:
            for k in ("router_w", "router_b4", "moe_w13", "moe_w2"):
                del self.in_shapes[k]
        prog_self = self

        class LazyI(dict):
            def __missing__(d, k):
                inp(k, prog_self.in_shapes[k])
                return d[k]
        self.I = LazyI()
        self.out = nc.dram_tensor("outT", [D, NTOK], F32, kind="ExternalOutput").ap()
        dk = "ExternalOutput" if debug else "Internal"

        def scr(name, shape, dt=F32, kind="Internal"):
            return nc.dram_tensor(name, list(shape), dt, kind=kind).ap()

        self.S = {
            "xmA": scr("xmA", (D, NTOK)), "x1A": scr("x1A", (D, NTOK)),
            "xmB": scr("xmB", (D, NTOK), kind=dk), "x1B": scr("x1B", (D, NTOK), kind=dk),
            "xm2": scr("xm2", (D, NTOK), kind=dk),
            "zsc": scr("zsc", (D, T)),
            "ssmW": scr("ssmW", (128, 32, 4, 128), BF16), "rot": scr("rot", (128, 32, 128)),
            "tabs": scr("tabs", (32, 128, 2, T)),
        }
        self.dB = {}
        with contextlib.ExitStack() as es:
            self.es = es
            self.c = Ctx(nc, es)
            self.build()

    def db(self, *key):
        if key not in self.dB:
            self.dB[key] = Buf(str(key))
        return self.dB[key]

    def gsb(self, name, shape, dt=F32):
        return self.es.enter_context(self.nc.sbuf_tensor(name, list(shape), dt)), Buf(name)

    def bank(self):
        i = self.rot_banks[self.bi]
        self.bi = (self.bi + 1) % len(self.rot_banks)
        return self.ps[i], self.psB[i]

    def mm(self, out, lhsT, rhs, start, stop, reads, wb, inc=None):
        nc = self.nc
        self.c.op("pe", lambda: nc.tensor.matmul(out, lhsT=lhsT, rhs=rhs, start=start, stop=stop),
                  reads=reads, writes=[wb], inc=(stop if inc is None else inc))

    def wload(self, src3, shape, q="pool"):
        t, b, key = self.wp.get()
        n = 1
        for s in shape[1:]:
            n *= s
        if len(shape) == 3:
            v = t[0:shape[0], 0:n].rearrange("p (k n) -> p k n", k=shape[1])
        else:
            v = t[0:shape[0], 0:n]
        self.c.dma(q, v, src3, "d_" + key, writes=[b])
        return v, b

    def act(self, out, in_, func, reads, writes, **kw):
        nc = self.nc
        return self.c.op("act", lambda: nc.scalar.activation(out=out, in_=in_, func=func, **kw), reads=reads, writes=writes)

    def tt(self, e, out, in0, in1, op, reads, writes):
        eng = self.c.eng[e]
        return self.c.op(e, lambda: eng.tensor_tensor(out=out, in0=in0, in1=in1, op=op), reads=reads, writes=writes)

    def ts(self, e, out, in0, s1, s2, op0, op1, reads, writes):
        eng = self.c.eng[e]
        if op1 is None:
            return self.c.op(e, lambda: eng.tensor_scalar(out=out, in0=in0, scalar1=s1, scalar2=None, op0=op0), reads=reads, writes=writes)
        return self.c.op(e, lambda: eng.tensor_scalar(out=out, in0=in0, scalar1=s1, scalar2=s2, op0=op0, op1=op1), reads=reads, writes=writes)

    def stt(self, e, out, in0, scalar, in1, op0, op1, reads, writes):
        eng = self.c.eng[e]
        return self.c.op(e, lambda: eng.scalar_tensor_tensor(out=out, in0=in0, scalar=scalar, in1=in1, op0=op0, op1=op1), reads=reads, writes=writes)

    def build(self):
        nc, c, es = self.nc, self.c, self.es
        I = self.I
        self.ps = [es.enter_context(nc.psum_tensor(f"ps{i}", [128, T], F32)) for i in range(8)]
        self.psB = [Buf(f"ps{i}") for i in range(8)]
        self.rot_banks = [0, 1, 2, 3, 4, 7]
        self.bi = 0
        G = {}
        for name, shape in (("ident", (128, 128)), ("swapm", (128, 128)), ("tril", (128, 128)),
                            ("sgn", (128, 2)), ("rowmask", (128, 8)), ("flag", (128, 1))):
            G[name] = self.gsb("g_" + name, shape)
            c.dma("sp", G[name][0][:], I[name], "gl", writes=[G[name][1]])
        c.sync_group("gl", [G[n_][1] for n_ in ("ident", "swapm", "tril", "sgn", "rowmask", "flag")])
        G["onesf"] = self.gsb("g_onesf", (128, 128))
        c.op("dve", lambda: nc.vector.memset(G["onesf"][0][:], 1.0), writes=[G["onesf"][1]])
        G["onesb"] = self.gsb("g_onesb", (128, 128), BF16)
        c.op("dve", lambda: nc.vector.memset(G["onesb"][0][:], 1.0), writes=[G["onesb"][1]])
        G["cTb"] = self.gsb("g_cTb", (128, KC), BF16)
        c.dma("pool", G["cTb"][0][:], I["cT"], "gl2", writes=[G["cTb"][1]])
        for name, shape, dt in (("rho", (128, 32), F32), ("modv", (128, 96), F32), ("mod1", (128, 96), F32),
                                ("WsT", (128, 8, 128), BF16), ("Gt", (128, 768), F32), ("Bt", (128, 768), F32),
                                ("diagD", (128, 4, 128), BF16), ("glub", (128, 4), F32), ("convw", (128, 6, 3), F32),
                                ("sgb", (128, 1024), BF16), ("carry", (128, 6, 2), F32)):
            G[name] = self.gsb("g_" + name, shape, dt)
        self.ginit = [self.gsb(f"g_ginit{q}", (128, 8)) for q in range(4)]
        self.G = G

        if self.ckpt == -1:
            c.barrier()
            return
        for l in range(self.n_layers):
            self.layer_init(l)
            if l == 0:
                self.zero_state()
                if not self.single:
                    self.mixer_phase(l, I["xA"], self.S["xmA"], "xA", "xmA")
                    self.ffn_phase(l, self.S["xmA"], self.S["x1A"], "xmA", "x1A")
                    self.apply_flag()
                self.mixer_phase(l, I["xB"], self.S["xmB"], "xB", "xmB")
                last = self.n_layers == 1
                self.ffn_phase(l, self.S["xmB"], self.out if last else self.S["x1B"], "xmB", "out" if last else "x1B")
            else:
                self.zero_state()
                if not self.single:
                    self.mixer_phase(l, self.S["x1A"], None, "x1A", None, state_only=True)
                    self.apply_flag()
                self.mixer_phase(l, self.S["x1B"], self.S["xm2"], "x1B", "xm2")
                if self.do_moe:
                    self.moe_phase(self.S["xm2"], self.out, "xm2", "out")
                else:
                    self.copy_phase(self.S["xm2"], self.out, "xm2", "out")
        c.barrier()

    def zero_state(self):
        nc, c = self.nc, self.c
        for q in range(4):
            t, b = self.ginit[q]
            c.op("dve", lambda: nc.vector.memset(t[:], 0.0), writes=[b])
        t, b = self.G["carry"]
        c.op("dve", lambda: nc.vector.memset(t[:], 0.0), writes=[b])

    def apply_flag(self):
        fl, fb = self.G["flag"]
        for q in range(4):
            t, b = self.ginit[q]
            self.ts("dve", t[:], t[:], fl[:, 0:1], None, ALU.mult, None, [b, fb], [b])
        t, b = self.G["carry"]
        self.ts("dve", t[:], t[:], fl[:, 0:1], None, ALU.mult, None, [b, fb], [b])

    def layer_init(self, l):
        self.phase_i += 1
        if self.stop_after is not None and self.phase_i > self.stop_after:
            return
        nc, c, I, G = self.nc, self.c, self.I, self.G
        c.barrier()
        with contextlib.ExitStack() as pes:
            def sb(name, shape, dt=F32):
                Pool.UID[0] += 1
                return pes.enter_context(nc.sbuf_tensor(f"{name}_u{Pool.UID[0]}", list(shape), dt)), Buf(name)
            self.wp = Pool(nc, pes, "w", 3, [128, 8192], BF16)
            cTb, cTbB = G["cTb"]
            cTrep, cTrepB = sb("cTrep", (128, KC, 128), BF16)
            c.op("dve", lambda: nc.vector.tensor_copy(out=cTrep[:], in_=cTb[:].unsqueeze(2).to_broadcast([128, KC, 128])), reads=[cTbB], writes=[cTrepB])
            mraw, mrawB = sb("mraw", (128, 96))
            dtmp, dtmpB = sb("dtmp", (128, 4, 128))
            ident, identB = G["ident"]
            for blk in range(24):
                wv, wb = self.wload(I["ada_w"][l].rearrange("(k p) n -> p k n", p=128)[:, :, blk * 512:(blk + 1) * 512], (128, KC, 512))
                pm, pmB = self.bank()
                for k in range(KC):
                    self.mm(pm[:], cTrep[:, k, :], wv[:, k, :], k == 0, k == KC - 1, [wb, cTrepB], pmB)
                self.tt("dve", dtmp[:], pm[:].rearrange("p (a b) -> p a b", a=4), ident[:].unsqueeze(1).to_broadcast([128, 4, 128]), ALU.mult, [pmB, identB], [dtmpB])
                c.op("dve", lambda: nc.vector.reduce_sum(out=mraw[:, blk * 4:(blk + 1) * 4], in_=dtmp[:], axis=mybir.AxisListType.X), reads=[dtmpB], writes=[mrawB])
            if self.ckpt == 1:
                c.barrier()
                return
            adab, adabB = sb("adab", (128, 96))
            c.dma("sp", adab[:], I["ada_bc"][l], "li0", writes=[adabB])
            mv, mvB = G["modv"]
            m1, m1B = G["mod1"]
            self.tt("dve", mv[:], mraw[:], adab[:], ALU.add, [mrawB, adabB], [mvB])
            self.ts("dve", m1[:], mv[:], 1.0, None, ALU.add, None, [mvB], [m1B])
            if self.ckpt == 2:
                c.barrier()
                return
            c.dma("sp", G["glub"][0][:], I["glu_bc"][l], "li1", writes=[G["glub"][1]])
            c.dma("sp", G["convw"][0][:], I["convw"][l], "li1", writes=[G["convw"][1]])
            c.sync_group("li1", [G["glub"][1], G["convw"][1]])
            c.dma("sp", G["Gt"][0][:], I["sg_ln_g"][l:l + 1, :].partition_broadcast(128), "li2", writes=[G["Gt"][1]])
            c.dma("sp", G["Bt"][0][:], I["sg_ln_b"][l:l + 1, :].partition_broadcast(128), "li2", writes=[G["Bt"][1]])
            c.sync_group("li2", [G["Gt"][1], G["Bt"][1]])
            c.op("dve", lambda: nc.vector.memset(G["sgb"][0][:], 0.0), writes=[G["sgb"][1]])
            c.dma("pool", G["sgb"][0][0:1, :], I["sg_b"][l], "li3", reads=[G["sgb"][1]], writes=[G["sgb"][1]])
            if self.ckpt == 3:
                c.barrier()
                return
            wsf, wsfB = sb("wsf", (128, 8, 128))
            c.dma("sp", wsf[:], I["sgwT"][l].rearrange("h s t -> s h t"), "li4", writes=[wsfB])
            tril, trilB = G["tril"]
            for h in range(8):
                self.tt("dve", G["WsT"][0][:, h, :], wsf[:, h, :], tril[:], ALU.mult, [wsfB, trilB], [G["WsT"][1]])
            dcol, dcolB = sb("dcol", (128, 4))
            c.dma("sp", dcol[:], I["dcol"][l], "li5", writes=[dcolB])
            for q in range(4):
                self.ts("dve", G["diagD"][0][:, q, :], ident[:], dcol[:, q:q + 1], None, ALU.mult, None, [identB, dcolB], [G["diagD"][1]])
            if self.ckpt == 4:
                c.barrier()
                return
            are, areB = sb("are", (128, 32)); aim, aimB = sb("aim", (128, 32)); dt_, dtB = sb("dt", (128, 32))
            c.dma("sp", are[:], I["a_re2"][l], "li6", writes=[areB])
            c.dma("sp", aim[:], I["a_im2"][l], "li6", writes=[aimB])
            c.dma("sp", dt_[:], I["ldt2"][l], "li6", writes=[dtB])
            c.sync_group("li6", [areB, aimB, dtB])
            self.act(dt_[:], dt_[:], AF.Exp, [dtB], [dtB])
            S = {}
            for nm in ("ang", "k", "r", "cosA", "sinA", "are_dt", "abr", "abi", "den", "nre", "t1", "t2", "fre", "fim",
                       "fimS", "freS", "th", "ang5", "Ere", "Eim", "Eim2", "rden"):
                S[nm] = sb("s_" + nm, (128, 32))
            rho, rhoB = G["rho"]

            def V(nm):
                return S[nm][0][:]

            def B_(nm):
                return S[nm][1]

            def reduce_sin(dn, sn, shift):
                self.ts("dve", V("k"), V(sn), 1.0, shift, ALU.mult, ALU.add, [B_(sn)], [B_("k")])
                self.ts("dve", V("r"), V("k"), 1.0 / TWO_PI, MAGIC, ALU.mult, ALU.add, [B_("k")], [B_("r")])
                self.ts("dve", V("r"), V("r"), MAGIC, None, ALU.subtract, None, [B_("r")], [B_("r")])
                self.stt("dve", V("r"), V("r"), -TWO_PI, V("k"), ALU.mult, ALU.add, [B_("r"), B_("k")], [B_("r")])
                self.ts("dve", V("r"), V("r"), -PI_CL, PI_CL, ALU.max, ALU.min, [B_("r")], [B_("r")])
                self.act(V(dn), V("r"), AF.Sin, [B_("r")], [B_(dn)])

            self.tt("dve", V("ang"), dt_[:], aim[:], ALU.mult, [dtB, aimB], [B_("ang")])
            self.tt("dve", V("are_dt"), dt_[:], are[:], ALU.mult, [dtB, areB], [B_("are_dt")])
            self.act(rho[:], V("are_dt"), AF.Exp, [B_("are_dt")], [rhoB])
            reduce_sin("sinA", "ang", 0.0)
            reduce_sin("cosA", "ang", math.pi / 2)
            self.tt("dve", V("abr"), rho[:], V("cosA"), ALU.mult, [rhoB, B_("cosA")], [B_("abr")])
            self.tt("dve", V("abi"), rho[:], V("sinA"), ALU.mult, [rhoB, B_("sinA")], [B_("abi")])
            self.tt("dve", V("den"), are[:], are[:], ALU.mult, [areB], [B_("den")])
            self.tt("dve", V("t1"), aim[:], aim[:], ALU.mult, [aimB], [B_("t1")])
            self.tt("dve", V("den"), V("den"), V("t1"), ALU.add, [B_("den"), B_("t1")], [B_("den")])
            c.op("dve", lambda: nc.vector.reciprocal(out=V("rden"), in_=V("den")), reads=[B_("den")], writes=[B_("rden")])
            self.ts("dve", V("nre"), V("abr"), -1.0, None, ALU.add, None, [B_("abr")], [B_("nre")])
            self.tt("dve", V("t1"), V("nre"), are[:], ALU.mult, [B_("nre"), areB], [B_("t1")])
            self.tt("dve", V("t2"), V("abi"), aim[:], ALU.mult, [B_("abi"), aimB], [B_("t2")])
            self.tt("dve", V("t1"), V("t1"), V("t2"), ALU.add, [B_("t1"), B_("t2")], [B_("t1")])
            self.tt("dve", V("fre"), V("t1"), V("rden"), ALU.mult, [B_("t1"), B_("rden")], [B_("fre")])
            self.tt("dve", V("t1"), V("abi"), are[:], ALU.mult, [B_("abi"), areB], [B_("t1")])
            self.tt("dve", V("t2"), V("nre"), aim[:], ALU.mult, [B_("nre"), aimB], [B_("t2")])
            self.tt("dve", V("t1"), V("t1"), V("t2"), ALU.subtract, [B_("t1"), B_("t2")], [B_("t1")])
            self.tt("dve", V("fim"), V("t1"), V("rden"), ALU.mult, [B_("t1"), B_("rden")], [B_("fim")])
            sgn, sgnB = G["sgn"]
            self.ts("dve", V("fimS"), V("fim"), sgn[:, 0:1], None, ALU.mult, None, [B_("fim"), sgnB], [B_("fimS")])
            self.ts("dve", V("freS"), V("fre"), sgn[:, 1:2], None, ALU.mult, None, [B_("fre"), sgnB], [B_("freS")])
            if self.ckpt == 5:
                c.barrier()
                return
            bA, bAB = sb("bA", (128, 32, 16)); bB, bBB = sb("bB", (128, 32, 16))
            c.dma("sp", bA[:], I["bA"][l], "li7", writes=[bAB])
            c.dma("sp", bB[:], I["bB"][l], "li7", writes=[bBB])
            c.sync_group("li7", [bAB, bBB])
            X1, X1B = sb("X1", (128, 32, 16)); X2, X2B = sb("X2", (128, 32, 16)); Xt, XtB = sb("Xt", (128, 32, 16))

            def bc16(nm):
                return S[nm][0][:].unsqueeze(2).to_broadcast([128, 32, 16])
            self.tt("dve", X1[:], bA[:], bc16("fre"), ALU.mult, [bAB, B_("fre")], [X1B])
            self.tt("dve", Xt[:], bB[:], bc16("fimS"), ALU.mult, [bBB, B_("fimS")], [XtB])
            self.tt("dve", X1[:], X1[:], Xt[:], ALU.add, [X1B, XtB], [X1B])
            self.tt("dve", X2[:], bB[:], bc16("freS"), ALU.mult, [bBB, B_("freS")], [X2B])
            self.tt("dve", Xt[:], bA[:], bc16("fim"), ALU.mult, [bAB, B_("fim")], [XtB])
            self.tt("dve", X2[:], X2[:], Xt[:], ALU.add, [X2B, XtB], [X2B])
            if self.ckpt == 6:
                c.barrier()
                return
            Wall, WallB = sb("Wall", (128, 32, 4, 128), BF16)
            c.op("pool", lambda: nc.gpsimd.memset(Wall[:], 0.0), writes=[WallB])
            rowmask, rmB = G["rowmask"]
            for mi, (X, XB) in enumerate(((X1, X1B), (X2, X2B))):
                for q in range(4):
                    pt, ptB = self.bank()
                    c.op("pe", lambda: nc.tensor.transpose(out=pt[:, 0:128], in_=X[:, q * 8:(q + 1) * 8, :].rearrange("p g c -> p (g c)"), identity=ident[:]),
                         reads=[XB, identB], writes=[ptB])
                    for j in range(8):
                        self.ts("dve", Wall[:, q * 8 + j, mi, :], pt[:, 0:128], rowmask[:, j:j + 1], None, ALU.mult, None, [ptB, rmB], [WallB])
            if self.ckpt == 7:
                c.barrier()
                return
            cA, cAB = sb("cA", (128, 32, 16)); cB, cBB = sb("cB", (128, 32, 16))
            c.dma("sp", cA[:], I["cA"][l], "li8", writes=[cAB])
            c.dma("sp", cB[:], I["cB"][l], "li8", writes=[cBB])
            c.sync_group("li8", [cAB, cBB])
            W5 = Wall[:].rearrange("p (q j) m (jj cc) -> p q j m jj cc", j=8, jj=8)
            for j in range(8):
                self.ts("dve", W5[:, :, j, 2, j, :], cA[:].rearrange("p (q j) cc -> p q j cc", j=8)[:, :, j, :], sgn[:, 1:2], None, ALU.mult, None, [cAB, sgnB], [WallB])
                self.ts("dve", W5[:, :, j, 3, j, :], cB[:].rearrange("p (q j) cc -> p q j cc", j=8)[:, :, j, :], -1.0, None, ALU.mult, None, [cBB], [WallB])
            SB_W = self.db("ssmW")
            c.dma("sp", self.S["ssmW"], Wall[:], "li9", reads=[WallB], writes=[SB_W])
            if self.ckpt == 8:
                c.barrier()
                return
            self.ts("dve", V("th"), V("ang"), 1.0 / TWO_PI, MAGIC, ALU.mult, ALU.add, [B_("ang")], [B_("th")])
            self.ts("dve", V("th"), V("th"), MAGIC, None, ALU.subtract, None, [B_("th")], [B_("th")])
            self.stt("dve", V("th"), V("th"), -TWO_PI, V("ang"), ALU.mult, ALU.add, [B_("th"), B_("ang")], [B_("th")])
            self.ts("dve", V("ang5"), V("th"), float(T), None, ALU.mult, None, [B_("th")], [B_("ang5")])
            reduce_sin("Eim", "ang5", 0.0)
            reduce_sin("Ere", "ang5", math.pi / 2)
            self.ts("dve", V("Eim2"), V("Eim"), sgn[:, 1:2], None, ALU.mult, None, [B_("Eim"), sgnB], [B_("Eim2")])
            rotS, rotSB = sb("rotS", (128, 32, 128))
            swapm, swB = G["swapm"]
            for g in range(32):
                self.ts("dve", rotS[:, g, :], ident[:], S["Ere"][0][:, g:g + 1], None, ALU.mult, None, [identB, B_("Ere")], [rotSB])
                self.stt("dve", rotS[:, g, :], swapm[:], S["Eim2"][0][:, g:g + 1], rotS[:, g, :], ALU.mult, ALU.add, [swB, B_("Eim2"), rotSB], [rotSB])
            SB_R = self.db("rot")
            c.dma("sp", self.S["rot"], rotS[:], "li9b", reads=[rotSB], writes=[SB_R])
            if self.ckpt == 9:
                c.barrier()
                return
            iota, iotaB = sb("iota", (128, T))
            c.dma("sp", iota[:], I["iota"], "li10", writes=[iotaB])
            tp = Pool(nc, pes, "tabt", 2, [128, 2, T], F32)
            xk, xkB = sb("xk", (128, T)); rr, rrB = sb("rr", (128, T))
            SB_T = self.db("tabs")
            for g in range(32):
                tb, tbB, key = tp.get()
                for which, shift in ((0, math.pi / 2), (1, 0.0)):
                    self.ts("dve", xk[:], iota[:], S["th"][0][:, g:g + 1], shift, ALU.mult, ALU.add, [iotaB, B_("th")], [xkB])
                    self.ts("dve", rr[:], xk[:], 1.0 / TWO_PI, MAGIC, ALU.mult, ALU.add, [xkB], [rrB])
                    self.ts("dve", rr[:], rr[:], MAGIC, None, ALU.subtract, None, [rrB], [rrB])
                    self.stt("dve", rr[:], rr[:], -TWO_PI, xk[:], ALU.mult, ALU.add, [rrB, xkB], [rrB])
                    self.ts("dve", rr[:], rr[:], -PI_CL, PI_CL, ALU.max, ALU.min, [rrB], [rrB])
                    self.act(tb[:, which, :], rr[:], AF.Sin, [rrB], [tbB])
                c.dma("sp", self.S["tabs"][g], tb[:], "s_" + key, reads=[tbB], writes=[SB_T])
            c.barrier()

    def post_begin(self):
        self.S1, self.S1B = self.ps[5], self.psB[5]
        self.S2, self.S2B = self.ps[6], self.psB[6]

    def post_chunk(self, f, ysrc, yB, xres_dram, xres_key, ti, gcol):
        nc, c, G = self.nc, self.c, self.G
        xr, xrB, key = self.ftp.get()
        c.dma("sp", xr[:, 0:T], xres_dram[f * 128:(f + 1) * 128, ti * T:(ti + 1) * T], "d_" + key,
              reads=[self.db(xres_key, ti, f)], writes=[xrB])
        self.act(xr[:, 0:T], xr[:, 0:T], AF.Copy, [xrB], [xrB], scale=ALPHA)
        m1, m1B = G["mod1"]
        self.stt("dve", xr[:, 0:T], ysrc, m1[:, gcol + f:gcol + f + 1], xr[:, 0:T], ALU.mult, ALU.add, [yB, m1B, xrB], [xrB])
        zq, zqB, _ = self.ftp.get()
        self.act(zq[:, 0:T], xr[:, 0:T], AF.Square, [xrB], [zqB])
        onesf, onesB = G["onesf"]
        self.mm(self.S1[:], onesf[:], xr[:, 0:T], f == 0, f == KC - 1, [onesB, xrB], self.S1B)
        self.mm(self.S2[:], onesf[:], zq[:, 0:T], f == 0, f == KC - 1, [onesB, zqB], self.S2B, inc=True)
        c.dma("sp", self.S["zsc"][f * 128:(f + 1) * 128, :], xr[:, 0:T], "s_" + key, reads=[xrB], writes=[self.db("zsc", f)])

    def post_finish(self, xout_dram, xout_key, ti, shcol, sccol, also_bf16=None):
        nc, c, G = self.nc, self.c, self.G
        (mean, meanB), (rstd, rstdB), (msq, msqB) = self.lnb
        self.act(mean[:, 0:T], self.S1[:], AF.Copy, [self.S1B], [meanB], scale=1.0 / D)
        self.tt("dve", msq[:, 0:T], mean[:, 0:T], mean[:, 0:T], ALU.mult, [meanB], [msqB])
        self.stt("dve", msq[:, 0:T], self.S2[:], 1.0 / D, msq[:, 0:T], ALU.mult, ALU.subtract, [self.S2B, msqB], [msqB])
        self.ts("dve", msq[:, 0:T], msq[:, 0:T], EPS, None, ALU.add, None, [msqB], [msqB])
        self.act(msq[:, 0:T], msq[:, 0:T], AF.Sqrt, [msqB], [msqB])
        c.op("dve", lambda: nc.vector.reciprocal(out=rstd[:, 0:T], in_=msq[:, 0:T]), reads=[msqB], writes=[rstdB])
        mv, mvB = G["modv"]
        m1, m1B = G["mod1"]
        for f in range(KC):
            zt, ztB, key = self.ftp.get()
            c.dma("sp", zt[:, 0:T], self.S["zsc"][f * 128:(f + 1) * 128, :], "d_" + key, reads=[self.db("zsc", f)], writes=[ztB])
            self.tt("dve", zt[:, 0:T], zt[:, 0:T], mean[:, 0:T], ALU.subtract, [ztB, meanB], [ztB])
            self.tt("dve", zt[:, 0:T], zt[:, 0:T], rstd[:, 0:T], ALU.mult, [ztB, rstdB], [ztB])
            self.act(zt[:, 0:T], zt[:, 0:T], AF.Identity, [ztB, m1B, mvB], [ztB],
                     scale=m1[:, sccol + f:sccol + f + 1], bias=mv[:, shcol + f:shcol + f + 1])
            c.dma("sp", xout_dram[f * 128:(f + 1) * 128, ti * T:(ti + 1) * T], zt[:, 0:T], "s_" + key, reads=[ztB],
                  writes=[self.db(xout_key, ti, f)])

    def mixer_phase(self, l, xin, xout, xin_key, xout_key, state_only=False):
        self.phase_i += 1
        if self.stop_after is not None and self.phase_i > self.stop_after:
            return
        nc, c, I, G = self.nc, self.c, self.I, self.G
        c.barrier()
        with contextlib.ExitStack() as pes:
            def sb(name, shape, dt=F32):
                Pool.UID[0] += 1
                return pes.enter_context(nc.sbuf_tensor(f"{name}_u{Pool.UID[0]}", list(shape), dt)), Buf(name)
            self.wp = Pool(nc, pes, "w", 6, [128, 4096], BF16)
            self.ftp = Pool(nc, pes, "f", 10, [128, T + 2], F32)
            self.lnb = [sb("lnb%d" % i, (128, T)) for i in range(3)]
            btp = Pool(nc, pes, "mb", 4, [128, T], BF16)
            tabp = Pool(nc, pes, "mt", 3, [128, 2, T], F32)
            xb, xbB = sb("xb", (128, KC, T), BF16)
            ub, ubB = sb("ub", (128, 4, T), BF16)
            csb, csB = sb("cs", (128, 8, 4, 128), BF16)
            rsb, rsB = sb("rs", (128, 8, 128))
            gsl, gslB = sb("gsl", (128, 32))
            if not state_only:
                zab, zabB = sb("zab", (128, 4, T), BF16)
                zag, zagB = sb("zag", (128, 4, T), BF16)
                zb, zbB = sb("zb", (128, 8, T), BF16)
                zcb, zcbB = sb("zcb", (128, 6, T), BF16)
                vnb, vnbB = sb("vnb", (128, 4, 768), BF16)
                vf, vfB = sb("vf", (128, 768))
                merged, mergedB = sb("merged", (128, KC, T), BF16)
                brA, brAB = sb("brA", (128, 10, 512), BF16)
                brB, brBB = sb("brB", (128, 8, 512), BF16)
                st, stB = sb("st", (128, 12))
            Yb_, YB = self.ps[5], self.psB[5]
            Gn, GnB = self.ps[6], self.psB[6]
            rho, rhoB = G["rho"]
            w_in = I["w_in"][l].rearrange("(k p) n -> p k n", p=128)
            for ti in range(self.NT):
                tsl = slice(ti * T, (ti + 1) * T)
                c.dma("pool", xb[:], xin.rearrange("(k p) t -> p k t", p=128)[:, :, tsl], "d_xb",
                      reads=[self.db(xin_key, ti, f) for f in range(KC)], writes=[xbB])
                wvs = [self.wload(w_in[:, :, hh * 256:(hh + 1) * 256], (128, KC, 256)) for hh in range(2)]
                for q in range(4):
                    wv, wb = wvs[q // 2]
                    pb, pbB = self.bank()
                    for k in range(KC):
                        self.mm(pb[:], wv[:, k, (q % 2) * 128:(q % 2 + 1) * 128], xb[:, k, :], k == 0, k == KC - 1, [wb, xbB], pbB)
                    self.act(ub[:, q, :], pb[:], AF.Copy, [pbB], [ubB])
                for q in range(4):
                    c.dma("sp", csb[:], self.S["ssmW"][:, q * 8:(q + 1) * 8], "d_cs", reads=[self.db("ssmW")], writes=[csB])
                    c.dma("sp", rsb[:], self.S["rot"][:, q * 8:(q + 1) * 8, :], "d_rs", reads=[self.db("rot")], writes=[rsB])
                    gi, giB = self.ginit[q]
                    if not state_only:
                        self.mm(Yb_[:], G["diagD"][0][:, q, :], ub[:, q, :], True, False, [G["diagD"][1], ubB], YB, inc=False)
                    for j in range(8):
                        g = q * 8 + j
                        tab, tabB, tkey = tabp.get()
                        c.dma("sp", tab[:], self.S["tabs"][g], "d_" + tkey, reads=[self.db("tabs")], writes=[tabB])
                        p1, p1B = self.bank()
                        self.mm(p1[:], csb[:, j, 0, :], ub[:, q, :], True, True, [csB, ubB], p1B)
                        p2, p2B = self.bank()
                        self.mm(p2[:], csb[:, j, 1, :], ub[:, q, :], True, True, [csB, ubB], p2B)
                        t1, t1B, _ = self.ftp.get()
                        t2, t2B, _ = self.ftp.get()
                        self.tt("dve", t1[:, 0:T], p1[:], tab[:, 0, :], ALU.mult, [p1B, tabB], [t1B])
                        self.tt("dve", t2[:, 0:T], p2[:], tab[:, 1, :], ALU.mult, [p2B, tabB], [t2B])
                        self.tt("dve", t1[:, 0:T], t1[:, 0:T], t2[:, 0:T], ALU.add, [t1B, t2B], [t1B])
                        gs, gsB, _ = self.ftp.get()
                        c.op("dve", lambda: nc.vector.tensor_tensor_scan(out=gs[:, 0:T], data0=rho[:, g:g + 1].to_broadcast([128, T]), data1=t1[:, 0:T],
                                                                          initial=gi[:, j:j + 1], op0=ALU.mult, op1=ALU.add),
                             reads=[rhoB, t1B, giB], writes=[gsB])
                        self.mm(Gn[:, g:g + 1], rsb[:, j, :], gs[:, T - 1:T], True, True, [rsB, gsB], GnB, inc=True)
                        if not state_only:
                            gc, gcB, _ = btp.get()
                            gsn, gsnB, _ = btp.get()
                            self.tt("dve", gc[:], gs[:, 0:T], tab[:, 0, :], ALU.mult, [gsB, tabB], [gcB])
                            self.tt("dve", gsn[:], gs[:, 0:T], tab[:, 1, :], ALU.mult, [gsB, tabB], [gsnB])
                            self.mm(Yb_[:], csb[:, j, 2, :], gc[:], False, False, [csB, gcB], YB, inc=False)
                            self.mm(Yb_[:], csb[:, j, 3, :], gsn[:], False, j == 7, [csB, gsnB], YB, inc=True)
                    self.act(gi[:], Gn[:, q * 8:(q + 1) * 8], AF.Copy, [GnB], [giB])
                    if not state_only:
                        self.act(zab[:, q, :], Yb_[:], AF.Gelu_apprx_tanh, [YB], [zabB])
                        if q < 3:
                            self.conv_z(l, w_in, xb, xbB, zcb, zcbB, parts=(q,))
                if state_only:
                    if ti == self.NT - 1:
                        self.conv_z(l, w_in, xb, xbB, None, None, carry_only=True)
                    continue
                gw, gwB = self.wload(I["glu_w"][l].rearrange("(k p) n -> p k n", p=128), (128, 4, 512))
                glub, glubB = G["glub"]
                for qo in range(4):
                    pb, pbB = self.bank()
                    for k in range(4):
                        self.mm(pb[:], gw[:, k, qo * 128:(qo + 1) * 128], zab[:, k, :], k == 0, k == 3, [gwB, zabB], pbB)
                    sg, sgB, _ = self.ftp.get()
                    self.act(sg[:, 0:T], pb[:], AF.Sigmoid, [pbB, glubB], [sgB], bias=glub[:, qo:qo + 1], scale=1.0)
                    self.tt("dve", zag[:, qo, :], zab[:, qo, :], sg[:, 0:T], ALU.mult, [zabB, sgB], [zagB])
                wvh = [self.wload(w_in[:, :, 1280 + j * 256:1280 + (j + 1) * 256], (128, KC, 256)) for j in range(3)]
                Gt, GtB = G["Gt"]
                Bt, BtB = G["Bt"]
                for cc in range(4):
                    pv = [self.bank() for _ in range(3)]
                    for j in range(3):
                        for k in range(KC):
                            self.mm(pv[j][0][:, 0:256], xb[:, k, cc * 128:(cc + 1) * 128], wvh[j][0][:, k, :], k == 0, k == KC - 1,
                                    [xbB, wvh[j][1]], pv[j][1])
                    jk, jkB, _ = self.ftp.get()
                    for j in range(3):
                        self.act(vf[:, j * 256:(j + 1) * 256], pv[j][0][:, 0:256], AF.Copy, [pv[j][1]], [vfB, stB], accum_out=st[:, j:j + 1])
                        self.act(jk[:, 0:256], pv[j][0][:, 0:256], AF.Square, [pv[j][1]], [jkB, stB], accum_out=st[:, 3 + j:4 + j])
                    self.tt("dve", st[:, 8:9], st[:, 0:1], st[:, 1:2], ALU.add, [stB], [stB])
                    self.tt("dve", st[:, 8:9], st[:, 8:9], st[:, 2:3], ALU.add, [stB], [stB])
                    self.ts("dve", st[:, 8:9], st[:, 8:9], 1.0 / 768, None, ALU.mult, None, [stB], [stB])
                    self.tt("dve", st[:, 9:10], st[:, 3:4], st[:, 4:5], ALU.add, [stB], [stB])
                    self.tt("dve", st[:, 9:10], st[:, 9:10], st[:, 5:6], ALU.add, [stB], [stB])
                    self.tt("dve", st[:, 10:11], st[:, 8:9], st[:, 8:9], ALU.mult, [stB], [stB])
                    self.stt("dve", st[:, 9:10], st[:, 9:10], 1.0 / 768, st[:, 10:11], ALU.mult, ALU.subtract, [stB], [stB])
                    self.ts("dve", st[:, 9:10], st[:, 9:10], EPS, None, ALU.add, None, [stB], [stB])
                    self.act(st[:, 9:10], st[:, 9:10], AF.Sqrt, [stB], [stB])
                    c.op("dve", lambda: nc.vector.reciprocal(out=st[:, 11:12], in_=st[:, 9:10]), reads=[stB], writes=[stB])
                    self.ts("dve", vf[:], vf[:], st[:, 8:9], st[:, 11:12], ALU.subtract, ALU.mult, [vfB, stB], [vfB])
                    self.tt("dve", vf[:], vf[:], Gt[:], ALU.mult, [vfB, GtB], [vfB])
                    self.tt("dve", vnb[:, cc, :], vf[:], Bt[:], ALU.add, [vfB, BtB], [vnbB])
                WsT, WsTB = G["WsT"]
                sgb, sgbB = G["sgb"]
                onesb, onesbB = G["onesb"]
                wuh = [self.wload(w_in[:, :, 512 + j * 192:512 + (j + 1) * 192], (128, KC, 192)) for j in range(4)]
                for h in range(8):
                    pu, puB = self.bank()
                    wu, wuB = wuh[h // 2]
                    for k in range(KC):
                        self.mm(pu[0:96, :], wu[:, k, (h % 2) * 96:(h % 2 + 1) * 96], xb[:, k, :], k == 0, k == KC - 1, [wuB, xbB], puB)
                    uf, ufB, _ = self.ftp.get()
                    self.act(uf[0:96, 0:T], pu[0:96, :], AF.Copy, [puB], [ufB])
                    pss, pssB = self.bank()
                    for cc in range(4):
                        self.mm(pss[0:96, cc * 128:(cc + 1) * 128], vnb[:, cc, h * 96:(h + 1) * 96], WsT[:, h, :], True, False, [vnbB, WsTB], pssB, inc=False)
                        self.mm(pss[0:96, cc * 128:(cc + 1) * 128], onesb[:, 0:96], sgb[:, h * 128:(h + 1) * 128], False, True,
                                [onesbB, sgbB], pssB, inc=(cc == 3))
                    self.tt("dve", zb[0:96, h, :], pss[0:96, :], uf[0:96, 0:T], ALU.mult, [pssB, ufB], [zbB])
                for fb8 in range(8):
                    fb, fh = fb8 // 2, fb8 % 2
                    wg = [self.wload(w_in[:, :, 4352 + j * D + fb8 * 256:4352 + j * D + (fb8 + 1) * 256], (128, KC, 256)) for j in range(3)]
                    if fh == 0:
                        c.dma("pool", brA[:, 0:4, :], I["w_branch_a"][l].rearrange("(k p) n -> p k n", p=128)[:, :, fb * 512:(fb + 1) * 512], "d_brA", writes=[brAB])
                        c.dma("pool", brA[:, 4:10, :], I["w_branch_c"][l].rearrange("(k p) n -> p k n", p=128)[:, :, fb * 512:(fb + 1) * 512], "d_brA", writes=[brAB])
                        c.dma("pool", brB[0:96, :, :], I["w_branch_b"][l].rearrange("(k p) n -> p k n", p=96)[:, :, fb * 512:(fb + 1) * 512], "d_brB", writes=[brBB])
                    for fi2 in range(2):
                        fi = fh * 2 + fi2
                        f = fb * 4 + fi
                        fs = slice(fi * 128, (fi + 1) * 128)
                        gsl_ = slice(fi2 * 128, (fi2 + 1) * 128)
                        sig = []
                        for j in range(3):
                            pg, pgB = self.bank()
                            for k in range(KC):
                                self.mm(pg[:], wg[j][0][:, k, gsl_], xb[:, k, :], k == 0, k == KC - 1, [wg[j][1], xbB], pgB)
                            s_, sB_, _ = self.ftp.get()
                            self.act(s_[:, 0:T], pg[:], AF.Sigmoid, [pgB], [sB_])
                            sig.append((s_, sB_))
                        ya, yaB = self.bank()
                        for k in range(4):
                            self.mm(ya[:], brA[:, k, fs], zag[:, k, :], k == 0, k == 3, [brAB, zagB], yaB)
                        self.tt("dve", sig[0][0][:, 0:T], ya[:], sig[0][0][:, 0:T], ALU.mult, [yaB, sig[0][1]], [sig[0][1]])
                        yb2, yb2B = self.bank()
                        for k in range(8):
                            self.mm(yb2[:], brB[0:96, k, fs], zb[0:96, k, :], k == 0, k == 7, [brBB, zbB], yb2B)
                        self.tt("dve", sig[1][0][:, 0:T], yb2[:], sig[1][0][:, 0:T], ALU.mult, [yb2B, sig[1][1]], [sig[1][1]])
                        yc, ycB = self.bank()
                        for k in range(6):
                            self.mm(yc[:], brA[:, 4 + k, fs], zcb[:, k, :], k == 0, k == 5, [brAB, zcbB], ycB)
                        self.tt("dve", sig[2][0][:, 0:T], yc[:], sig[2][0][:, 0:T], ALU.mult, [ycB, sig[2][1]], [sig[2][1]])
                        self.tt("dve", sig[0][0][:, 0:T], sig[0][0][:, 0:T], sig[1][0][:, 0:T], ALU.add, [sig[0][1], sig[1][1]], [sig[0][1]])
                        self.tt("dve", merged[:, f, :], sig[0][0][:, 0:T], sig[2][0][:, 0:T], ALU.add, [sig[0][1], sig[2][1]], [mergedB])
                self.post_begin()
                for fb in range(8):
                    wo, woB = self.wload(I["w_o"][l].rearrange("(k p) n -> p k n", p=128)[:, :, fb * 256:(fb + 1) * 256], (128, KC, 256))
                    for fi in range(2):
                        f = fb * 2 + fi
                        py, pyB = self.bank()
                        for k in range(KC):
                            self.mm(py[:], wo[:, k, fi * 128:(fi + 1) * 128], merged[:, k, :], k == 0, k == KC - 1, [woB, mergedB], pyB)
                        self.post_chunk(f, py[:], pyB, xin, xin_key, ti, 32)
                self.post_finish(xout, xout_key, ti, 0, 16)
            c.barrier()

    def conv_z(self, l, w_in, xb, xbB, zcb, zcbB, carry_only=False, parts=(0, 1, 2)):
        nc, c, G = self.nc, self.c, self.G
        carry, carryB = G["carry"]
        convw, convwB = G["convw"]
        for hv in parts:
            names = ("c", "h") if carry_only else ("b", "c", "h")
            col0 = {"b": 2048, "c": 2816, "h": 3584}
            w3 = {nm: self.wload(w_in[:, :, col0[nm] + hv * 256:col0[nm] + (hv + 1) * 256], (128, KC, 256)) for nm in names}
            for j in range(2):
                ch = hv * 2 + j
                pp = {}
                for nm in names:
                    pb, pbB = self.bank()
                    for k in range(KC):
                        self.mm(pb[:], w3[nm][0][:, k, j * 128:(j + 1) * 128], xb[:, k, :], k == 0, k == KC - 1, [w3[nm][1], xbB], pbB)
                    pp[nm] = (pb, pbB)
                cf, cfB, _ = self.ftp.get()
                self.act(cf[:, 0:T], pp["c"][0][:], AF.Copy, [pp["c"][1]], [cfB])
                zbuf, zbufB, _ = self.ftp.get()
                self.tt("dve", zbuf[:, 2:T + 2], cf[:, 0:T], pp["h"][0][:], ALU.mult, [cfB, pp["h"][1]], [zbufB])
                if not carry_only:
                    self.act(zbuf[:, 0:2], carry[:, ch, :], AF.Copy, [carryB], [zbufB])
                self.act(carry[:, ch, :], zbuf[:, T:T + 2], AF.Copy, [zbufB], [carryB])
                if carry_only:
                    continue
                acc, accB, _ = self.ftp.get()
                self.ts("dve", acc[:, 0:T], zbuf[:, 2:T + 2], convw[:, ch, 2:3], None, ALU.mult, None, [zbufB, convwB], [accB])
                self.stt("dve", acc[:, 0:T], zbuf[:, 1:T + 1], convw[:, ch, 1:2], acc[:, 0:T], ALU.mult, ALU.add, [zbufB, convwB, accB], [accB])
                self.stt("dve", acc[:, 0:T], zbuf[:, 0:T], convw[:, ch, 0:1], acc[:, 0:T], ALU.mult, ALU.add, [zbufB, convwB, accB], [accB])
                self.tt("dve", zcb[:, ch, :], acc[:, 0:T], pp["b"][0][:], ALU.mult, [accB, pp["b"][1]], [zcbB])

    def ffn_phase(self, l, xin, xout, xin_key, xout_key):
        self.phase_i += 1
        if self.stop_after is not None and self.phase_i > self.stop_after:
            return
        nc, c, I, G = self.nc, self.c, self.I, self.G
        c.barrier()
        with contextlib.ExitStack() as pes:
            def sb(name, shape, dt=F32):
                Pool.UID[0] += 1
                return pes.enter_context(nc.sbuf_tensor(f"{name}_u{Pool.UID[0]}", list(shape), dt)), Buf(name)
            self.wp = Pool(nc, pes, "w", 4, [128, 8192], BF16)
            w2p = Pool(nc, pes, "w2_", 2, [128, 44, 256], BF16)
            self.ftp = Pool(nc, pes, "f", 6, [128, T + 2], F32)
            self.lnb = [sb("lnb%d" % i, (128, T)) for i in range(3)]
            xb, xbB = sb("xb2", (128, KC, T), BF16)
            hb, hbB = sb("hb", (128, 44, T), BF16)
            w13 = I["ffn_w13"][0].rearrange("(k p) n -> p k n", p=128)
            w2 = I["ffn_w2"][0].rearrange("(k p) n -> p k n", p=128)
            for ti in range(self.NT):
                tsl = slice(ti * T, (ti + 1) * T)
                c.dma("pool", xb[:], xin.rearrange("(k p) t -> p k t", p=128)[:, :, tsl], "d_xb",
                      reads=[self.db(xin_key, ti, f) for f in range(KC)], writes=[xbB])
                for hb4 in range(11):
                    wgt, wgtB = self.wload(w13[:, :, hb4 * 512:(hb4 + 1) * 512], (128, KC, 512))
                    wup, wupB = self.wload(w13[:, :, FFN + hb4 * 512:FFN + (hb4 + 1) * 512], (128, KC, 512))
                    for hi in range(4):
                        hc = hb4 * 4 + hi
                        hs = slice(hi * 128, (hi + 1) * 128)
                        pg, pgB = self.bank()
                        for k in range(KC):
                            self.mm(pg[:], wgt[:, k, hs], xb[:, k, :], k == 0, k == KC - 1, [wgtB, xbB], pgB)
                        pu, puB = self.bank()
                        for k in range(KC):
                            self.mm(pu[:], wup[:, k, hs], xb[:, k, :], k == 0, k == KC - 1, [wupB, xbB], puB)
                        sg, sgB, _ = self.ftp.get()
                        self.act(sg[:, 0:T], pg[:], AF.Silu, [pgB], [sgB])
                        self.tt("dve", hb[:, hc, :], sg[:, 0:T], pu[:], ALU.mult, [sgB, puB], [hbB])
                self.post_begin()
                for fp in range(8):
                    wt, wtB, key = w2p.get()
                    for k0 in range(0, 44, 11):
                        c.dma("pool", wt[:, k0:k0 + 11, :], w2[:, k0:k0 + 11, fp * 256:(fp + 1) * 256], "d_" + key, writes=[wtB])
                    for fi in range(2):
                        f = fp * 2 + fi
                        py, pyB = self.bank()
                        for k in range(44):
                            self.mm(py[:], wt[:, k, fi * 128:(fi + 1) * 128], hb[:, k, :], k == 0, k == 43, [wtB, hbB], pyB)
                        self.post_chunk(f, py[:], pyB, xin, xin_key, ti, 80)
                self.post_finish(xout, xout_key, ti, 48, 64)
            c.barrier()

    def copy_phase(self, xin, xout, xin_key, xout_key):
        self.phase_i += 1
        if self.stop_after is not None and self.phase_i > self.stop_after:
            return
        nc, c = self.nc, self.c
        c.barrier()
        with contextlib.ExitStack() as pes:
            self.ftp = Pool(nc, pes, "f", 4, [128, T + 2], F32)
            for ti in range(self.NT):
                for f in range(KC):
                    t, b, key = self.ftp.get()
                    c.dma("sp", t[:, 0:T], xin[f * 128:(f + 1) * 128, ti * T:(ti + 1) * T], "d_" + key, reads=[self.db(xin_key, ti, f)], writes=[b])
                    c.dma("sp", xout[f * 128:(f + 1) * 128, ti * T:(ti + 1) * T], t[:, 0:T], "s_" + key, reads=[b], writes=[self.db(xout_key, ti, f)])
            c.barrier()

    def moe_phase(self, xin, xout, xin_key, xout_key):
        self.phase_i += 1
        if self.stop_after is not None and self.phase_i > self.stop_after:
            return
        nc, c, I, G = self.nc, self.c, self.I, self.G
        assert self.NT % 2 == 0
        HP, HC = 7, 8
        c.barrier()
        with contextlib.ExitStack() as pes:
            def sb(name, shape, dt=F32):
                Pool.UID[0] += 1
                return pes.enter_context(nc.sbuf_tensor(f"{name}_u{Pool.UID[0]}", list(shape), dt)), Buf(name)
            self.wp = Pool(nc, pes, "w", 5, [128, 4096], BF16)
            w2p = Pool(nc, pes, "w2_", 2, [128, HC, 256], BF16)
            self.ftp = Pool(nc, pes, "f", 6, [128, T + 2], F32)
            self.lnb = [sb("lnb%d" % i, (128, T)) for i in range(3)]
            xbs = [sb("xb3_%d" % h, (128, KC, T), BF16) for h in range(2)]
            hb, hbB = sb("hbe", (128, HC, 2 * T), BF16)
            yacc, yaccB = sb("yacc", (128, KC, 2 * T))
            rw, rwB = sb("rw", (128, KC, NE))
            rb4, rb4B = sb("rb4", (128, 4 * NE))
            lg, lgB = sb("lg", (128, 4 * NE))
            gwts = [sb("gwt%d" % h, (128, 4 * NE)) for h in range(2)]
            m8, m8B = sb("m8", (128, 8))
            sm, smB = sb("sm", (128, 4))
            dg, dgB = sb("dg", (128, 128))
            gwbs = [sb("gwb%d" % h, (128, T)) for h in range(2)]
            c.dma("sp", rw[:], I["router_w"].rearrange("(k p) e -> p k e", p=128), "d_rw", writes=[rwB])
            c.dma("sp", rb4[:], I["router_b4"].partition_broadcast(128), "d_rw", writes=[rb4B])
            c.sync_group("d_rw", [rwB, rb4B])
            ident, identB = G["ident"]
            onesf, onesB = G["onesf"]
            for st in range(self.NT // 2):
                for h in range(2):
                    ti = st * 2 + h
                    tsl = slice(ti * T, (ti + 1) * T)
                    xb, xbB = xbs[h]
                    gwt, gwtB = gwts[h]
                    c.dma("pool", xb[:], xin.rearrange("(k p) t -> p k t", p=128)[:, :, tsl], "d_xb%d" % h,
                          reads=[self.db(xin_key, ti, f) for f in range(KC)], writes=[xbB])
                    Lgs = [self.bank() for _ in range(4)]
                    for k in range(KC):
                        xf, xfB, key = self.ftp.get()
                        c.dma("sp", xf[:, 0:T], xin[k * 128:(k + 1) * 128, tsl], "d_" + key, reads=[self.db(xin_key, ti, k)], writes=[xfB])
                        for cc in range(4):
                            self.mm(Lgs[cc][0][:, 0:NE], xf[:, cc * 128:(cc + 1) * 128], rw[:, k, :], k == 0, k == KC - 1,
                                    [xfB, rwB], Lgs[cc][1], inc=(cc == 3 or k == KC - 1))
                    for cc in range(4):
                        self.tt("dve", lg[:, cc * NE:(cc + 1) * NE], Lgs[cc][0][:, 0:NE], rb4[:, cc * NE:(cc + 1) * NE], ALU.add, [Lgs[cc][1], rb4B], [lgB])
                    for cc in range(4):
                        cs_ = slice(cc * NE, (cc + 1) * NE)
                        c.op("dve", lambda: nc.vector.max(out=m8[:], in_=lg[:, cs_]), reads=[lgB], writes=[m8B])
                        self.ts("dve", sm[:, 0:1], m8[:, 0:1], -1.0, None, ALU.mult, None, [m8B], [smB])
                        self.act(gwt[:, cs_], lg[:, cs_], AF.Exp, [lgB, smB], [gwtB], bias=sm[:, 0:1], scale=1.0)
                        self.stt("dve", gwt[:, cs_], lg[:, cs_], m8[:, 1:2], gwt[:, cs_], ALU.is_ge, ALU.mult, [lgB, m8B, gwtB], [gwtB])
                        c.op("dve", lambda: nc.vector.reduce_sum(out=sm[:, 1:2], in_=gwt[:, cs_], axis=mybir.AxisListType.X), reads=[gwtB], writes=[smB])
                        c.op("dve", lambda: nc.vector.reciprocal(out=sm[:, 2:3], in_=sm[:, 1:2]), reads=[smB], writes=[smB])
                        self.ts("dve", gwt[:, cs_], gwt[:, cs_], sm[:, 2:3], None, ALU.mult, None, [gwtB, smB], [gwtB])
                for e in range(NE):
                    for h in range(2):
                        gwt, gwtB = gwts[h]
                        gwb, gwbB = gwbs[h]
                        pgw, pgwB = self.bank()
                        for cc in range(4):
                            self.ts("dve", dg[:], ident[:], gwt[:, cc * NE + e:cc * NE + e + 1], None, ALU.mult, None, [identB, gwtB], [dgB])
                            self.mm(pgw[:, cc * 128:(cc + 1) * 128], onesf[:], dg[:], True, True, [onesB, dgB], pgwB, inc=True)
                        self.act(gwb[:], pgw[:], AF.Copy, [pgwB], [gwbB])
                    w13 = I["moe_w13"][e].rearrange("(k p) n -> p k n", p=128)
                    w2 = I["moe_w2"][e].rearrange("(k p) n -> p k n", p=128)
                    for hp in range(HP):
                        for hb2 in range(HC // 2):
                            col = (hp * HC + hb2 * 2) * 128
                            wgt, wgtB = self.wload(w13[:, :, col:col + 256], (128, KC, 256))
                            wup, wupB = self.wload(w13[:, :, EXP + col:EXP + col + 256], (128, KC, 256))
                            for hi in range(2):
                                hc = hb2 * 2 + hi
                                hs = slice(hi * 128, (hi + 1) * 128)
                                for h in range(2):
                                    xb, xbB = xbs[h]
                                    pg, pgB = self.bank()
                                    for k in range(KC):
                                        self.mm(pg[:], wgt[:, k, hs], xb[:, k, :], k == 0, k == KC - 1, [wgtB, xbB], pgB)
                                    pu, puB = self.bank()
                                    for k in range(KC):
                                        self.mm(pu[:], wup[:, k, hs], xb[:, k, :], k == 0, k == KC - 1, [wupB, xbB], puB)
                                    sg, sgB, _ = self.ftp.get()
                                    self.act(sg[:, 0:T], pg[:], AF.Silu, [pgB], [sgB])
                                    self.tt("dve", sg[:, 0:T], sg[:, 0:T], pu[:], ALU.mult, [sgB, puB], [sgB])
                                    self.tt("dve", hb[:, hc, h * T:(h + 1) * T], sg[:, 0:T], gwbs[h][0][:], ALU.mult, [sgB, gwbs[h][1]], [hbB])
                        for fp in range(KC // 2):
                            wt, wtB, key = w2p.get()
                            c.dma("pool", wt[:], w2[:, hp * HC:(hp + 1) * HC, fp * 256:(fp + 1) * 256], "d_" + key, writes=[wtB])
                            for fi in range(2):
                                f = fp * 2 + fi
                                for h in range(2):
                                    py, pyB = self.bank()
                                    for k in range(HC):
                                        self.mm(py[:], wt[:, k, fi * 128:(fi + 1) * 128], hb[:, k, h * T:(h + 1) * T], k == 0, k == HC - 1, [wtB, hbB], pyB)
                                    ysl = yacc[:, f, h * T:(h + 1) * T]
                                    if e == 0 and hp == 0:
                                        self.act(ysl, py[:], AF.Copy, [pyB], [yaccB])
                                    else:
                                        self.tt("dve", ysl, ysl, py[:], ALU.add, [yaccB, pyB], [yaccB])
                for h in range(2):
                    ti = st * 2 + h
                    self.post_begin()
                    for f in range(KC):
                        self.post_chunk(f, yacc[:, f, h * T:(h + 1) * T], yaccB, xin, xin_key, ti, 80)
                    self.post_finish(xout, xout_key, ti, 48, 64)
            c.barrier()


def _consts():
    ident = np.eye(128, dtype=np.float32)
    swapm = np.zeros((128, 128), np.float32)
    for n in range(64):
        swapm[n, 64 + n] = 1.0
        swapm[64 + n, n] = 1.0
    tril = np.triu(np.ones((128, 128), np.float32))
    iota = np.tile(np.arange(T, dtype=np.float32)[None, :], (128, 1))
    sgn = np.ones((128, 2), np.float32)
    sgn[:64, 0] = -1.0
    sgn[64:, 1] = -1.0
    rowmask = np.zeros((128, 8), np.float32)
    for j in range(8):
        rowmask[j * 16:(j + 1) * 16, j] = 1.0
    return {"ident": ident, "swapm": swapm, "tril": tril, "iota": iota, "sgn": sgn, "rowmask": rowmask}


def layout_weights(w, do_moe=True):
    f = lambda a: np.ascontiguousarray(np.asarray(a, dtype=np.float32))
    L = 2
    o = dict(_consts())
    o["w_in"] = f(w["w_in"])
    are = np.transpose(np.asarray(w["ssm_a_re"]), (0, 2, 1))
    aim = np.transpose(np.asarray(w["ssm_a_im"]), (0, 2, 1))
    o["a_re2"] = f(np.concatenate([are, are], axis=1))
    o["a_im2"] = f(np.concatenate([aim, aim], axis=1))
    o["ldt2"] = f(np.broadcast_to(np.asarray(w["ssm_log_dt"])[:, None, :], (L, 128, 32)))
    bre = np.transpose(np.asarray(w["ssm_b_re"]), (0, 2, 1, 3))
    bim = np.transpose(np.asarray(w["ssm_b_im"]), (0, 2, 1, 3))
    o["bA"] = f(np.concatenate([bre, bim], axis=1))
    o["bB"] = f(np.concatenate([bim, bre], axis=1))
    cre = np.transpose(np.asarray(w["ssm_c_re"]), (0, 3, 1, 2))
    cim = np.transpose(np.asarray(w["ssm_c_im"]), (0, 3, 1, 2))
    o["cA"] = f(np.concatenate([cre, cim], axis=1))
    o["cB"] = f(np.concatenate([cim, cre], axis=1))
    o["dcol"] = f(np.transpose(np.asarray(w["ssm_d"]).reshape(L, 4, 128), (0, 2, 1)))
    o["glu_w"] = f(w["glu_w"])
    o["glu_bc"] = f(np.transpose(np.asarray(w["glu_b"]).reshape(L, 4, 128), (0, 2, 1)))
    o["sg_ln_g"] = f(w["sg_ln_g"])
    o["sg_ln_b"] = f(w["sg_ln_b"])
    o["sgwT"] = f(np.transpose(np.asarray(w["sg_w"]), (0, 1, 3, 2)))
    o["sg_b"] = f(np.asarray(w["sg_b"]).reshape(L, 1, 1024))
    o["convw"] = f(np.transpose(np.asarray(w["conv_w"])[:, :, 0, :].reshape(L, 3, 6, 128), (0, 3, 2, 1)))
    for k in ("w_branch_a", "w_branch_b", "w_branch_c", "w_o", "ada_w", "ffn_w13", "ffn_w2"):
        o[k] = f(w[k])
    o["ada_bc"] = f(np.transpose(np.asarray(w["ada_b"]).reshape(L, 96, 128), (0, 2, 1)))
    if do_moe:
        o["router_w"] = f(np.asarray(w["moe_router_w"])[0])
        o["router_b4"] = f(np.tile(np.asarray(w["moe_router_b"])[0][None, :], (1, 4)))
        o["moe_w13"] = f(np.asarray(w["moe_w13"])[0])
        o["moe_w2"] = f(np.asarray(w["moe_w2"])[0])
    return o


def run_single(x, c, w, do_moe=True, debug=False, n_layers=2, trace=False, cores=None):
    x = np.asarray(x, dtype=np.float32)
    c = np.asarray(c, dtype=np.float32)
    NT = x.shape[1] // T
    prog = Prog(NT, do_moe=do_moe, debug=debug, n_layers=n_layers, single=True)
    shared = layout_weights(w, do_moe)
    in_maps = []
    for b in range(x.shape[0]):
        m = {k: v for k, v in shared.items() if k in prog.I}
        m["xB"] = np.ascontiguousarray(x[b].T)
        m["cT"] = np.ascontiguousarray(c[b].reshape(KC, 128).T)
        m["flag"] = np.zeros((128, 1), np.float32)
        m = {k: v for k, v in m.items() if k in prog.I}
        in_maps.append(m)
    cores = list(range(len(in_maps))) if cores is None else cores
    res = run_bass_kernel_spmd(prog.nc, [in_maps[i] for i in cores], core_ids=list(range(len(cores))), **({"trace": True} if trace else {}))
    out = np.zeros_like(x)
    for i, b in enumerate(cores):
        out[b] = res.results[i]["outT"].T
    return out, res


def run(x, c, w, NT, do_moe=True, debug=False, n_layers=2, trace=False, stop_after=None, cores=None):
    prog = Prog(NT, do_moe=do_moe, debug=debug, n_layers=n_layers, stop_after=stop_after)
    shared = layout_weights(w, do_moe)
    NTOK = NT * T
    in_maps = []
    x = np.asarray(x, dtype=np.float32)
    c = np.asarray(c, dtype=np.float32)
    for core in range(8):
        b, half = core // 2, core % 2
        m = {k: v for k, v in shared.items() if k in prog.I}
        xB = np.ascontiguousarray(x[b, half * NTOK:(half + 1) * NTOK, :].T)
        xA = np.ascontiguousarray(x[b, 0:NTOK, :].T) if half == 1 else np.zeros((D, NTOK), np.float32)
        m["xA"], m["xB"] = xA, xB
        m["cT"] = np.ascontiguousarray(c[b].reshape(KC, 128).T)
        m["flag"] = np.full((128, 1), float(half), np.float32)
        m = {k: v for k, v in m.items() if k in prog.I}
        in_maps.append(m)
    cores = list(range(8)) if cores is None else cores
    res = run_bass_kernel_spmd(prog.nc, [in_maps[i] for i in cores], core_ids=list(range(len(cores))), **({"trace": True} if trace else {}))
    res.results = {core: res.results[i] for i, core in enumerate(cores)}
    out = np.zeros((4, 2 * NTOK, D), np.float32)
    for core in cores:
        b, half = core // 2, core % 2
        out[b, half * NTOK:(half + 1) * NTOK, :] = res.results[core]["outT"].T
    return out, res


def kernel(**inputs):
    x = np.asarray(inputs["x"])
    out, _ = run(x, inputs["c"], inputs, NT=x.shape[1] // (2 * T), do_moe=True)
    return out
```

```python
import contextlib
import math
import numpy as np
import concourse.bass as bass
import concourse.mybir as mybir
from concourse.bass_utils import run_bass_kernel_spmd

F32 = mybir.dt.float32
BF16 = mybir.dt.bfloat16
AF = mybir.ActivationFunctionType
ALU = mybir.AluOpType

D = 2048
KC = 16
T = 512
DIN = 10496
FFN = 5632
EXP = 7168
NE = 8
ALPHA = 4.0 ** 0.25
EPS = 1e-5
TWO_PI = 2.0 * math.pi
MAGIC = 12582912.0
PI_CL = 3.1415925


class Buf:
    __slots__ = ("w", "r", "name")

    def __init__(self, name=""):
        self.w = None
        self.r = {}
        self.name = name


class Ctx:
    SELF_WAIT = True

    def __init__(self, nc, es):
        self.nc = nc
        self.es = es
        self.eng = {"pe": nc.tensor, "act": nc.scalar, "dve": nc.vector, "pool": nc.gpsimd, "sp": nc.sync}
        self.sems = {}
        self.cnt = {}
        self.waited = {e: {} for e in self.eng}
        self.pending = {e: [] for e in self.eng}
        self.nops = 0

    def sem(self, key):
        if key not in self.sems:
            self.sems[key] = self.es.enter_context(self.nc.semaphore(key))
            self.cnt[key] = 0
        return self.sems[key]

    def wait(self, e, ev):
        if ev is None:
            return
        k, v = ev
        if self.waited[e].get(k, 0) >= v:
            return
        if k == "c_" + e and (e == "pe" or not self.SELF_WAIT):
            return
        self.eng[e].wait_ge(self.sems[k], v)
        self.waited[e][k] = v

    def _deps(self, e, reads, writes):
        for b in reads:
            self.wait(e, b.w)
        for b in writes:
            self.wait(e, b.w)
            for k, v in list(b.r.items()):
                self.wait(e, (k, v))

    def op(self, e, fn, reads=(), writes=(), inc=True):
        self._deps(e, reads, writes)
        ins = fn()
        self.nops += 1
        pend = self.pending[e]
        for b in reads:
            pend.append((b, 0))
        for b in writes:
            pend.append((b, 1))
        if inc:
            k = "c_" + e
            self.sem(k)
            self.cnt[k] += 1
            ins.then_inc(self.sems[k], 1)
            ev = (k, self.cnt[k])
            for b, iw in pend:
                if iw:
                    b.w = ev
                    b.r = {}
            for b, iw in pend:
                if not iw:
                    b.r[k] = ev[1]
            pend.clear()
            return ev
        return None

    def dma(self, q, out_ap, in_ap, semkey, reads=(), writes=()):
        self._deps(q, reads, writes)
        self.sem(semkey)
        self.cnt[semkey] += 16
        self.eng[q].dma_start(out=out_ap, in_=in_ap).then_inc(self.sems[semkey], 16)
        ev = (semkey, self.cnt[semkey])
        self.nops += 1
        for b in writes:
            b.w = ev
            b.r = {}
        for b in reads:
            b.r[semkey] = ev[1]
        return ev

    def sync_group(self, key, bufs):
        ev = (key, self.cnt[key])
        for b in bufs:
            b.w = ev

    def barrier(self):
        for e in self.eng:
            assert not self.pending[e], e
        for e in self.eng:
            for k, v in self.cnt.items():
                if v > 0:
                    self.wait(e, (k, v))


class Pool:
    UID = [0]

    def __init__(self, nc, es, name, n, shape, dt):
        Pool.UID[0] += 1
        self.name = name
        name = f"{name}u{Pool.UID[0]}_"
        self.t = [es.enter_context(nc.sbuf_tensor(f"{name}{i}", shape, dt)) for i in range(n)]
        self.b = [Buf(f"{name}{i}") for i in range(n)]
        self.i = 0
        self.n = n
        self.uses = 0
        self.R = {"w": 3, "w2_": 4}.get(self.name, 1)

    def get(self):
        i = self.i
        self.i = (i + 1) % self.n
        r = (self.uses // self.n) % self.R
        self.uses += 1
        return self.t[i], self.b[i], f"{self.name}{i}r{r}"


class Prog:
    def __init__(self, NT, do_moe=True, debug=False, n_layers=2, stop_after=None, single=False):
        self.single = single
        self.stop_after = stop_after
        import os as _os
        self.ckpt = int(_os.environ.get("DBG_CKPT", "0"))
        self.phase_i = 0
        self.NT = NT
        self.NTOK = NT * T
        self.debug = debug
        self.do_moe = do_moe
        self.n_layers = n_layers
        nc = bass.Bass("TRN2", target_bir_lowering=False)
        self.nc = nc
        self.I = {}
        NTOK = self.NTOK

        def inp(name, shape):
            self.I[name] = nc.dram_tensor(name, list(shape), F32, kind="ExternalInput").ap()

        self.in_shapes = {
            "xA": (D, NTOK), "xB": (D, NTOK), "cT": (128, KC), "flag": (128, 1),
            "ident": (128, 128), "swapm": (128, 128), "tril": (128, 128), "iota": (128, T),
            "sgn": (128, 2), "rowmask": (128, 8),
            "w_in": (2, D, DIN), "a_re2": (2, 128, 32), "a_im2": (2, 128, 32), "ldt2": (2, 128, 32),
            "bA": (2, 128, 32, 16), "bB": (2, 128, 32, 16), "cA": (2, 128, 32, 16), "cB": (2, 128, 32, 16),
            "dcol": (2, 128, 4), "glu_w": (2, 512, 512), "glu_bc": (2, 128, 4),
            "sg_ln_g": (2, 768), "sg_ln_b": (2, 768), "sgwT": (2, 8, 128, 128), "sg_b": (2, 1, 1024),
            "convw": (2, 128, 6, 3), "w_branch_a": (2, 512, D), "w_branch_b": (2, 768, D),
            "w_branch_c": (2, 768, D), "w_o": (2, D, D), "ada_w": (2, D, 6 * D), "ada_bc": (2, 128, 96),
            "ffn_w13": (1, D, 2 * FFN), "ffn_w2": (1, FFN, D),
            "router_w": (D, NE), "router_b4": (1, 4 * NE),
            "moe_w13": (NE, D, 2 * EXP), "moe_w2": (NE, EXP, D),
        }
        if not do_moe:
            for k in ("router_w", "router_b4", "moe_w13", "moe_w2"):
                del self.in_shapes[k]
        prog_self = self

        class LazyI(dict):
            def __missing__(d, k):
                inp(k, prog_self.in_shapes[k])
                return d[k]
        self.I = LazyI()
        self.out = nc.dram_tensor("outT", [D, NTOK], F32, kind="ExternalOutput").ap()
        dk = "ExternalOutput" if debug else "Internal"

        def scr(name, shape, dt=F32, kind="Internal"):
            return nc.dram_tensor(name, list(shape), dt, kind=kind).ap()

        self.S = {
            "xmA": scr("xmA", (D, NTOK)), "x1A": scr("x1A", (D, NTOK)),
            "xmB": scr("xmB", (D, NTOK), kind=dk), "x1B": scr("x1B", (D, NTOK), kind=dk),
            "xm2": scr("xm2", (D, NTOK), kind=dk),
            "zsc": scr("zsc", (D, T)),
            "ssmW": scr("ssmW", (128, 32, 4, 128), BF16), "rot": scr("rot", (128, 32, 128)),
            "tabs": scr("tabs", (32, 128, 2, T)),
        }
        self.dB = {}
        with contextlib.ExitStack() as es:
            self.es = es
            self.c = Ctx(nc, es)
            self.build()

    def db(self, *key):
        if key not in self.dB:
            self.dB[key] = Buf(str(key))
        return self.dB[key]

    def gsb(self, name, shape, dt=F32):
        return self.es.enter_context(self.nc.sbuf_tensor(name, list(shape), dt)), Buf(name)

    def bank(self):
        i = self.rot_banks[self.bi]
        self.bi = (self.bi + 1) % len(self.rot_banks)
        return self.ps[i], self.psB[i]

    def mm(self, out, lhsT, rhs, start, stop, reads, wb, inc=None):
        nc = self.nc
        self.c.op("pe", lambda: nc.tensor.matmul(out, lhsT=lhsT, rhs=rhs, start=start, stop=stop),
                  reads=reads, writes=[wb], inc=(stop if inc is None else inc))

    def wload(self, src3, shape, q="pool"):
        t, b, key = self.wp.get()
        n = 1
        for s in shape[1:]:
            n *= s
        if len(shape) == 3:
            v = t[0:shape[0], 0:n].rearrange("p (k n) -> p k n", k=shape[1])
        else:
            v = t[0:shape[0], 0:n]
        self.c.dma(q, v, src3, "d_" + key, writes=[b])
        return v, b

    def act(self, out, in_, func, reads, writes, **kw):
        nc = self.nc
        return self.c.op("act", lambda: nc.scalar.activation(out=out, in_=in_, func=func, **kw), reads=reads, writes=writes)

    def tt(self, e, out, in0, in1, op, reads, writes):
        eng = self.c.eng[e]
        return self.c.op(e, lambda: eng.tensor_tensor(out=out, in0=in0, in1=in1, op=op), reads=reads, writes=writes)

    def ts(self, e, out, in0, s1, s2, op0, op1, reads, writes):
        eng = self.c.eng[e]
        if op1 is None:
            return self.c.op(e, lambda: eng.tensor_scalar(out=out, in0=in0, scalar1=s1, scalar2=None, op0=op0), reads=reads, writes=writes)
        return self.c.op(e, lambda: eng.tensor_scalar(out=out, in0=in0, scalar1=s1, scalar2=s2, op0=op0, op1=op1), reads=reads, writes=writes)

    def stt(self, e, out, in0, scalar, in1, op0, op1, reads, writes):
        eng = self.c.eng[e]
        return self.c.op(e, lambda: eng.scalar_tensor_tensor(out=out, in0=in0, scalar=scalar, in1=in1, op0=op0, op1=op1), reads=reads, writes=writes)

    def build(self):
        nc, c, es = self.nc, self.c, self.es
        I = self.I
        self.ps = [es.enter_context(nc.psum_tensor(f"ps{i}", [128, T], F32)) for i in range(8)]
        self.psB = [Buf(f"ps{i}") for i in range(8)]
        self.rot_banks = [0, 1, 2, 3, 4, 7]
        self.bi = 0
        G = {}
        for name, shape in (("ident", (128, 128)), ("swapm", (128, 128)), ("tril", (128, 128)),
                            ("sgn", (128, 2)), ("rowmask", (128, 8)), ("flag", (128, 1))):
            G[name] = self.gsb("g_" + name, shape)
            c.dma("sp", G[name][0][:], I[name], "gl", writes=[G[name][1]])
        c.sync_group("gl", [G[n_][1] for n_ in ("ident", "swapm", "tril", "sgn", "rowmask", "flag")])
        G["onesf"] = self.gsb("g_onesf", (128, 128))
        c.op("dve", lambda: nc.vector.memset(G["onesf"][0][:], 1.0), writes=[G["onesf"][1]])
        G["onesb"] = self.gsb("g_onesb", (128, 128), BF16)
        c.op("dve", lambda: nc.vector.memset(G["onesb"][0][:], 1.0), writes=[G["onesb"][1]])
        G["cTb"] = self.gsb("g_cTb", (128, KC), BF16)
        c.dma("pool", G["cTb"][0][:], I["cT"], "gl2", writes=[G["cTb"][1]])
        for name, shape, dt in (("rho", (128, 32), F32), ("modv", (128, 96), F32), ("mod1", (128, 96), F32),
                                ("WsT", (128, 8, 128), BF16), ("Gt", (128, 768), F32), ("Bt", (128, 768), F32),
                                ("diagD", (128, 4, 128), BF16), ("glub", (128, 4), F32), ("convw", (128, 6, 3), F32),
                                ("sgb", (128, 1024), BF16), ("carry", (128, 6, 2), F32)):
            G[name] = self.gsb("g_" + name, shape, dt)
        self.ginit = [self.gsb(f"g_ginit{q}", (128, 8)) for q in range(4)]
        self.G = G

        if self.ckpt == -1:
            c.barrier()
            return
        for l in range(self.n_layers):
            self.layer_init(l)
            if l == 0:
                self.zero_state()
                if not self.single:
                    self.mixer_phase(l, I["xA"], self.S["xmA"], "xA", "xmA")
                    self.ffn_phase(l, self.S["xmA"], self.S["x1A"], "xmA", "x1A")
                    self.apply_flag()
                self.mixer_phase(l, I["xB"], self.S["xmB"], "xB", "xmB")
                last = self.n_layers == 1
                self.ffn_phase(l, self.S["xmB"], self.out if last else self.S["x1B"], "xmB", "out" if last else "x1B")
            else:
                self.zero_state()
                if not self.single:
                    self.mixer_phase(l, self.S["x1A"], None, "x1A", None, state_only=True)
                    self.apply_flag()
                self.mixer_phase(l, self.S["x1B"], self.S["xm2"], "x1B", "xm2")
                if self.do_moe:
                    self.moe_phase(self.S["xm2"], self.out, "xm2", "out")
                else:
                    self.copy_phase(self.S["xm2"], self.out, "xm2", "out")
        c.barrier()

    def zero_state(self):
        nc, c = self.nc, self.c
        for q in range(4):
            t, b = self.ginit[q]
            c.op("dve", lambda: nc.vector.memset(t[:], 0.0), writes=[b])
        t, b = self.G["carry"]
        c.op("dve", lambda: nc.vector.memset(t[:], 0.0), writes=[b])

    def apply_flag(self):
        fl, fb = self.G["flag"]
        for q in range(4):
            t, b = self.ginit[q]
            self.ts("dve", t[:], t[:], fl[:, 0:1], None, ALU.mult, None, [b, fb], [b])
        t, b = self.G["carry"]
        self.ts("dve", t[:], t[:], fl[:, 0:1], None, ALU.mult, None, [b, fb], [b])

    def layer_init(self, l):
        self.phase_i += 1
        if self.stop_after is not None and self.phase_i > self.stop_after:
            return
        nc, c, I, G = self.nc, self.c, self.I, self.G
        c.barrier()
        with contextlib.ExitStack() as pes:
            def sb(name, shape, dt=F32):
                Pool.UID[0] += 1
                return pes.enter_context(nc.sbuf_tensor(f"{name}_u{Pool.UID[0]}", list(shape), dt)), Buf(name)
            self.wp = Pool(nc, pes, "w", 3, [128, 8192], BF16)
            cTb, cTbB = G["cTb"]
            cTrep, cTrepB = sb("cTrep", (128, KC, 128), BF16)
            c.op("dve", lambda: nc.vector.tensor_copy(out=cTrep[:], in_=cTb[:].unsqueeze(2).to_broadcast([128, KC, 128])), reads=[cTbB], writes=[cTrepB])
            mraw, mrawB = sb("mraw", (128, 96))
            dtmp, dtmpB = sb("dtmp", (128, 4, 128))
            ident, identB = G["ident"]
            for blk in range(24):
                wv, wb = self.wload(I["ada_w"][l].rearrange("(k p) n -> p k n", p=128)[:, :, blk * 512:(blk + 1) * 512], (128, KC, 512))
                pm, pmB = self.bank()
                for k in range(KC):
                    self.mm(pm[:], cTrep[:, k, :], wv[:, k, :], k == 0, k == KC - 1, [wb, cTrepB], pmB)
                self.tt("dve", dtmp[:], pm[:].rearrange("p (a b) -> p a b", a=4), ident[:].unsqueeze(1).to_broadcast([128, 4, 128]), ALU.mult, [pmB, identB], [dtmpB])
                c.op("dve", lambda: nc.vector.reduce_sum(out=mraw[:, blk * 4:(blk + 1) * 4], in_=dtmp[:], axis=mybir.AxisListType.X), reads=[dtmpB], writes=[mrawB])
            if self.ckpt == 1:
                c.barrier()
                return
            adab, adabB = sb("adab", (128, 96))
            c.dma("sp", adab[:], I["ada_bc"][l], "li0", writes=[adabB])
            mv, mvB = G["modv"]
            m1, m1B = G["mod1"]
            self.tt("dve", mv[:], mraw[:], adab[:], ALU.add, [mrawB, adabB], [mvB])
            self.ts("dve", m1[:], mv[:], 1.0, None, ALU.add, None, [mvB], [m1B])
            if self.ckpt == 2:
                c.barrier()
                return
            c.dma("sp", G["glub"][0][:], I["glu_bc"][l], "li1", writes=[G["glub"][1]])
            c.dma("sp", G["convw"][0][:], I["convw"][l], "li1", writes=[G["convw"][1]])
            c.sync_group("li1", [G["glub"][1], G["convw"][1]])
            c.dma("sp", G["Gt"][0][:], I["sg_ln_g"][l:l + 1, :].partition_broadcast(128), "li2", writes=[G["Gt"][1]])
            c.dma("sp", G["Bt"][0][:], I["sg_ln_b"][l:l + 1, :].partition_broadcast(128), "li2", writes=[G["Bt"][1]])
            c.sync_group("li2", [G["Gt"][1], G["Bt"][1]])
            c.op("dve", lambda: nc.vector.memset(G["sgb"][0][:], 0.0), writes=[G["sgb"][1]])
            c.dma("pool", G["sgb"][0][0:1, :], I["sg_b"][l], "li3", reads=[G["sgb"][1]], writes=[G["sgb"][1]])
            if self.ckpt == 3:
                c.barrier()
                return
            wsf, wsfB = sb("wsf", (128, 8, 128))
            c.dma("sp", wsf[:], I["sgwT"][l].rearrange("h s t -> s h t"), "li4", writes=[wsfB])
            tril, trilB = G["tril"]
            for h in range(8):
                self.tt("dve", G["WsT"][0][:, h, :], wsf[:, h, :], tril[:], ALU.mult, [wsfB, trilB], [G["WsT"][1]])
            dcol, dcolB = sb("dcol", (128, 4))
            c.dma("sp", dcol[:], I["dcol"][l], "li5", writes=[dcolB])
            for q in range(4):
                self.ts("dve", G["diagD"][0][:, q, :], ident[:], dcol[:, q:q + 1], None, ALU.mult, None, [identB, dcolB], [G["diagD"][1]])
            if self.ckpt == 4:
                c.barrier()
                return
            are, areB = sb("are", (128, 32)); aim, aimB = sb("aim", (128, 32)); dt_, dtB = sb("dt", (128, 32))
            c.dma("sp", are[:], I["a_re2"][l], "li6", writes=[areB])
            c.dma("sp", aim[:], I["a_im2"][l], "li6", writes=[aimB])
            c.dma("sp", dt_[:], I["ldt2"][l], "li6", writes=[dtB])
            c.sync_group("li6", [areB, aimB, dtB])
            self.act(dt_[:], dt_[:], AF.Exp, [dtB], [dtB])
            S = {}
            for nm in ("ang", "k", "r", "cosA", "sinA", "are_dt", "abr", "abi", "den", "nre", "t1", "t2", "fre", "fim",
                       "fimS", "freS", "th", "ang5", "Ere", "Eim", "Eim2", "rden"):
                S[nm] = sb("s_" + nm, (128, 32))
            rho, rhoB = G["rho"]

            def V(nm):
                return S[nm][0][:]

            def B_(nm):
                return S[nm][1]

            def reduce_sin(dn, sn, shift):
                self.ts("dve", V("k"), V(sn), 1.0, shift, ALU.mult, ALU.add, [B_(sn)], [B_("k")])
                self.ts("dve", V("r"), V("k"), 1.0 / TWO_PI, MAGIC, ALU.mult, ALU.add, [B_("k")], [B_("r")])
                self.ts("dve", V("r"), V("r"), MAGIC, None, ALU.subtract, None, [B_("r")], [B_("r")])
                self.stt("dve", V("r"), V("r"), -TWO_PI, V("k"), ALU.mult, ALU.add, [B_("r"), B_("k")], [B_("r")])
                self.ts("dve", V("r"), V("r"), -PI_CL, PI_CL, ALU.max, ALU.min, [B_("r")], [B_("r")])
                self.act(V(dn), V("r"), AF.Sin, [B_("r")], [B_(dn)])

            self.tt("dve", V("ang"), dt_[:], aim[:], ALU.mult, [dtB, aimB], [B_("ang")])
            self.tt("dve", V("are_dt"), dt_[:], are[:], ALU.mult, [dtB, areB], [B_("are_dt")])
            self.act(rho[:], V("are_dt"), AF.Exp, [B_("are_dt")], [rhoB])
            reduce_sin("sinA", "ang", 0.0)
            reduce_sin("cosA", "ang", math.pi / 2)
            self.tt("dve", V("abr"), rho[:], V("cosA"), ALU.mult, [rhoB, B_("cosA")], [B_("abr")])
            self.tt("dve", V("abi"), rho[:], V("sinA"), ALU.mult, [rhoB, B_("sinA")], [B_("abi")])
            self.tt("dve", V("den"), are[:], are[:], ALU.mult, [areB], [B_("den")])
            self.tt("dve", V("t1"), aim[:], aim[:], ALU.mult, [aimB], [B_("t1")])
            self.tt("dve", V("den"), V("den"), V("t1"), ALU.add, [B_("den"), B_("t1")], [B_("den")])
            c.op("dve", lambda: nc.vector.reciprocal(out=V("rden"), in_=V("den")), reads=[B_("den")], writes=[B_("rden")])
            self.ts("dve", V("nre"), V("abr"), -1.0, None, ALU.add, None, [B_("abr")], [B_("nre")])
            self.tt("dve", V("t1"), V("nre"), are[:], ALU.mult, [B_("nre"), areB], [B_("t1")])
            self.tt("dve", V("t2"), V("abi"), aim[:], ALU.mult, [B_("abi"), aimB], [B_("t2")])
            self.tt("dve", V("t1"), V("t1"), V("t2"), ALU.add, [B_("t1"), B_("t2")], [B_("t1")])
            self.tt("dve", V("fre"), V("t1"), V("rden"), ALU.mult, [B_("t1"), B_("rden")], [B_("fre")])
            self.tt("dve", V("t1"), V("abi"), are[:], ALU.mult, [B_("abi"), areB], [B_("t1")])
            self.tt("dve", V("t2"), V("nre"), aim[:], ALU.mult, [B_("nre"), aimB], [B_("t2")])
            self.tt("dve", V("t1"), V("t1"), V("t2"), ALU.subtract, [B_("t1"), B_("t2")], [B_("t1")])
            self.tt("dve", V("fim"), V("t1"), V("rden"), ALU.mult, [B_("t1"), B_("rden")], [B_("fim")])
            sgn, sgnB = G["sgn"]
            self.ts("dve", V("fimS"), V("fim"), sgn[:, 0:1], None, ALU.mult, None, [B_("fim"), sgnB], [B_("fimS")])
            self.ts("dve", V("freS"), V("fre"), sgn[:, 1:2], None, ALU.mult, None, [B_("fre"), sgnB], [B_("freS")])
            if self.ckpt == 5:
                c.barrier()
                return
            bA, bAB = sb("bA", (128, 32, 16)); bB, bBB = sb("bB", (128, 32, 16))
            c.dma("sp", bA[:], I["bA"][l], "li7", writes=[bAB])
            c.dma("sp", bB[:], I["bB"][l], "li7", writes=[bBB])
            c.sync_group("li7", [bAB, bBB])
            X1, X1B = sb("X1", (128, 32, 16)); X2, X2B = sb("X2", (128, 32, 16)); Xt, XtB = sb("Xt", (128, 32, 16))

            def bc16(nm):
                return S[nm][0][:].unsqueeze(2).to_broadcast([128, 32, 16])
            self.tt("dve", X1[:], bA[:], bc16("fre"), ALU.mult, [bAB, B_("fre")], [X1B])
            self.tt("dve", Xt[:], bB[:], bc16("fimS"), ALU.mult, [bBB, B_("fimS")], [XtB])
            self.tt("dve", X1[:], X1[:], Xt[:], ALU.add, [X1B, XtB], [X1B])
            self.tt("dve", X2[:], bB[:], bc16("freS"), ALU.mult, [bBB, B_("freS")], [X2B])
            self.tt("dve", Xt[:], bA[:], bc16("fim"), ALU.mult, [bAB, B_("fim")], [XtB])
            self.tt("dve", X2[:], X2[:], Xt[:], ALU.add, [X2B, XtB], [X2B])
            if self.ckpt == 6:
                c.barrier()
                return
            Wall, WallB = sb("Wall", (128, 32, 4, 128), BF16)
            c.op("pool", lambda: nc.gpsimd.memset(Wall[:], 0.0), writes=[WallB])
            rowmask, rmB = G["rowmask"]
            for mi, (X, XB) in enumerate(((X1, X1B), (X2, X2B))):
                for q in range(4):
                    pt, ptB = self.bank()
                    c.op("pe", lambda: nc.tensor.transpose(out=pt[:, 0:128], in_=X[:, q * 8:(q + 1) * 8, :].rearrange("p g c -> p (g c)"), identity=ident[:]),
                         reads=[XB, identB], writes=[ptB])
                    for j in range(8):
                        self.ts("dve", Wall[:, q * 8 + j, mi, :], pt[:, 0:128], rowmask[:, j:j + 1], None, ALU.mult, None, [ptB, rmB], [WallB])
            if self.ckpt == 7:
                c.barrier()
                return
            cA, cAB = sb("cA", (128, 32, 16)); cB, cBB = sb("cB", (128, 32, 16))
            c.dma("sp", cA[:], I["cA"][l], "li8", writes=[cAB])
            c.dma("sp", cB[:], I["cB"][l], "li8", writes=[cBB])
            c.sync_group("li8", [cAB, cBB])
            W5 = Wall[:].rearrange("p (q j) m (jj cc) -> p q j m jj cc", j=8, jj=8)
            for j in range(8):
                self.ts("dve", W5[:, :, j, 2, j, :], cA[:].rearrange("p (q j) cc -> p q j cc", j=8)[:, :, j, :], sgn[:, 1:2], None, ALU.mult, None, [cAB, sgnB], [WallB])
                self.ts("dve", W5[:, :, j, 3, j, :], cB[:].rearrange("p (q j) cc -> p q j cc", j=8)[:, :, j, :], -1.0, None, ALU.mult, None, [cBB], [WallB])
            SB_W = self.db("ssmW")
            c.dma("sp", self.S["ssmW"], Wall[:], "li9", reads=[WallB], writes=[SB_W])
            if self.ckpt == 8:
                c.barrier()
                return
            self.ts("dve", V("th"), V("ang"), 1.0 / TWO_PI, MAGIC, ALU.mult, ALU.add, [B_("ang")], [B_("th")])
            self.ts("dve", V("th"), V("th"), MAGIC, None, ALU.subtract, None, [B_("th")], [B_("th")])
            self.stt("dve", V("th"), V("th"), -TWO_PI, V("ang"), ALU.mult, ALU.add, [B_("th"), B_("ang")], [B_("th")])
            self.ts("dve", V("ang5"), V("th"), float(T), None, ALU.mult, None, [B_("th")], [B_("ang5")])
            reduce_sin("Eim", "ang5", 0.0)
            reduce_sin("Ere", "ang5", math.pi / 2)
            self.ts("dve", V("Eim2"), V("Eim"), sgn[:, 1:2], None, ALU.mult, None, [B_("Eim"), sgnB], [B_("Eim2")])
            rotS, rotSB = sb("rotS", (128, 32, 128))
            swapm, swB = G["swapm"]
            for g in range(32):
                self.ts("dve", rotS[:, g, :], ident[:], S["Ere"][0][:, g:g + 1], None, ALU.mult, None, [identB, B_("Ere")], [rotSB])
                self.stt("dve", rotS[:, g, :], swapm[:], S["Eim2"][0][:, g:g + 1], rotS[:, g, :], ALU.mult, ALU.add, [swB, B_("Eim2"), rotSB], [rotSB])
            SB_R = self.db("rot")
            c.dma("sp", self.S["rot"], rotS[:], "li9b", reads=[rotSB], writes=[SB_R])
            if self.ckpt == 9:
                c.barrier()
                return
            iota, iotaB = sb("iota", (128, T))
            c.dma("sp", iota[:], I["iota"], "li10", writes=[iotaB])
            tp = Pool(nc, pes, "tabt", 2, [128, 2, T], F32)
            xk, xkB = sb("xk", (128, T)); rr, rrB = sb("rr", (128, T))
            SB_T = self.db("tabs")
            for g in range(32):
                tb, tbB, key = tp.get()
                for which, shift in ((0, math.pi / 2), (1, 0.0)):
                    self.ts("dve", xk[:], iota[:], S["th"][0][:, g:g + 1], shift, ALU.mult, ALU.add, [iotaB, B_("th")], [xkB])
                    self.ts("dve", rr[:], xk[:], 1.0 / TWO_PI, MAGIC, ALU.mult, ALU.add, [xkB], [rrB])
                    self.ts("dve", rr[:], rr[:], MAGIC, None, ALU.subtract, None, [rrB], [rrB])
                    self.stt("dve", rr[:], rr[:], -TWO_PI, xk[:], ALU.mult, ALU.add, [rrB, xkB], [rrB])
                    self.ts("dve", rr[:], rr[:], -PI_CL, PI_CL, ALU.max, ALU.min, [rrB], [rrB])
                    self.act(tb[:, which, :], rr[:], AF.Sin, [rrB], [tbB])
                c.dma("sp", self.S["tabs"][g], tb[:], "s_" + key, reads=[tbB], writes=[SB_T])
            c.barrier()

    def post_begin(self):
        self.S1, self.S1B = self.ps[5], self.psB[5]
        self.S2, self.S2B = self.ps[6], self.psB[6]

    def post_chunk(self, f, ysrc, yB, xres_dram, xres_key, ti, gcol):
        nc, c, G = self.nc, self.c, self.G
        xr, xrB, key = self.ftp.get()
        c.dma("sp", xr[:, 0:T], xres_dram[f * 128:(f + 1) * 128, ti * T:(ti + 1) * T], "d_" + key,
              reads=[self.db(xres_key, ti, f)], writes=[xrB])
        self.act(xr[:, 0:T], xr[:, 0:T], AF.Copy, [xrB], [xrB], scale=ALPHA)
        m1, m1B = G["mod1"]
        self.stt("dve", xr[:, 0:T], ysrc, m1[:, gcol + f:gcol + f + 1], xr[:, 0:T], ALU.mult, ALU.add, [yB, m1B, xrB], [xrB])
        zq, zqB, _ = self.ftp.get()
        self.act(zq[:, 0:T], xr[:, 0:T], AF.Square, [xrB], [zqB])
        onesf, onesB = G["onesf"]
        self.mm(self.S1[:], onesf[:], xr[:, 0:T], f == 0, f == KC - 1, [onesB, xrB], self.S1B)
        self.mm(self.S2[:], onesf[:], zq[:, 0:T], f == 0, f == KC - 1, [onesB, zqB], self.S2B, inc=True)
        c.dma("sp", self.S["zsc"][f * 128:(f + 1) * 128, :], xr[:, 0:T], "s_" + key, reads=[xrB], writes=[self.db("zsc", f)])

    def post_finish(self, xout_dram, xout_key, ti, shcol, sccol, also_bf16=None):
        nc, c, G = self.nc, self.c, self.G
        (mean, meanB), (rstd, rstdB), (msq, msqB) = self.lnb
        self.act(mean[:, 0:T], self.S1[:], AF.Copy, [self.S1B], [meanB], scale=1.0 / D)
        self.tt("dve", msq[:, 0:T], mean[:, 0:T], mean[:, 0:T], ALU.mult, [meanB], [msqB])
        self.stt("dve", msq[:, 0:T], self.S2[:], 1.0 / D, msq[:, 0:T], ALU.mult, ALU.subtract, [self.S2B, msqB], [msqB])
        self.ts("dve", msq[:, 0:T], msq[:, 0:T], EPS, None, ALU.add, None, [msqB], [msqB])
        self.act(msq[:, 0:T], msq[:, 0:T], AF.Sqrt, [msqB], [msqB])
        c.op("dve", lambda: nc.vector.reciprocal(out=rstd[:, 0:T], in_=msq[:, 0:T]), reads=[msqB], writes=[rstdB])
        mv, mvB = G["modv"]
        m1, m1B = G["mod1"]
        for f in range(KC):
            zt, ztB, key = self.ftp.get()
            c.dma("sp", zt[:, 0:T], self.S["zsc"][f * 128:(f + 1) * 128, :], "d_" + key, reads=[self.db("zsc", f)], writes=[ztB])
            self.tt("dve", zt[:, 0:T], zt[:, 0:T], mean[:, 0:T], ALU.subtract, [ztB, meanB], [ztB])
            self.tt("dve", zt[:, 0:T], zt[:, 0:T], rstd[:, 0:T], ALU.mult, [ztB, rstdB], [ztB])
            self.act(zt[:, 0:T], zt[:, 0:T], AF.Identity, [ztB, m1B, mvB], [ztB],
                     scale=m1[:, sccol + f:sccol + f + 1], bias=mv[:, shcol + f:shcol + f + 1])
            c.dma("sp", xout_dram[f * 128:(f + 1) * 128, ti * T:(ti + 1) * T], zt[:, 0:T], "s_" + key, reads=[ztB],
                  writes=[self.db(xout_key, ti, f)])

    def mixer_phase(self, l, xin, xout, xin_key, xout_key, state_only=False):
        self.phase_i += 1
        if self.stop_after is not None and self.phase_i > self.stop_after:
            return
        nc, c, I, G = self.nc, self.c, self.I, self.G
        c.barrier()
        with contextlib.ExitStack() as pes:
            def sb(name, shape, dt=F32):
                Pool.UID[0] += 1
                return pes.enter_context(nc.sbuf_tensor(f"{name}_u{Pool.UID[0]}", list(shape), dt)), Buf(name)
            self.wp = Pool(nc, pes, "w", 6, [128, 4096], BF16)
            self.ftp = Pool(nc, pes, "f", 10, [128, T + 2], F32)
            self.lnb = [sb("lnb%d" % i, (128, T)) for i in range(3)]
            btp = Pool(nc, pes, "mb", 4, [128, T], BF16)
            tabp = Pool(nc, pes, "mt", 3, [128, 2, T], F32)
            xb, xbB = sb("xb", (128, KC, T), BF16)
            ub, ubB = sb("ub", (128, 4, T), BF16)
            csb, csB = sb("cs", (128, 8, 4, 128), BF16)
            rsb, rsB = sb("rs", (128, 8, 128))
            gsl, gslB = sb("gsl", (128, 32))
            if not state_only:
                zab, zabB = sb("zab", (128, 4, T), BF16)
                zag, zagB = sb("zag", (128, 4, T), BF16)
                zb, zbB = sb("zb", (128, 8, T), BF16)
                zcb, zcbB = sb("zcb", (128, 6, T), BF16)
                vnb, vnbB = sb("vnb", (128, 4, 768), BF16)
                vf, vfB = sb("vf", (128, 768))
                merged, mergedB = sb("merged", (128, KC, T), BF16)
                brA, brAB = sb("brA", (128, 10, 512), BF16)
                brB, brBB = sb("brB", (128, 8, 512), BF16)
                st, stB = sb("st", (128, 12))
            Yb_, YB = self.ps[5], self.psB[5]
            Gn, GnB = self.ps[6], self.psB[6]
            rho, rhoB = G["rho"]
            w_in = I["w_in"][l].rearrange("(k p) n -> p k n", p=128)
            for ti in range(self.NT):
                tsl = slice(ti * T, (ti + 1) * T)
                c.dma("pool", xb[:], xin.rearrange("(k p) t -> p k t", p=128)[:, :, tsl], "d_xb",
                      reads=[self.db(xin_key, ti, f) for f in range(KC)], writes=[xbB])
                wvs = [self.wload(w_in[:, :, hh * 256:(hh + 1) * 256], (128, KC, 256)) for hh in range(2)]
                for q in range(4):
                    wv, wb = wvs[q // 2]
                    pb, pbB = self.bank()
                    for k in range(KC):
                        self.mm(pb[:], wv[:, k, (q % 2) * 128:(q % 2 + 1) * 128], xb[:, k, :], k == 0, k == KC - 1, [wb, xbB], pbB)
                    self.act(ub[:, q, :], pb[:], AF.Copy, [pbB], [ubB])
                for q in range(4):
                    c.dma("sp", csb[:], self.S["ssmW"][:, q * 8:(q + 1) * 8], "d_cs", reads=[self.db("ssmW")], writes=[csB])
                    c.dma("sp", rsb[:], self.S["rot"][:, q * 8:(q + 1) * 8, :], "d_rs", reads=[self.db("rot")], writes=[rsB])
                    gi, giB = self.ginit[q]
                    if not state_only:
                        self.mm(Yb_[:], G["diagD"][0][:, q, :], ub[:, q, :], True, False, [G["diagD"][1], ubB], YB, inc=False)
                    for j in range(8):
                        g = q * 8 + j
                        tab, tabB, tkey = tabp.get()
                        c.dma("sp", tab[:], self.S["tabs"][g], "d_" + tkey, reads=[self.db("tabs")], writes=[tabB])
                        p1, p1B = self.bank()
                        self.mm(p1[:], csb[:, j, 0, :], ub[:, q, :], True, True, [csB, ubB], p1B)
                        p2, p2B = self.bank()
                        self.mm(p2[:], csb[:, j, 1, :], ub[:, q, :], True, True, [csB, ubB], p2B)
                        t1, t1B, _ = self.ftp.get()
                        t2, t2B, _ = self.ftp.get()
                        self.tt("dve", t1[:, 0:T], p1[:], tab[:, 0, :], ALU.mult, [p1B, tabB], [t1B])
                        self.tt("dve", t2[:, 0:T], p2[:], tab[:, 1, :], ALU.mult, [p2B, tabB], [t2B])
                        self.tt("dve", t1[:, 0:T], t1[:, 0:T], t2[:, 0:T], ALU.add, [t1B, t2B], [t1B])
                        gs, gsB, _ = self.ftp.get()
                        c.op("dve", lambda: nc.vector.tensor_tensor_scan(out=gs[:, 0:T], data0=rho[:, g:g + 1].to_broadcast([128, T]), data1=t1[:, 0:T],
                                                                          initial=gi[:, j:j + 1], op0=ALU.mult, op1=ALU.add),
                             reads=[rhoB, t1B, giB], writes=[gsB])
                        self.mm(Gn[:, g:g + 1], rsb[:, j, :], gs[:, T - 1:T], True, True, [rsB, gsB], GnB, inc=True)
                        if not state_only:
                            gc, gcB, _ = btp.get()
                            gsn, gsnB, _ = btp.get()
                            self.tt("dve", gc[:], gs[:, 0:T], tab[:, 0, :], ALU.mult, [gsB, tabB], [gcB])
                            self.tt("dve", gsn[:], gs[:, 0:T], tab[:, 1, :], ALU.mult, [gsB, tabB], [gsnB])
                            self.mm(Yb_[:], csb[:, j, 2, :], gc[:], False, False, [csB, gcB], YB, inc=False)
                            self.mm(Yb_[:], csb[:, j, 3, :], gsn[:], False, j == 7, [csB, gsnB], YB, inc=True)
                    self.act(gi[:], Gn[:, q * 8:(q + 1) * 8], AF.Copy, [GnB], [giB])
                    if not state_only:
                        self.act(zab[:, q, :], Yb_[:], AF.Gelu_apprx_tanh, [YB], [zabB])
                        if q < 3:
                            self.conv_z(l, w_in, xb, xbB, zcb, zcbB, parts=(q,))
                if state_only:
                    if ti == self.NT - 1:
                        self.conv_z(l, w_in, xb, xbB, None, None, carry_only=True)
                    continue
                gw, gwB = self.wload(I["glu_w"][l].rearrange("(k p) n -> p k n", p=128), (128, 4, 512))
                glub, glubB = G["glub"]
                for qo in range(4):
                    pb, pbB = self.bank()
                    for k in range(4):
                        self.mm(pb[:], gw[:, k, qo * 128:(qo + 1) * 128], zab[:, k, :], k == 0, k == 3, [gwB, zabB], pbB)
                    sg, sgB, _ = self.ftp.get()
                    self.act(sg[:, 0:T], pb[:], AF.Sigmoid, [pbB, glubB], [sgB], bias=glub[:, qo:qo + 1], scale=1.0)
                    self.tt("dve", zag[:, qo, :], zab[:, qo, :], sg[:, 0:T], ALU.mult, [zabB, sgB], [zagB])
                wvh = [self.wload(w_in[:, :, 1280 + j * 256:1280 + (j + 1) * 256], (128, KC, 256)) for j in range(3)]
                Gt, GtB = G["Gt"]
                Bt, BtB = G["Bt"]
                for cc in range(4):
                    pv = [self.bank() for _ in range(3)]
                    for j in range(3):
                        for k in range(KC):
                            self.mm(pv[j][0][:, 0:256], xb[:, k, cc * 128:(cc + 1) * 128], wvh[j][0][:, k, :], k == 0, k == KC - 1,
                                    [xbB, wvh[j][1]], pv[j][1])
                    jk, jkB, _ = self.ftp.get()
                    for j in range(3):
                        self.act(vf[:, j * 256:(j + 1) * 256], pv[j][0][:, 0:256], AF.Copy, [pv[j][1]], [vfB, stB], accum_out=st[:, j:j + 1])
                        self.act(jk[:, 0:256], pv[j][0][:, 0:256], AF.Square, [pv[j][1]], [jkB, stB], accum_out=st[:, 3 + j:4 + j])
                    self.tt("dve", st[:, 8:9], st[:, 0:1], st[:, 1:2], ALU.add, [stB], [stB])
                    self.tt("dve", st[:, 8:9], st[:, 8:9], st[:, 2:3], ALU.add, [stB], [stB])
                    self.ts("dve", st[:, 8:9], st[:, 8:9], 1.0 / 768, None, ALU.mult, None, [stB], [stB])
                    self.tt("dve", st[:, 9:10], st[:, 3:4], st[:, 4:5], ALU.add, [stB], [stB])
                    self.tt("dve", st[:, 9:10], st[:, 9:10], st[:, 5:6], ALU.add, [stB], [stB])
                    self.tt("dve", st[:, 10:11], st[:, 8:9], st[:, 8:9], ALU.mult, [stB], [stB])
                    self.stt("dve", st[:, 9:10], st[:, 9:10], 1.0 / 768, st[:, 10:11], ALU.mult, ALU.subtract, [stB], [stB])
                    self.ts("dve", st[:, 9:10], st[:, 9:10], EPS, None, ALU.add, None, [stB], [stB])
                    self.act(st[:, 9:10], st[:, 9:10], AF.Sqrt, [stB], [stB])
                    c.op("dve", lambda: nc.vector.reciprocal(out=st[:, 11:12], in_=st[:, 9:10]), reads=[stB], writes=[stB])
                    self.ts("dve", vf[:], vf[:], st[:, 8:9], st[:, 11:12], ALU.subtract, ALU.mult, [vfB, stB], [vfB])
                    self.tt("dve", vf[:], vf[:], Gt[:], ALU.mult, [vfB, GtB], [vfB])
                    self.tt("dve", vnb[:, cc, :], vf[:], Bt[:], ALU.add, [vfB, BtB], [vnbB])
                WsT, WsTB = G["WsT"]
                sgb, sgbB = G["sgb"]
                onesb, onesbB = G["onesb"]
                wuh = [self.wload(w_in[:, :, 512 + j * 192:512 + (j + 1) * 192], (128, KC, 192)) for j in range(4)]
                for h in range(8):
                    pu, puB = self.bank()
                    wu, wuB = wuh[h // 2]
                    for k in range(KC):
                        self.mm(pu[0:96, :], wu[:, k, (h % 2) * 96:(h % 2 + 1) * 96], xb[:, k, :], k == 0, k == KC - 1, [wuB, xbB], puB)
                    uf, ufB, _ = self.ftp.get()
                    self.act(uf[0:96, 0:T], pu[0:96, :], AF.Copy, [puB], [ufB])
                    pss, pssB = self.bank()
                    for cc in range(4):
                        self.mm(pss[0:96, cc * 128:(cc + 1) * 128], vnb[:, cc, h * 96:(h + 1) * 96], WsT[:, h, :], True, False, [vnbB, WsTB], pssB, inc=False)
                        self.mm(pss[0:96, cc * 128:(cc + 1) * 128], onesb[:, 0:96], sgb[:, h * 128:(h + 1) * 128], False, True,
                                [onesbB, sgbB], pssB, inc=(cc == 3))
                    self.tt("dve", zb[0:96, h, :], pss[0:96, :], uf[0:96, 0:T], ALU.mult, [pssB, ufB], [zbB])
                for fb8 in range(8):
                    fb, fh = fb8 // 2, fb8 % 2
                    wg = [self.wload(w_in[:, :, 4352 + j * D + fb8 * 256:4352 + j * D + (fb8 + 1) * 256], (128, KC, 256)) for j in range(3)]
                    if fh == 0:
                        c.dma("pool", brA[:, 0:4, :], I["w_branch_a"][l].rearrange("(k p) n -> p k n", p=128)[:, :, fb * 512:(fb + 1) * 512], "d_brA", writes=[brAB])
                        c.dma("pool", brA[:, 4:10, :], I["w_branch_c"][l].rearrange("(k p) n -> p k n", p=128)[:, :, fb * 512:(fb + 1) * 512], "d_brA", writes=[brAB])
                        c.dma("pool", brB[0:96, :, :], I["w_branch_b"][l].rearrange("(k p) n -> p k n", p=96)[:, :, fb * 512:(fb + 1) * 512], "d_brB", writes=[brBB])
                    for fi2 in range(2):
                        fi = fh * 2 + fi2
                        f = fb * 4 + fi
                        fs = slice(fi * 128, (fi + 1) * 128)
                        gsl_ = slice(fi2 * 128, (fi2 + 1) * 128)
                        sig = []
                        for j in range(3):
                            pg, pgB = self.bank()
                            for k in range(KC):
                                self.mm(pg[:], wg[j][0][:, k, gsl_], xb[:, k, :], k == 0, k == KC - 1, [wg[j][1], xbB], pgB)
                            s_, sB_, _ = self.ftp.get()
                            self.act(s_[:, 0:T], pg[:], AF.Sigmoid, [pgB], [sB_])
                            sig.append((s_, sB_))
                        ya, yaB = self.bank()
                        for k in range(4):
                            self.mm(ya[:], brA[:, k, fs], zag[:, k, :], k == 0, k == 3, [brAB, zagB], yaB)
                        self.tt("dve", sig[0][0][:, 0:T], ya[:], sig[0][0][:, 0:T], ALU.mult, [yaB, sig[0][1]], [sig[0][1]])
                        yb2, yb2B = self.bank()
                        for k in range(8):
                            self.mm(yb2[:], brB[0:96, k, fs], zb[0:96, k, :], k == 0, k == 7, [brBB, zbB], yb2B)
                        self.tt("dve", sig[1][0][:, 0:T], yb2[:], sig[1][0][:, 0:T], ALU.mult, [yb2B, sig[1][1]], [sig[1][1]])
                        yc, ycB = self.bank()
                        for k in range(6):
                            self.mm(yc[:], brA[:, 4 + k, fs], zcb[:, k, :], k == 0, k == 5, [brAB, zcbB], ycB)
                        self.tt("dve", sig[2][0][:, 0:T], yc[:], sig[2][0][:, 0:T], ALU.mult, [ycB, sig[2][1]], [sig[2][1]])
                        self.tt("dve", sig[0][0][:, 0:T], sig[0][0][:, 0:T], sig[1][0][:, 0:T], ALU.add, [sig[0][1], sig[1][1]], [sig[0][1]])
                        self.tt("dve", merged[:, f, :], sig[0][0][:, 0:T], sig[2][0][:, 0:T], ALU.add, [sig[0][1], sig[2][1]], [mergedB])
                self.post_begin()
                for fb in range(8):
                    wo, woB = self.wload(I["w_o"][l].rearrange("(k p) n -> p k n", p=128)[:, :, fb * 256:(fb + 1) * 256], (128, KC, 256))
                    for fi in range(2):
                        f = fb * 2 + fi
                        py, pyB = self.bank()
                        for k in range(KC):
                            self.mm(py[:], wo[:, k, fi * 128:(fi + 1) * 128], merged[:, k, :], k == 0, k == KC - 1, [woB, mergedB], pyB)
                        self.post_chunk(f, py[:], pyB, xin, xin_key, ti, 32)
                self.post_finish(xout, xout_key, ti, 0, 16)
            c.barrier()

    def conv_z(self, l, w_in, xb, xbB, zcb, zcbB, carry_only=False, parts=(0, 1, 2)):
        nc, c, G = self.nc, self.c, self.G
        carry, carryB = G["carry"]
        convw, convwB = G["convw"]
        for hv in parts:
            names = ("c", "h") if carry_only else ("b", "c", "h")
            col0 = {"b": 2048, "c": 2816, "h": 3584}
            w3 = {nm: self.wload(w_in[:, :, col0[nm] + hv * 256:col0[nm] + (hv + 1) * 256], (128, KC, 256)) for nm in names}
            for j in range(2):
                ch = hv * 2 + j
                pp = {}
                for nm in names:
                    pb, pbB = self.bank()
                    for k in range(KC):
                        self.mm(pb[:], w3[nm][0][:, k, j * 128:(j + 1) * 128], xb[:, k, :], k == 0, k == KC - 1, [w3[nm][1], xbB], pbB)
                    pp[nm] = (pb, pbB)
                cf, cfB, _ = self.ftp.get()
                self.act(cf[:, 0:T], pp["c"][0][:], AF.Copy, [pp["c"][1]], [cfB])
                zbuf, zbufB, _ = self.ftp.get()
                self.tt("dve", zbuf[:, 2:T + 2], cf[:, 0:T], pp["h"][0][:], ALU.mult, [cfB, pp["h"][1]], [zbufB])
                if not carry_only:
                    self.act(zbuf[:, 0:2], carry[:, ch, :], AF.Copy, [carryB], [zbufB])
                self.act(carry[:, ch, :], zbuf[:, T:T + 2], AF.Copy, [zbufB], [carryB])
                if carry_only:
                    continue
                acc, accB, _ = self.ftp.get()
                self.ts("dve", acc[:, 0:T], zbuf[:, 2:T + 2], convw[:, ch, 2:3], None, ALU.mult, None, [zbufB, convwB], [accB])
                self.stt("dve", acc[:, 0:T], zbuf[:, 1:T + 1], convw[:, ch, 1:2], acc[:, 0:T], ALU.mult, ALU.add, [zbufB, convwB, accB], [accB])
                self.stt("dve", acc[:, 0:T], zbuf[:, 0:T], convw[:, ch, 0:1], acc[:, 0:T], ALU.mult, ALU.add, [zbufB, convwB, accB], [accB])
                self.tt("dve", zcb[:, ch, :], acc[:, 0:T], pp["b"][0][:], ALU.mult, [accB, pp["b"][1]], [zcbB])

    def ffn_phase(self, l, xin, xout, xin_key, xout_key):
        self.phase_i += 1
        if self.stop_after is not None and self.phase_i > self.stop_after:
            return
        nc, c, I, G = self.nc, self.c, self.I, self.G
        c.barrier()
        with contextlib.ExitStack() as pes:
            def sb(name, shape, dt=F32):
                Pool.UID[0] += 1
                return pes.enter_context(nc.sbuf_tensor(f"{name}_u{Pool.UID[0]}", list(shape), dt)), Buf(name)
            self.wp = Pool(nc, pes, "w", 4, [128, 8192], BF16)
            w2p = Pool(nc, pes, "w2_", 2, [128, 44, 256], BF16)
            self.ftp = Pool(nc, pes, "f", 6, [128, T + 2], F32)
            self.lnb = [sb("lnb%d" % i, (128, T)) for i in range(3)]
            xb, xbB = sb("xb2", (128, KC, T), BF16)
            hb, hbB = sb("hb", (128, 44, T), BF16)
            w13 = I["ffn_w13"][0].rearrange("(k p) n -> p k n", p=128)
            w2 = I["ffn_w2"][0].rearrange("(k p) n -> p k n", p=128)
            for ti in range(self.NT):
                tsl = slice(ti * T, (ti + 1) * T)
                c.dma("pool", xb[:], xin.rearrange("(k p) t -> p k t", p=128)[:, :, tsl], "d_xb",
                      reads=[self.db(xin_key, ti, f) for f in range(KC)], writes=[xbB])
                for hb4 in range(11):
                    wgt, wgtB = self.wload(w13[:, :, hb4 * 512:(hb4 + 1) * 512], (128, KC, 512))
                    wup, wupB = self.wload(w13[:, :, FFN + hb4 * 512:FFN + (hb4 + 1) * 512], (128, KC, 512))
                    for hi in range(4):
                        hc = hb4 * 4 + hi
                        hs = slice(hi * 128, (hi + 1) * 128)
                        pg, pgB = self.bank()
                        for k in range(KC):
                            self.mm(pg[:], wgt[:, k, hs], xb[:, k, :], k == 0, k == KC - 1, [wgtB, xbB], pgB)
                        pu, puB = self.bank()
                        for k in range(KC):
                            self.mm(pu[:], wup[:, k, hs], xb[:, k, :], k == 0, k == KC - 1, [wupB, xbB], puB)
                        sg, sgB, _ = self.ftp.get()
                        self.act(sg[:, 0:T], pg[:], AF.Silu, [pgB], [sgB])
                        self.tt("dve", hb[:, hc, :], sg[:, 0:T], pu[:], ALU.mult, [sgB, puB], [hbB])
                self.post_begin()
                for fp in range(8):
                    wt, wtB, key = w2p.get()
                    for k0 in range(0, 44, 11):
                        c.dma("pool", wt[:, k0:k0 + 11, :], w2[:, k0:k0 + 11, fp * 256:(fp + 1) * 256], "d_" + key, writes=[wtB])
                    for fi in range(2):
                        f = fp * 2 + fi
                        py, pyB = self.bank()
                        for k in range(44):
                            self.mm(py[:], wt[:, k, fi * 128:(fi + 1) * 128], hb[:, k, :], k == 0, k == 43, [wtB, hbB], pyB)
                        self.post_chunk(f, py[:], pyB, xin, xin_key, ti, 80)
                self.post_finish(xout, xout_key, ti, 48, 64)
            c.barrier()

    def copy_phase(self, xin, xout, xin_key, xout_key):
        self.phase_i += 1
        if self.stop_after is not None and self.phase_i > self.stop_after:
            return
        nc, c = self.nc, self.c
        c.barrier()
        with contextlib.ExitStack() as pes:
            self.ftp = Pool(nc, pes, "f", 4, [128, T + 2], F32)
            for ti in range(self.NT):
                for f in range(KC):
                    t, b, key = self.ftp.get()
                    c.dma("sp", t[:, 0:T], xin[f * 128:(f + 1) * 128, ti * T:(ti + 1) * T], "d_" + key, reads=[self.db(xin_key, ti, f)], writes=[b])
                    c.dma("sp", xout[f * 128:(f + 1) * 128, ti * T:(ti + 1) * T], t[:, 0:T], "s_" + key, reads=[b], writes=[self.db(xout_key, ti, f)])
            c.barrier()

    def moe_phase(self, xin, xout, xin_key, xout_key):
        self.phase_i += 1
        if self.stop_after is not None and self.phase_i > self.stop_after:
            return
        nc, c, I, G = self.nc, self.c, self.I, self.G
        assert self.NT % 2 == 0
        HP, HC = 7, 8
        c.barrier()
        self.rot_banks = list(range(8))
        self.bi = 0
        with contextlib.ExitStack() as pes:
            def sb(name, shape, dt=F32):
                Pool.UID[0] += 1
                return pes.enter_context(nc.sbuf_tensor(f"{name}_u{Pool.UID[0]}", list(shape), dt)), Buf(name)
            self.wp = Pool(nc, pes, "w", 5, [128, 4096], BF16)
            w2p = Pool(nc, pes, "w2_", 2, [128, HC, 256], BF16)
            self.ftp = Pool(nc, pes, "f", 6, [128, T + 2], F32)
            self.lnb = [sb("lnb%d" % i, (128, T)) for i in range(3)]
            xbs = [sb("xb3_%d" % h, (128, KC, T), BF16) for h in range(2)]
            hb, hbB = sb("hbe", (128, HC, 2 * T), BF16)
            yacc, yaccB = sb("yacc", (128, KC, 2 * T))
            rw, rwB = sb("rw", (128, KC, NE))
            rb4, rb4B = sb("rb4", (128, 4 * NE))
            lg, lgB = sb("lg", (128, 4 * NE))
            gwts = [sb("gwt%d" % h, (128, 4 * NE)) for h in range(2)]
            m8, m8B = sb("m8", (128, 8))
            sm, smB = sb("sm", (128, 4))
            dg, dgB = sb("dg", (128, 128))
            gwbs = [sb("gwb%d" % h, (128, T)) for h in range(2)]
            c.dma("sp", rw[:], I["router_w"].rearrange("(k p) e -> p k e", p=128), "d_rw", writes=[rwB])
            c.dma("sp", rb4[:], I["router_b4"].partition_broadcast(128), "d_rw", writes=[rb4B])
            c.sync_group("d_rw", [rwB, rb4B])
            ident, identB = G["ident"]
            onesf, onesB = G["onesf"]
            for st in range(self.NT // 2):
                for h in range(2):
                    ti = st * 2 + h
                    tsl = slice(ti * T, (ti + 1) * T)
                    xb, xbB = xbs[h]
                    gwt, gwtB = gwts[h]
                    c.dma("pool", xb[:], xin.rearrange("(k p) t -> p k t", p=128)[:, :, tsl], "d_xb%d" % h,
                          reads=[self.db(xin_key, ti, f) for f in range(KC)], writes=[xbB])
                    Lgs = [self.bank() for _ in range(4)]
                    for k in range(KC):
                        xf, xfB, key = self.ftp.get()
                        c.dma("sp", xf[:, 0:T], xin[k * 128:(k + 1) * 128, tsl], "d_" + key, reads=[self.db(xin_key, ti, k)], writes=[xfB])
                        for cc in range(4):
                            self.mm(Lgs[cc][0][:, 0:NE], xf[:, cc * 128:(cc + 1) * 128], rw[:, k, :], k == 0, k == KC - 1,
                                    [xfB, rwB], Lgs[cc][1], inc=(cc == 3 or k == KC - 1))
                    for cc in range(4):
                        self.tt("dve", lg[:, cc * NE:(cc + 1) * NE], Lgs[cc][0][:, 0:NE], rb4[:, cc * NE:(cc + 1) * NE], ALU.add, [Lgs[cc][1], rb4B], [lgB])
                    for cc in range(4):
                        cs_ = slice(cc * NE, (cc + 1) * NE)
                        c.op("dve", lambda: nc.vector.max(out=m8[:], in_=lg[:, cs_]), reads=[lgB], writes=[m8B])
                        self.ts("dve", sm[:, 0:1], m8[:, 0:1], -1.0, None, ALU.mult, None, [m8B], [smB])
                        self.act(gwt[:, cs_], lg[:, cs_], AF.Exp, [lgB, smB], [gwtB], bias=sm[:, 0:1], scale=1.0)
                        self.stt("dve", gwt[:, cs_], lg[:, cs_], m8[:, 1:2], gwt[:, cs_], ALU.is_ge, ALU.mult, [lgB, m8B, gwtB], [gwtB])
                        c.op("dve", lambda: nc.vector.reduce_sum(out=sm[:, 1:2], in_=gwt[:, cs_], axis=mybir.AxisListType.X), reads=[gwtB], writes=[smB])
                        c.op("dve", lambda: nc.vector.reciprocal(out=sm[:, 2:3], in_=sm[:, 1:2]), reads=[smB], writes=[smB])
                        self.ts("dve", gwt[:, cs_], gwt[:, cs_], sm[:, 2:3], None, ALU.mult, None, [gwtB, smB], [gwtB])
                for e in range(NE):
                    for h in range(2):
                        gwt, gwtB = gwts[h]
                        gwb, gwbB = gwbs[h]
                        pgw, pgwB = self.bank()
                        for cc in range(4):
                            self.ts("dve", dg[:], ident[:], gwt[:, cc * NE + e:cc * NE + e + 1], None, ALU.mult, None, [identB, gwtB], [dgB])
                            self.mm(pgw[:, cc * 128:(cc + 1) * 128], onesf[:], dg[:], True, True, [onesB, dgB], pgwB, inc=True)
                        self.act(gwb[:], pgw[:], AF.Copy, [pgwB], [gwbB])
                    w13 = I["moe_w13"][e].rearrange("(k p) n -> p k n", p=128)
                    w2 = I["moe_w2"][e].rearrange("(k p) n -> p k n", p=128)
                    for hp in range(HP):
                        for hb2 in range(HC // 2):
                            col = (hp * HC + hb2 * 2) * 128
                            wgt, wgtB = self.wload(w13[:, :, col:col + 256], (128, KC, 256))
                            wup, wupB = self.wload(w13[:, :, EXP + col:EXP + col + 256], (128, KC, 256))
                            for hi in range(2):
                                hc = hb2 * 2 + hi
                                hs = slice(hi * 128, (hi + 1) * 128)
                                for h in range(2):
                                    xb, xbB = xbs[h]
                                    pg, pgB = self.bank()
                                    for k in range(KC):
                                        self.mm(pg[:], wgt[:, k, hs], xb[:, k, :], k == 0, k == KC - 1, [wgtB, xbB], pgB)
                                    pu, puB = self.bank()
                                    for k in range(KC):
                                        self.mm(pu[:], wup[:, k, hs], xb[:, k, :], k == 0, k == KC - 1, [wupB, xbB], puB)
                                    sg, sgB, _ = self.ftp.get()
                                    self.act(sg[:, 0:T], pg[:], AF.Silu, [pgB], [sgB])
                                    self.tt("dve", sg[:, 0:T], sg[:, 0:T], pu[:], ALU.mult, [sgB, puB], [sgB])
                                    self.tt("dve", hb[:, hc, h * T:(h + 1) * T], sg[:, 0:T], gwbs[h][0][:], ALU.mult, [sgB, gwbs[h][1]], [hbB])
                        for fp in range(KC // 2):
                            wt, wtB, key = w2p.get()
                            c.dma("pool", wt[:], w2[:, hp * HC:(hp + 1) * HC, fp * 256:(fp + 1) * 256], "d_" + key, writes=[wtB])
                            for fi in range(2):
                                f = fp * 2 + fi
                                for h in range(2):
                                    py, pyB = self.bank()
                                    for k in range(HC):
                                        self.mm(py[:], wt[:, k, fi * 128:(fi + 1) * 128], hb[:, k, h * T:(h + 1) * T], k == 0, k == HC - 1, [wtB, hbB], pyB)
                                    ysl = yacc[:, f, h * T:(h + 1) * T]
                                    if e == 0 and hp == 0:
                                        self.act(ysl, py[:], AF.Copy, [pyB], [yaccB])
                                    else:
                                        self.tt("dve", ysl, ysl, py[:], ALU.add, [yaccB, pyB], [yaccB])
                for h in range(2):
                    ti = st * 2 + h
                    self.post_begin()
                    for f in range(KC):
                        self.post_chunk(f, yacc[:, f, h * T:(h + 1) * T], yaccB, xin, xin_key, ti, 80)
                    self.post_finish(xout, xout_key, ti, 48, 64)
            c.barrier()
        self.rot_banks = [0, 1, 2, 3, 4, 7]
        self.bi = 0


def _consts():
    ident = np.eye(128, dtype=np.float32)
    swapm = np.zeros((128, 128), np.float32)
    for n in range(64):
        swapm[n, 64 + n] = 1.0
        swapm[64 + n, n] = 1.0
    tril = np.triu(np.ones((128, 128), np.float32))
    iota = np.tile(np.arange(T, dtype=np.float32)[None, :], (128, 1))
    sgn = np.ones((128, 2), np.float32)
    sgn[:64, 0] = -1.0
    sgn[64:, 1] = -1.0
    rowmask = np.zeros((128, 8), np.float32)
    for j in range(8):
        rowmask[j * 16:(j + 1) * 16, j] = 1.0
    return {"ident": ident, "swapm": swapm, "tril": tril, "iota": iota, "sgn": sgn, "rowmask": rowmask}


def layout_weights(w, do_moe=True):
    f = lambda a: np.ascontiguousarray(np.asarray(a, dtype=np.float32))
    L = 2
    o = dict(_consts())
    o["w_in"] = f(w["w_in"])
    are = np.transpose(np.asarray(w["ssm_a_re"]), (0, 2, 1))
    aim = np.transpose(np.asarray(w["ssm_a_im"]), (0, 2, 1))
    o["a_re2"] = f(np.concatenate([are, are], axis=1))
    o["a_im2"] = f(np.concatenate([aim, aim], axis=1))
    o["ldt2"] = f(np.broadcast_to(np.asarray(w["ssm_log_dt"])[:, None, :], (L, 128, 32)))
    bre = np.transpose(np.asarray(w["ssm_b_re"]), (0, 2, 1, 3))
    bim = np.transpose(np.asarray(w["ssm_b_im"]), (0, 2, 1, 3))
    o["bA"] = f(np.concatenate([bre, bim], axis=1))
    o["bB"] = f(np.concatenate([bim, bre], axis=1))
    cre = np.transpose(np.asarray(w["ssm_c_re"]), (0, 3, 1, 2))
    cim = np.transpose(np.asarray(w["ssm_c_im"]), (0, 3, 1, 2))
    o["cA"] = f(np.concatenate([cre, cim], axis=1))
    o["cB"] = f(np.concatenate([cim, cre], axis=1))
    o["dcol"] = f(np.transpose(np.asarray(w["ssm_d"]).reshape(L, 4, 128), (0, 2, 1)))
    o["glu_w"] = f(w["glu_w"])
    o["glu_bc"] = f(np.transpose(np.asarray(w["glu_b"]).reshape(L, 4, 128), (0, 2, 1)))
    o["sg_ln_g"] = f(w["sg_ln_g"])
    o["sg_ln_b"] = f(w["sg_ln_b"])
    o["sgwT"] = f(np.transpose(np.asarray(w["sg_w"]), (0, 1, 3, 2)))
    o["sg_b"] = f(np.asarray(w["sg_b"]).reshape(L, 1, 1024))
    o["convw"] = f(np.transpose(np.asarray(w["conv_w"])[:, :, 0, :].reshape(L, 3, 6, 128), (0, 3, 2, 1)))
    for k in ("w_branch_a", "w_branch_b", "w_branch_c", "w_o", "ada_w", "ffn_w13", "ffn_w2"):
        o[k] = f(w[k])
    o["ada_bc"] = f(np.transpose(np.asarray(w["ada_b"]).reshape(L, 96, 128), (0, 2, 1)))
    if do_moe:
        o["router_w"] = f(np.asarray(w["moe_router_w"])[0])
        o["router_b4"] = f(np.tile(np.asarray(w["moe_router_b"])[0][None, :], (1, 4)))
        o["moe_w13"] = f(np.asarray(w["moe_w13"])[0])
        o["moe_w2"] = f(np.asarray(w["moe_w2"])[0])
    return o


def run_single(x, c, w, do_moe=True, debug=False, n_layers=2, trace=False, cores=None):
    x = np.asarray(x, dtype=np.float32)
    c = np.asarray(c, dtype=np.float32)
    NT = x.shape[1] // T
    prog = Prog(NT, do_moe=do_moe, debug=debug, n_layers=n_layers, single=True)
    shared = layout_weights(w, do_moe)
    in_maps = []
    for b in range(x.shape[0]):
        m = {k: v for k, v in shared.items() if k in prog.I}
        m["xB"] = np.ascontiguousarray(x[b].T)
        m["cT"] = np.ascontiguousarray(c[b].reshape(KC, 128).T)
        m["flag"] = np.zeros((128, 1), np.float32)
        m = {k: v for k, v in m.items() if k in prog.I}
        in_maps.append(m)
    cores = list(range(len(in_maps))) if cores is None else cores
    res = run_bass_kernel_spmd(prog.nc, [in_maps[i] for i in cores], core_ids=list(range(len(cores))), **({"trace": True} if trace else {}))
    out = np.zeros_like(x)
    for i, b in enumerate(cores):
        out[b] = res.results[i]["outT"].T
    return out, res


def run(x, c, w, NT, do_moe=True, debug=False, n_layers=2, trace=False, stop_after=None, cores=None):
    prog = Prog(NT, do_moe=do_moe, debug=debug, n_layers=n_layers, stop_after=stop_after)
    shared = layout_weights(w, do_moe)
    NTOK = NT * T
    in_maps = []
    x = np.asarray(x, dtype=np.float32)
    c = np.asarray(c, dtype=np.float32)
    for core in range(8):
        b, half = core // 2, core % 2
        m = {k: v for k, v in shared.items() if k in prog.I}
        xB = np.ascontiguousarray(x[b, half * NTOK:(half + 1) * NTOK, :].T)
        xA = np.ascontiguousarray(x[b, 0:NTOK, :].T) if half == 1 else np.zeros((D, NTOK), np.float32)
        m["xA"], m["xB"] = xA, xB
        m["cT"] = np.ascontiguousarray(c[b].reshape(KC, 128).T)
        m["flag"] = np.full((128, 1), float(half), np.float32)
        m = {k: v for k, v in m.items() if k in prog.I}
        in_maps.append(m)
    cores = list(range(8)) if cores is None else cores
    res = run_bass_kernel_spmd(prog.nc, [in_maps[i] for i in cores], core_ids=list(range(len(cores))), **({"trace": True} if trace else {}))
    res.results = {core: res.results[i] for i, core in enumerate(cores)}
    out = np.zeros((4, 2 * NTOK, D), np.float32)
    for core in cores:
        b, half = core // 2, core % 2
        out[b, half * NTOK:(half + 1) * NTOK, :] = res.results[core]["outT"].T
    return out, res


def kernel(**inputs):
    x = np.asarray(inputs["x"])
    out, _ = run(x, inputs["c"], inputs, NT=x.shape[1] // (2 * T), do_moe=True)
    return out
```
